# Optimizing a Trainium2 kernel written in Bass

```python
import math
import jax, jax.numpy as jnp
from jax import lax
import numpy as np

D_MODEL = 2048
BATCH = 2
SEQ = 16384
DEPTH = 1

POOL_WIDTH = D_MODEL // 2
POOL_WINDOWS = (2, 4, 8, 16)
N_POOL_GROUPS = len(POOL_WINDOWS)
POOL_GROUP = POOL_WIDTH // N_POOL_GROUPS
ATTN_WIDTH = D_MODEL - POOL_WIDTH
DIFF_HEAD_DIM = 64
N_DIFF_HEADS = ATTN_WIDTH // (2 * DIFF_HEAD_DIM)
DIFF_V_DIM = 2 * DIFF_HEAD_DIM
ROPE_DIM = DIFF_HEAD_DIM // 4
ROPE_THETA = 500000.0
Q_BLOCK = 128
IN_WIDTH = POOL_WIDTH + 3 * ATTN_WIDTH
N_GROUPS = 8
EXPERTS_PER_GROUP = 8
N_EXPERTS = N_GROUPS * EXPERTS_PER_GROUP
TOP_K_INNER = 2
D_EXPERT = D_MODEL // 4
MOE_BLOCK = 256
EPS = 1e-6

kernel_name = "hybrid_pool_diffattn_hmoe"


def rmsnorm(x, g):
    xf = x.astype(jnp.float32)
    y = xf * lax.rsqrt(jnp.mean(xf * xf, axis=-1, keepdims=True) + EPS)
    return (y * g.astype(jnp.float32)).astype(x.dtype)


def rope_tables(seq):
    inv = ROPE_THETA ** (-jnp.arange(0, ROPE_DIM, 2, dtype=jnp.float32) / ROPE_DIM)
    ang = jnp.arange(seq, dtype=jnp.float32)[:, None] * inv[None, :]
    ang = jnp.concatenate([ang, ang], axis=-1)
    return jnp.cos(ang), jnp.sin(ang)


def apply_partial_rope(t, cos, sin):
    rot = t[..., :ROPE_DIM].astype(jnp.float32)
    rest = t[..., ROPE_DIM:]
    half = ROPE_DIM // 2
    rotated = jnp.concatenate([-rot[..., half:], rot[..., :half]], axis=-1)
    c = cos[None, :, None, None, :]
    s = sin[None, :, None, None, :]
    rot = rot * c + rotated * s
    return jnp.concatenate([rot.astype(t.dtype), rest], axis=-1)


def pool_mixer(u, w_pool, pool_scale):
    B, S, _ = u.shape
    uf = u.astype(jnp.float32).reshape(B, S, N_POOL_GROUPS, POOL_GROUP)
    csum = jnp.cumsum(uf, axis=1)
    pos = jnp.arange(S)
    outs = []
    for g, w in enumerate(POOL_WINDOWS):
        c = csum[:, :, g]
        c_prev = jnp.pad(c, ((0, 0), (w, 0), (0, 0)))[:, :S]
        count = jnp.minimum(pos + 1, w).astype(jnp.float32)[None, :, None]
        outs.append((c - c_prev) / count - uf[:, :, g])
    pooled = jnp.stack(outs, axis=2).astype(u.dtype)
    mixed = jnp.einsum('bsgc,gcd->bsgd', pooled, w_pool)
    return mixed.reshape(B, S, POOL_WIDTH) * pool_scale


def diff_attention(q, k, v, lam, subln_g, lam_init):
    B, S, H = q.shape[0], q.shape[1], q.shape[2]
    n_blk = S // Q_BLOCK
    q_blocks = (q * (DIFF_HEAD_DIM ** -0.5)).reshape(B, n_blk, Q_BLOCK, H, 2, DIFF_HEAD_DIM)
    q_blocks = q_blocks.transpose(1, 0, 2, 3, 4, 5)
    k_pos = jnp.arange(S)

    def one_block(args):
        qb, start = args
        s = jnp.einsum('bqhcd,bkhcd->bhcqk', qb, k, preferred_element_type=jnp.float32)
        q_pos = start + jnp.arange(Q_BLOCK)
        mask = k_pos[None, :] <= q_pos[:, None]
        s = jnp.where(mask, s, -jnp.inf)
        p = jax.nn.softmax(s, axis=-1)
        a = p[:, :, 0] - lam * p[:, :, 1]
        return jnp.einsum('bhqk,bkhe->bqhe', a.astype(v.dtype), v)

    starts = jnp.arange(n_blk) * Q_BLOCK
    o = lax.map(one_block, (q_blocks, starts))
    o = o.transpose(1, 0, 2, 3, 4).reshape(B, S, H, DIFF_V_DIM)
    o = rmsnorm(o, subln_g) * (1.0 - lam_init)
    return o.reshape(B, S, ATTN_WIDTH)


def hier_moe(x, w_grp, b_grp, w_exp, b_exp, w_gate, w_up, w_down):
    B, S, D = x.shape
    N = B * S
    xt = x.reshape(N, D)
    grp_prob = jax.nn.softmax(jnp.matmul(xt, w_grp).astype(jnp.float32) + b_grp.astype(jnp.float32), axis=-1)
    grp_gate, grp_idx = lax.top_k(grp_prob, 1)
    exp_logits = jnp.einsum('nd,gde->nge', xt, w_exp).astype(jnp.float32) + b_exp.astype(jnp.float32)
    sel_logits = jnp.take_along_axis(exp_logits, grp_idx[:, :, None], axis=1)[:, 0]
    in_logits, in_idx = lax.top_k(sel_logits, TOP_K_INNER)
    gates = grp_gate * jax.nn.softmax(in_logits, axis=-1)
    expert_id = grp_idx * EXPERTS_PER_GROUP + in_idx

    A = N * TOP_K_INNER
    flat_e = expert_id.reshape(A).astype(jnp.int32)
    flat_tok = jnp.repeat(jnp.arange(N, dtype=jnp.int32), TOP_K_INNER)
    flat_g = gates.reshape(A)
    order = jnp.argsort(flat_e)
    e_sorted = flat_e[order]
    counts = jnp.bincount(flat_e, length=N_EXPERTS)
    starts = jnp.cumsum(counts) - counts
    padded = (counts + MOE_BLOCK - 1) // MOE_BLOCK * MOE_BLOCK
    pends = jnp.cumsum(padded)
    pstarts = pends - padded
    dest = pstarts[e_sorted] + (jnp.arange(A) - starts[e_sorted])
    n_blocks = -(-(A + N_EXPERTS * (MOE_BLOCK - 1)) // MOE_BLOCK)
    P = n_blocks * MOE_BLOCK
    row_tok = jnp.full((P,), N, jnp.int32).at[dest].set(flat_tok[order])
    row_gate = jnp.zeros((P,), jnp.float32).at[dest].set(flat_g[order])
    blk_start = jnp.arange(n_blocks) * MOE_BLOCK
    blk_expert = jnp.minimum(jnp.searchsorted(pends, blk_start, side='right'), N_EXPERTS - 1)

    x_pad = jnp.concatenate([xt, jnp.zeros((1, D), xt.dtype)], axis=0)
    xs = x_pad[row_tok].reshape(n_blocks, MOE_BLOCK, D)

    def expert_block(args):
        xb, e = args
        h = jax.nn.silu(xb @ w_gate[e]) * (xb @ w_up[e])
        return h @ w_down[e]

    ys = lax.map(expert_block, (xs, blk_expert)).reshape(P, D)
    ys = ys * row_gate[:, None].astype(ys.dtype)
    out = jax.ops.segment_sum(ys, row_tok, num_segments=N + 1)[:N]
    return out.reshape(B, S, D)


def setup_inputs(seed: int = 0) -> dict:
    key = jax.random.key(seed)
    ks = jax.random.split(key, 20)
    f32 = jnp.float32
    L = DEPTH
    nrm = lambda k, shape, scale: jax.random.normal(k, shape, f32) * scale
    return {
        "x": jax.random.normal(ks[0], (BATCH, SEQ, D_MODEL), f32),
        "norm_mix_g": 1.0 + nrm(ks[1], (L, D_MODEL), 0.02),
        "w_in": nrm(ks[2], (L, D_MODEL, IN_WIDTH), D_MODEL ** -0.5),
        "w_pool": nrm(ks[3], (L, N_POOL_GROUPS, POOL_GROUP, POOL_GROUP), POOL_GROUP ** -0.5),
        "pool_scale": 1.0 + nrm(ks[4], (L, POOL_WIDTH), 0.02),
        "lambda_q1": nrm(ks[5], (L, DIFF_HEAD_DIM), 0.1),
        "lambda_k1": nrm(ks[6], (L, DIFF_HEAD_DIM), 0.1),
        "lambda_q2": nrm(ks[7], (L, DIFF_HEAD_DIM), 0.1),
        "lambda_k2": nrm(ks[8], (L, DIFF_HEAD_DIM), 0.1),
        "subln_g": 1.0 + nrm(ks[9], (L, DIFF_V_DIM), 0.02),
        "w_out": nrm(ks[10], (L, D_MODEL, D_MODEL), D_MODEL ** -0.5),
        "norm_ffn_g": 1.0 + nrm(ks[11], (L, D_MODEL), 0.02),
        "w_grp": nrm(ks[12], (L, D_MODEL, N_GROUPS), D_MODEL ** -0.5),
        "b_grp": nrm(ks[13], (L, N_GROUPS), 0.01),
        "w_exp": nrm(ks[14], (L, N_GROUPS, D_MODEL, EXPERTS_PER_GROUP), D_MODEL ** -0.5),
        "b_exp": nrm(ks[15], (L, N_GROUPS, EXPERTS_PER_GROUP), 0.01),
        "w_gate": nrm(ks[16], (L, N_EXPERTS, D_MODEL, D_EXPERT), D_MODEL ** -0.5),
        "w_up": nrm(ks[17], (L, N_EXPERTS, D_MODEL, D_EXPERT), D_MODEL ** -0.5),
        "w_down": nrm(ks[18], (L, N_EXPERTS, D_EXPERT, D_MODEL), D_EXPERT ** -0.5),
        "norm_final_g": 1.0 + nrm(ks[19], (D_MODEL,), 0.02),
    }


def reference(x, norm_mix_g, w_in, w_pool, pool_scale, lambda_q1, lambda_k1, lambda_q2, lambda_k2,
              subln_g, w_out, norm_ffn_g, w_grp, b_grp, w_exp, b_exp, w_gate, w_up, w_down,
              norm_final_g):
    B, S, _ = x.shape
    H, d = N_DIFF_HEADS, DIFF_HEAD_DIM
    cos, sin = rope_tables(S)
    h = x
    for l in range(DEPTH):
        lam_init = 0.8 - 0.6 * math.exp(-0.3 * l)
        u = rmsnorm(h, norm_mix_g[l])
        proj = jnp.matmul(u, w_in[l])
        pool_in = proj[..., :POOL_WIDTH]
        q = proj[..., POOL_WIDTH:POOL_WIDTH + ATTN_WIDTH].reshape(B, S, H, 2, d)
        k = proj[..., POOL_WIDTH + ATTN_WIDTH:POOL_WIDTH + 2 * ATTN_WIDTH].reshape(B, S, H, 2, d)
        v = proj[..., POOL_WIDTH + 2 * ATTN_WIDTH:].reshape(B, S, H, DIFF_V_DIM)
        q = apply_partial_rope(q, cos, sin)
        k = apply_partial_rope(k, cos, sin)
        lq1 = lambda_q1[l].astype(jnp.float32)
        lk1 = lambda_k1[l].astype(jnp.float32)
        lq2 = lambda_q2[l].astype(jnp.float32)
        lk2 = lambda_k2[l].astype(jnp.float32)
        lam = jnp.exp(jnp.sum(lq1 * lk1)) - jnp.exp(jnp.sum(lq2 * lk2)) + lam_init
        pool_out = pool_mixer(pool_in, w_pool[l], pool_scale[l])
        attn_out = diff_attention(q, k, v, lam, subln_g[l], lam_init)
        mixed = jnp.concatenate([pool_out.astype(h.dtype), attn_out.astype(h.dtype)], axis=-1)
        h = h + jnp.matmul(mixed, w_out[l])
        h = h + hier_moe(rmsnorm(h, norm_ffn_g[l]), w_grp[l], b_grp[l], w_exp[l], b_exp[l],
                         w_gate[l], w_up[l], w_down[l])
    return rmsnorm(h, norm_final_g)
```

```python
from contextlib import ExitStack
import math
import numpy as np
import ml_dtypes
import concourse.bass as bass
import concourse.mybir as mybir
from concourse.bass_utils import run_bass_kernel_spmd

F32 = mybir.dt.float32
BF16 = mybir.dt.bfloat16
I32 = mybir.dt.int32
ACTF = mybir.ActivationFunctionType
ALU = mybir.AluOpType
AX = mybir.AxisListType

CFG = dict(S=16384, NG=8)
D = 2048
KC = 16
NH = 8
EPS = 1e-6
LAM_INIT = 0.8 - 0.6 * math.exp(0.0)
WINS = (2, 4, 8, 16)
MB = 256


class Buf:
    __slots__ = ("w", "r")

    def __init__(self):
        self.w = None
        self.r = []


class Op:
    __slots__ = ("eng", "fn", "deps", "signal", "sig", "dma", "dsem", "dval", "ring_wait")

    def __init__(self, eng, fn, dma):
        self.eng = eng
        self.fn = fn
        self.dma = dma
        self.deps = []
        self.signal = False
        self.sig = 0
        self.dsem = None
        self.dval = 0
        self.ring_wait = None


class Sched:
    ENGS = ("pe", "act", "dve", "pool", "sp")
    RING = {"sp": 8, "pool": 8, "act": 2}

    def __init__(self, nc):
        self.nc = nc
        self.q = {e: [] for e in self.ENGS}
        self.ndma = {e: 0 for e in self.ENGS}
        self.dma_since = []

    def add(self, eng, fn, reads=(), writes=(), dma=False, extra=()):
        op = Op(eng, fn, dma)
        raw = set()
        other = set(extra)
        for b in reads:
            if b.w is not None:
                raw.add(b.w)
        for b in writes:
            if b.w is not None:
                other.add(b.w)
            other.update(b.r)
        for b in reads:
            b.r.append(op)
        for b in writes:
            b.w = op
            b.r = []
        deps = []
        for d in raw | other:
            if d is op:
                continue
            if (not d.dma) and d.eng == eng and not dma and d not in extra:
                if eng == "pe" or d not in raw:
                    continue
            deps.append(d)
        op.deps = deps
        if dma:
            n = self.ndma[eng]
            self.ndma[eng] = n + 1
            K = self.RING[eng]
            op.dsem = n % K
            op.dval = 16 * (n // K + 1)
            if n >= K:
                op.ring_wait = (n % K, 16 * (n // K))
            self.dma_since.append(op)
        self.q[eng].append(op)
        return op

    def barrier(self):
        last = [self.q[e][-1] for e in ("pe", "act", "dve", "pool") if self.q[e]]
        last = [o for o in last if o.fn is not None]
        lasts = []
        for e in ("pe", "act", "dve", "pool"):
            for o in reversed(self.q[e]):
                if o.fn is not None and not o.dma:
                    lasts.append(o)
                    break
        dm = list(self.dma_since)
        self.dma_since = []
        for e in self.ENGS:
            op = Op(e, None, False)
            op.deps = [o for o in lasts if o.eng != e] + dm
            self.q[e].append(op)

    def emit(self, stack):
        nc = self.nc
        for e in self.ENGS:
            for op in self.q[e]:
                for d in op.deps:
                    if not d.dma:
                        d.signal = True
        for e in self.ENGS:
            c = 0
            for op in self.q[e]:
                if op.signal and not op.dma:
                    c += 1
                    op.sig = c
        csem = {e: stack.enter_context(nc.semaphore("c_" + e)) for e in ("pe", "act", "dve", "pool")}
        rsem = {e: [stack.enter_context(nc.semaphore("r_%s%d" % (e, i))) for i in range(self.RING[e])]
                for e in ("sp", "pool", "act")}
        block = stack.enter_context(nc.Block())
        engmap = {"pe": block.tensor, "act": block.scalar, "dve": block.vector, "pool": block.gpsimd,
                  "sp": block.sync}

        def mk(e):
            ops = self.q[e]

            def body(eng):
                waited = {}

                def wait(sem, key, val):
                    if waited.get(key, 0) >= val:
                        return
                    waited[key] = val
                    eng.wait_ge(sem, val)

                for op in ops:
                    for d in op.deps:
                        if d.dma:
                            wait(rsem[d.eng][d.dsem], (d.eng, d.dsem), d.dval)
                        else:
                            wait(csem[d.eng], d.eng, d.sig)
                    if op.ring_wait is not None:
                        wait(rsem[e][op.ring_wait[0]], (e, op.ring_wait[0]), op.ring_wait[1])
                    if op.fn is None:
                        continue
                    ins = op.fn(eng)
                    if op.dma:
                        ins.then_inc(rsem[e][op.dsem], 16)
                    elif op.signal:
                        ins.then_inc(csem[e], 1)
                if e in rsem:
                    n = self.ndma[e]
                    K = self.RING[e]
                    for s in range(min(n, K)):
                        wait(rsem[e][s], (e, s), 16 * ((n - 1 - s) // K + 1))
            return body

        for e in self.ENGS:
            engmap[e](mk(e))


def build(cfg, debug=False):
    S_ = cfg["S"]
    NG = cfg["NG"]
    NE = NG * 8
    NCH = S_ // 2048
    NQ = NCH * 512
    NTQ = NQ // 128
    NTKV = S_ // 128
    NR = 8 + NE
    NB = -(-(2 * NQ + NE * (MB - 1)) // MB)
    NBMAX = -(-NQ // MB)

    nc = bass.Bass("TRN2", target_bir_lowering=False)
    din = lambda n, s, dt=F32: nc.dram_tensor(n, list(s), dt, kind="ExternalInput").ap()
    dscr = lambda n, s, dt: nc.dram_tensor(n, list(s), dt, kind="Internal").ap()
    xkv = din("xkv", [S_, D])
    xq = din("xq", [NCH, 640, D])
    cs_kv_d = din("cs_kv", [128, NTKV, 32])
    cs_q_d = din("cs_q", [128, NTQ, 32])
    masks_d = din("masks", [128, 16, 512], BF16)
    bands_d = din("bands", [128, 3, 4, 128], BF16)
    cst_d = din("cst", [128, 3, 128], BF16)
    thr_d = din("thr", [128, NE, NBMAX])
    bst_d = din("bst", [128, NB, NE])
    iop_d = din("iop", [128, 1])
    g_mix = din("norm_mix_g", [1, D])
    w_in = din("w_in", [D, 4096])
    w_pool = din("w_pool", [4, 256, 256])
    pool_scale = din("pool_scale", [128, 8])
    lams = din("lams", [1, 256])
    subln_g = din("subln_g", [128, 1])
    w_out = din("w_out", [D, D])
    g_ffn = din("norm_ffn_g", [1, D])
    w_r = din("w_r", [D, NR])
    b_r = din("b_r", [1, NR])
    w_gate = din("w_gate", [NE, D, 512])
    w_up = din("w_up", [NE, D, 512])
    w_down = din("w_down", [NE, 512, D])
    g_fin = din("norm_final_g", [1, D])
    out_d = nc.dram_tensor("out", [NQ, D], F32, kind="ExternalOutput").ap()
    dbg = {}
    if debug:
        dbg["mixT"] = nc.dram_tensor("dbg_mixT", [16, 128, NQ], BF16, kind="ExternalOutput").ap()
        dbg["h"] = nc.dram_tensor("dbg_h", [NQ, D], F32, kind="ExternalOutput").ap()
        dbg["lall"] = nc.dram_tensor("dbg_lall", [128, NTQ, NR], F32, kind="ExternalOutput").ap()
        dbg["dest"] = nc.dram_tensor("dbg_dest", [128, 2, NTQ], I32, kind="ExternalOutput").ap()
        dbg["gate"] = nc.dram_tensor("dbg_gate", [128, 2, NTQ], F32, kind="ExternalOutput").ap()
        dbg["bexp"] = nc.dram_tensor("dbg_bexp", [128, NB], I32, kind="ExternalOutput").ap()

    KT_d = dscr("KT_d", [NH, 128, S_], BF16)
    V_d = dscr("V_d", [NH, 128, NTKV, 128], BF16)
    QT_d = dscr("QT_d", [NH, 128, NQ], BF16)
    MIXT_d = dbg["mixT"] if debug else dscr("MIXT_d", [16, 128, NQ], BF16)
    H_d = dbg["h"] if debug else dscr("H_d", [NQ, D], F32)
    HN_d = dscr("HN_d", [NQ, D], BF16)
    XS_d = dscr("XS_d", [NB * MB, D], BF16)
    YS_d = dscr("YS_d", [NB * MB, D], BF16)

    S = Sched(nc)
    A = S.add

    def DMA(q, out, in_, r=(), w=()):
        return S.add(q, lambda e: e.dma_start(out=out, in_=in_), r, w, dma=True)

    with ExitStack() as st:
        ARENA = 94000
        arena = st.enter_context(nc.sbuf_tensor("arena", [128, ARENA], BF16))
        psum = st.enter_context(nc.psum_tensor("psum", [128, 8, 512], F32))
        state = {"off": 0, "base": 0}

        def carve(shape, dt):
            n = int(np.prod(shape[1:]))
            nb = n * (2 if dt in (F32, I32) else 1)
            nb = (nb + 15) // 16 * 16
            o = state["off"]
            assert o + nb <= ARENA, ("SBUF arena overflow", o, nb)
            state["off"] = o + nb
            v = arena[:, o:o + nb]
            if dt != BF16:
                v = v.bitcast(dt)
            v = v[:, 0:n]
            if len(shape) == 3:
                v = v.rearrange("p (a b) -> p a b", b=shape[2])
            elif len(shape) == 4:
                v = v.rearrange("p (a b c) -> p a b c", b=shape[2], c=shape[3])
            return v

        def new_phase():
            S.barrier()
            state["off"] = state["base"]

        def pbank(b, n=1, dt=F32):
            v = psum[:, b:b + n, :].rearrange("p a b -> p (a b)")
            if dt != F32:
                v = v.bitcast(dt)
            return v

        cst = carve([128, 3, 128], BF16); b_cst = Buf()
        ident, ones, tri = cst[:, 0, :], cst[:, 1, :], cst[:, 2, :]
        gmix_bc = carve([128, D], F32); b_gmix = Buf()
        lall = carve([128, NTQ, NR], F32); b_lall = Buf()
        neglam = carve([128, 1], F32); b_neglam = Buf()
        gsc = carve([128, 1], F32); b_gsc = Buf()
        iop = carve([128, 1], F32); b_iop = Buf()
        dest_i = carve([128, 2, NTQ], I32); b_dest = Buf()
        gates = carve([128, 2, NTQ], F32); b_gates = Buf()
        widx = carve([128, NB], I32); b_widx = Buf()
        small = carve([128, 8], F32)
        DMA("sp", cst, cst_d, w=[b_cst])
        DMA("sp", gmix_bc, g_mix.partition_broadcast(128).rearrange("p o d -> p (o d)"), w=[b_gmix])
        DMA("sp", iop, iop_d, w=[b_iop])
        state["base"] = state["off"]

        lam_t = carve([128, 256], F32); b_lamt = Buf()
        lam_p = carve([128, 128], F32); b_lamp = Buf()
        lam_s = carve([128, 2], F32); b_lams = Buf()
        DMA("sp", lam_t, lams.partition_broadcast(128).rearrange("p o d -> p (o d)"), w=[b_lamt])
        lv = lam_t.rearrange("p (a b c) -> p a b c", a=2, b=2)
        A("dve", lambda e: e.tensor_tensor(out=lam_p.rearrange("p (a c) -> p a c", a=2), in0=lv[:, :, 0, :],
                                           in1=lv[:, :, 1, :], op=ALU.mult), [b_lamt], [b_lamp])
        A("dve", lambda e: e.tensor_reduce(out=lam_s, in_=lam_p.rearrange("p (a c) -> p a c", a=2), axis=AX.X,
                                           op=ALU.add), [b_lamp], [b_lams])
        A("act", lambda e: e.activation(out=lam_s, in_=lam_s, func=ACTF.Exp), [b_lams], [b_lams])
        A("dve", lambda e: e.scalar_tensor_tensor(out=neglam, in0=lam_s[:, 1:2], scalar=-LAM_INIT, in1=lam_s[:, 0:1],
                                                  op0=ALU.add, op1=ALU.subtract), [b_lams], [b_neglam])
        DMA("sp", gsc, subln_g, w=[b_gsc])
        A("dve", lambda e: e.tensor_scalar(out=gsc, in0=gsc, scalar1=1.0 - LAM_INIT, scalar2=None, op0=ALU.mult),
          [b_gsc], [b_gsc])

        def norm_transpose(src, xt, b_xt, junk, b_junk, ss, b_ss, xb, b_xb, xT, b_xT, gbc, b_gbc, pT, b_pT):
            DMA("sp", xt, src, w=[b_xt])
            A("act", lambda e: e.activation(out=junk, in_=xt, func=ACTF.Square, accum_out=ss), [b_xt], [b_junk, b_ss])
            A("act", lambda e: e.activation(out=ss, in_=ss, func=ACTF.Sqrt, scale=1.0 / D, bias=EPS), [b_ss], [b_ss])
            A("dve", lambda e: e.reciprocal(out=ss, in_=ss), [b_ss], [b_ss])
            A("dve", lambda e: e.scalar_tensor_tensor(out=xb, in0=xt, scalar=ss[:, 0:1], in1=gbc, op0=ALU.mult,
                                                      op1=ALU.mult), [b_xt, b_ss, b_gbc], [b_xb])
            if xT is None:
                return
            for k in range(KC):
                A("pe", lambda e, k=k: e.transpose(out=pT[:, k * 128:(k + 1) * 128], in_=xb[:, k * 128:(k + 1) * 128],
                                                   identity=ident), [b_xb, b_cst], [b_pT])
            A("act", lambda e: e.activation(out=xT[:, 0:8, :].rearrange("p a b -> p (a b)"), in_=pT[:, 0:1024],
                                            func=ACTF.Copy), [b_pT], [b_xT])
            A("dve", lambda e: e.tensor_copy(out=xT[:, 8:16, :].rearrange("p a b -> p (a b)"), in_=pT[:, 1024:2048]),
              [b_pT], [b_xT])

        def rope(pk, cs_t, ksb, tmp1, tmp2, rb, wb, b_tmp):
            for hb in range(2):
                pv = pk[:, hb * 512:(hb + 1) * 512].rearrange("p (g d) -> p g d", d=64)
                kv = ksb[:, hb * 512:(hb + 1) * 512].rearrange("p (g d) -> p g d", d=64)
                t1 = tmp1[:, hb * 8:(hb + 1) * 8, :]
                t2 = tmp2[:, hb * 8:(hb + 1) * 8, :]
                cosb = cs_t[:, 0:16].unsqueeze(1).to_broadcast([128, 8, 16])
                s0 = cs_t[:, 16:24].unsqueeze(1).to_broadcast([128, 8, 8])
                s1 = cs_t[:, 24:32].unsqueeze(1).to_broadcast([128, 8, 8])
                import os as _os
                RV = 3
                A("act", lambda e, pv=pv, kv=kv: e.activation(out=kv[:, :, 16:64], in_=pv[:, :, 16:64], func=ACTF.Copy), rb, wb)
                if RV == 1:
                    continue
                if RV == 3:
                    t3 = tmp2[:, hb * 8:(hb + 1) * 8, :]
                    A("act", lambda e, pv=pv, t3=t3: e.activation(out=t3, in_=pv[:, :, 0:16], func=ACTF.Copy), rb, [b_tmp])
                    A("dve", lambda e, t1=t1, t3=t3, cosb=cosb: e.tensor_tensor(out=t1, in0=t3, in1=cosb, op=ALU.mult), rb + [b_tmp], [b_tmp])
                    A("dve", lambda e, kv=kv, t3=t3, s0=s0: e.tensor_tensor(out=kv[:, :, 0:8], in0=t3[:, :, 8:16], in1=s0, op=ALU.mult), rb + [b_tmp], wb)
                    A("dve", lambda e, kv=kv, t3=t3, s1=s1: e.tensor_tensor(out=kv[:, :, 8:16], in0=t3[:, :, 0:8], in1=s1, op=ALU.mult), rb + [b_tmp], wb)
                    A("dve", lambda e, kv=kv, t1=t1: e.tensor_tensor(out=kv[:, :, 0:16], in0=kv[:, :, 0:16], in1=t1, op=ALU.add), [b_tmp] + wb, wb)
                    continue
                if RV == 2:
                    A("dve", lambda e, pv=pv, t1=t1, t2=t2: e.tensor_tensor(out=t1, in0=pv[:, :, 0:16], in1=t2, op=ALU.mult), rb, [b_tmp])
                    A("dve", lambda e, kv=kv, t1=t1, t2=t2: e.tensor_tensor(out=kv[:, :, 0:16], in0=t1, in1=t2, op=ALU.add), [b_tmp], wb)
                    continue
                A("dve", lambda e, pv=pv, t1=t1, cosb=cosb: e.tensor_tensor(out=t1, in0=pv[:, :, 0:16], in1=cosb, op=ALU.mult), rb, [b_tmp])
                A("dve", lambda e, pv=pv, t2=t2, s0=s0: e.tensor_tensor(out=t2[:, :, 0:8], in0=pv[:, :, 8:16], in1=s0, op=ALU.mult), rb, [b_tmp])
                A("dve", lambda e, pv=pv, t2=t2, s1=s1: e.tensor_tensor(out=t2[:, :, 8:16], in0=pv[:, :, 0:8], in1=s1, op=ALU.mult), rb, [b_tmp])
                A("dve", lambda e, kv=kv, t1=t1, t2=t2: e.tensor_tensor(out=kv[:, :, 0:16], in0=t1, in1=t2, op=ALU.add), [b_tmp], wb)

        b_KTd = Buf(); b_Vd = Buf(); b_QTd = Buf(); b_MIXd = Buf(); b_Hd = Buf(); b_HNd = Buf(); b_XSd = Buf(); b_YSd = Buf()
        def ph1():
            wkv = carve([128, KC, 2048], BF16); b_wkv = Buf()
            for k in range(KC):
                S.add("pool", lambda e, k=k: e.dma_start(out=wkv[:, k, :], in_=w_in[k * 128:(k + 1) * 128, 2048:4096]),
                      (), [b_wkv], dma=True)
            cs_kv = carve([128, NTKV, 32], F32); b_cskv = Buf()
            DMA("sp", cs_kv, cs_kv_d, w=[b_cskv])
            xt = [carve([128, D], F32) for _ in range(2)]; b_xt = [Buf() for _ in range(2)]
            junk = carve([128, D], BF16); b_junk = Buf()
            ssb = [carve([128, 1], F32) for _ in range(2)]; b_ss = [Buf() for _ in range(2)]
            xb = [carve([128, D], BF16) for _ in range(2)]; b_xb = [Buf() for _ in range(2)]
            xT = [carve([128, KC, 128], BF16) for _ in range(2)]; b_xT = [Buf() for _ in range(2)]
            ksb = [carve([128, 1024], BF16) for _ in range(2)]; b_ksb = [Buf() for _ in range(2)]
            vsb = [carve([128, 4, 1024], BF16) for _ in range(2)]; b_vsb = [Buf() for _ in range(2)]
            kTs = [carve([128, NH, 512], BF16) for _ in range(2)]; b_kTs = [Buf() for _ in range(2)]
            tmp1 = carve([128, 16, 16], F32); tmp2 = carve([128, 16, 16], F32); b_tmp = Buf()
            pT = pbank(0, 2, BF16); b_pT = Buf()
            pK = pbank(2, 2); b_pK = Buf()
            pV = pbank(4, 2); b_pV = Buf()
            pKT = pbank(6, 1, BF16); b_pKT = Buf()
            for t in range(NTKV):
                s2 = t % 2
                g4 = (t // 4) % 2
                norm_transpose(xkv[t * 128:(t + 1) * 128, :], xt[s2], b_xt[s2], junk, b_junk, ssb[s2], b_ss[s2], xb[s2],
                               b_xb[s2], xT[s2], b_xT[s2], gmix_bc, b_gmix, pT, b_pT)
                LV = cfg.get("lv", 9)
                if LV < 1:
                    continue
                for cg in range(4):
                    dst, bd = (pK, b_pK) if cg < 2 else (pV, b_pV)
                    for k in range(KC):
                        import os as _os
                        _N = int(_os.environ.get("EXPN", 512))
                        if _os.environ.get("WSRC"):
                            A("pe", lambda e, k=k, cg=cg, dst=dst, s2=s2: e.matmul(
                                dst[:, (cg % 2) * 512:(cg % 2) * 512 + _N], lhsT=xT[s2][:, k, :],
                                rhs=xb[s2][:, 0:_N], start=(k == 0), stop=(k == KC - 1)),
                              [b_xT[s2], b_xb[s2]], [bd])
                        else:
                            A("pe", lambda e, k=k, cg=cg, dst=dst, s2=s2: e.matmul(
                                dst[:, (cg % 2) * 512:(cg % 2) * 512 + _N], lhsT=xT[s2][:, k, :],
                                rhs=wkv[:, k, cg * 512:cg * 512 + _N], start=(k == 0), stop=(k == KC - 1)),
                              [b_xT[s2], b_wkv], [bd])
                if LV < 2:
                    continue
                rope(pK, cs_kv[:, t, :], ksb[s2], tmp1, tmp2, [b_pK, b_cskv], [b_ksb[s2]], b_tmp)
                if LV < 3:
                    continue
                A("act", lambda e, t=t, g4=g4: e.activation(out=vsb[g4][:, t % 4, :], in_=pV, func=ACTF.Copy),
                  [b_pV], [b_vsb[g4]])
                if LV < 4:
                    continue
                for h in range(NH):
                    A("pe", lambda e, h=h, s2=s2: e.transpose(out=pKT[:, h * 128:(h + 1) * 128],
                                                              in_=ksb[s2][:, h * 128:(h + 1) * 128], identity=ident),
                      [b_ksb[s2], b_cst], [b_pKT])
                A("dve", lambda e, t=t, g4=g4: e.tensor_copy(out=kTs[g4][:, :, (t % 4) * 128:(t % 4 + 1) * 128],
                                                             in_=pKT.rearrange("p (h c) -> p h c", c=128)),
                  [b_pKT], [b_kTs[g4]])
                if t % 4 == 3 and LV >= 5:
                    t0 = (t // 4) * 4
                    DMA("sp", KT_d[:, :, t0 * 128:(t0 + 4) * 128].rearrange("h p c -> p h c"), kTs[g4], [b_kTs[g4]], [b_KTd])
                    for tt in range(4):
                        DMA("sp", V_d[:, :, t0 + tt, :].rearrange("h p e -> p h e"),
                            vsb[g4][:, tt, :].rearrange("p (h e) -> p h e", e=128), [b_vsb[g4]], [b_Vd])

        if cfg.get('maxph', 8) >= 1:
            ph1()
        def ph2():
            new_phase()
            wq = carve([128, KC, 2048], BF16); b_wq = Buf()
            for k in range(KC):
                S.add("pool", lambda e, k=k: e.dma_start(out=wq[:, k, :], in_=w_in[k * 128:(k + 1) * 128, 0:2048]),
                      (), [b_wq], dma=True)
            wp = carve([128, 8, 256], BF16); b_wp = Buf()
            S.add("pool", lambda e: e.dma_start(out=wp, in_=w_pool.rearrange("g (cc p) d -> p (g cc) d", p=128)),
                  (), [b_wp], dma=True)
            psc = carve([128, 8], F32); b_psc = Buf()
            DMA("sp", psc, pool_scale, w=[b_psc])
            bands = carve([128, 3, 4, 128], BF16); b_bands = Buf()
            DMA("sp", bands, bands_d, w=[b_bands])
            cs_q = carve([128, NTQ, 32], F32); b_csq = Buf()
            DMA("sp", cs_q, cs_q_d, w=[b_csq])
            xt = [carve([128, D], F32) for _ in range(2)]; b_xt = [Buf() for _ in range(2)]
            junk = carve([128, D], BF16); b_junk = Buf()
            ssb = [carve([128, 1], F32) for _ in range(2)]; b_ss = [Buf() for _ in range(2)]
            xb = [carve([128, D], BF16) for _ in range(2)]; b_xb = [Buf() for _ in range(2)]
            xT = [carve([128, KC, 128], BF16) for _ in range(2)]; b_xT = [Buf() for _ in range(2)]
            qsb = [carve([128, 1024], BF16) for _ in range(2)]; b_qsb = [Buf() for _ in range(2)]
            pin = [carve([128, 1024], BF16) for _ in range(3)]; b_pin = [Buf() for _ in range(3)]
            qTs = [carve([128, NH, 128], BF16) for _ in range(2)]; b_qTs = [Buf() for _ in range(2)]
            pldT = [carve([128, 8, 128], BF16) for _ in range(2)]; b_pldT = [Buf() for _ in range(2)]
            mxT = [carve([128, 8, 128], BF16) for _ in range(2)]; b_mxT = [Buf() for _ in range(2)]
            tmp1 = carve([128, 16, 16], F32); tmp2 = carve([128, 16, 16], F32); b_tmp = Buf()
            pT = pbank(0, 2, BF16); b_pT = Buf()
            pP = pbank(2, 2); b_pP = Buf()
            pQ = pbank(4, 2); b_pQ = Buf()
            pQT = pbank(6, 1, BF16); b_pQT = Buf()
            pM = pbank(7, 1); b_pM = Buf()
            cnt = 0
            for i in range(NCH):
                for r in range(5):
                    s2 = cnt % 2
                    s3 = cnt % 3
                    sp3 = (cnt - 1) % 3
                    cnt += 1
                    norm_transpose(xq[i, r * 128:(r + 1) * 128, :], xt[s2], b_xt[s2], junk, b_junk, ssb[s2], b_ss[s2],
                                   xb[s2], b_xb[s2], xT[s2], b_xT[s2], gmix_bc, b_gmix, pT, b_pT)
                    for cg in range(2 if r == 0 else 4):
                        dst, bd = (pP, b_pP) if cg < 2 else (pQ, b_pQ)
                        for k in range(KC):
                            A("pe", lambda e, k=k, cg=cg, dst=dst, s2=s2: e.matmul(
                                dst[:, (cg % 2) * 512:(cg % 2 + 1) * 512], lhsT=xT[s2][:, k, :],
                                rhs=wq[:, k, cg * 512:(cg + 1) * 512], start=(k == 0), stop=(k == KC - 1)),
                              [b_xT[s2], b_wq], [bd])
                    A("act", lambda e, s3=s3: e.activation(out=pin[s3], in_=pP, func=ACTF.Copy), [b_pP], [b_pin[s3]])
                    if r == 0:
                        continue
                    tq = i * 4 + (r - 1)
                    rope(pQ, cs_q[:, tq, :], qsb[s2], tmp1, tmp2, [b_pQ, b_csq], [b_qsb[s2]], b_tmp)
                    for h in range(NH):
                        A("pe", lambda e, h=h, s2=s2: e.transpose(out=pQT[:, h * 128:(h + 1) * 128],
                                                                  in_=qsb[s2][:, h * 128:(h + 1) * 128], identity=ident),
                          [b_qsb[s2], b_cst], [b_pQT])
                    A("dve", lambda e, s2=s2: e.tensor_copy(out=qTs[s2].rearrange("p h c -> p (h c)"), in_=pQT),
                      [b_pQT], [b_qTs[s2]])
                    DMA("sp", QT_d[:, :, tq * 128:(tq + 1) * 128].rearrange("h p c -> p h c"), qTs[s2], [b_qTs[s2]], [b_QTd])
                    bsel = 2 if (i == 0 and r == 1) else 0
                    for half in range(2):
                        for u in range(4):
                            gc = half * 4 + u
                            g = gc // 2
                            o_ = pM[:, u * 128:(u + 1) * 128]
                            A("pe", lambda e, gc=gc, g=g, o_=o_, s3=s3, bsel=bsel: e.matmul(
                                o_, lhsT=pin[s3][:, gc * 128:(gc + 1) * 128], rhs=bands[:, bsel, g, :], start=True, stop=False),
                              [b_pin[s3], b_bands], [b_pM])
                            A("pe", lambda e, gc=gc, g=g, o_=o_, sp3=sp3: e.matmul(
                                o_, lhsT=pin[sp3][:, gc * 128:(gc + 1) * 128], rhs=bands[:, 1, g, :], start=False, stop=True),
                              [b_pin[sp3], b_bands], [b_pM])
                        A("dve", lambda e, half=half, s2=s2: e.tensor_copy(
                            out=pldT[s2][:, half * 4:(half + 1) * 4, :].rearrange("p a b -> p (a b)"), in_=pM),
                          [b_pM], [b_pldT[s2]])
                    for half in range(2):
                        for u in range(4):
                            gd = half * 4 + u
                            g, dd = gd // 2, gd % 2
                            o_ = pM[:, u * 128:(u + 1) * 128]
                            for cc in range(2):
                                A("pe", lambda e, g=g, dd=dd, cc=cc, o_=o_, s2=s2: e.matmul(
                                    o_, lhsT=wp[:, g * 2 + cc, dd * 128:(dd + 1) * 128], rhs=pldT[s2][:, g * 2 + cc, :],
                                    start=(cc == 0), stop=(cc == 1)), [b_wp, b_pldT[s2]], [b_pM])
                        for u in range(4):
                            gd = half * 4 + u
                            A("act", lambda e, gd=gd, u=u, s2=s2: e.activation(
                                out=mxT[s2][:, gd, :], in_=pM[:, u * 128:(u + 1) * 128], func=ACTF.Identity,
                                scale=psc[:, gd:gd + 1]), [b_pM, b_psc], [b_mxT[s2]])
                    DMA("sp", MIXT_d[0:8, :, tq * 128:(tq + 1) * 128].rearrange("f p c -> p f c"), mxT[s2], [b_mxT[s2]], [b_MIXd])

        if cfg.get('maxph', 8) >= 2:
            ph2()
        def ph3():
            new_phase()
            NSEG = 4
            SEG = S_ // NSEG
            kt_sb = carve([128, S_], BF16); b_kt = [Buf() for _ in range(NSEG)]
            v_sb = carve([128, NTKV, 128], BF16); b_v = [Buf() for _ in range(NSEG)]
            qt_sb = [carve([128, NQ], BF16) for _ in range(2)]; b_qt = [Buf() for _ in range(2)]
            msk = carve([128, 16, 512], BF16); b_msk = Buf()
            DMA("sp", msk, masks_d, w=[b_msk])
            pt = [[carve([128, 512], BF16) for _ in range(3)] for _ in range(2)]
            b_pt = [[Buf() for _ in range(3)] for _ in range(2)]
            rr = [carve([128, 512], F32) for _ in range(2)]; b_rr = [Buf() for _ in range(2)]
            oo = [carve([128, 512], F32) for _ in range(2)]; b_oo = [Buf() for _ in range(2)]
            sq = carve([128, 512], BF16); b_sq = Buf()
            rs = carve([128, 512], F32); b_rs = Buf()
            at = [carve([128, 512], BF16) for _ in range(2)]; b_at = [Buf() for _ in range(2)]
            pS = [[pbank(c * 2 + s) for s in range(2)] for c in range(2)]
            b_pS = [[Buf() for _ in range(2)] for _ in range(2)]
            pO = [pbank(4 + c) for c in range(2)]; b_pO = [Buf() for _ in range(2)]
            pL = [pbank(6 + c) for c in range(2)]; b_pL = [Buf() for _ in range(2)]
            ucnt = 0
            ecnt = 0
            for h in range(NH):
                hs = h % 2
                for sg in range(NSEG):
                    DMA("sp", kt_sb[:, sg * SEG:(sg + 1) * SEG], KT_d[h, :, sg * SEG:(sg + 1) * SEG], [b_KTd], [b_kt[sg]])
                    DMA("sp", v_sb[:, sg * SEG // 128:(sg + 1) * SEG // 128, :],
                        V_d[h, :, sg * SEG // 128:(sg + 1) * SEG // 128, :], [b_Vd], [b_v[sg]])
                DMA("sp", qt_sb[hs], QT_d[h], [b_QTd], [b_qt[hs]])
                for i in range(NCH):
                    nkb = 16 * (i + 1)
                    for kb in range(nkb):
                        sg = (kb * 128) // SEG
                        sl = ucnt % 2
                        ps3 = ucnt % 3
                        ucnt += 1
                        for c in range(2):
                            A("pe", lambda e, c=c, kb=kb, i=i, sl=sl, hs=hs: e.matmul(
                                pS[c][sl], lhsT=kt_sb[c * 64:(c + 1) * 64, kb * 128:(kb + 1) * 128],
                                rhs=qt_sb[hs][c * 64:(c + 1) * 64, i * 512:(i + 1) * 512], start=True, stop=True),
                              [b_kt[sg], b_qt[hs]], [b_pS[c][sl]])
                        for c in range(2):
                            A("act", lambda e, c=c, sl=sl, ps3=ps3: e.activation(out=pt[c][ps3], in_=pS[c][sl], func=ACTF.Exp,
                                                                               scale=0.125),
                              [b_pS[c][sl]], [b_pt[c][ps3]])
                        if kb >= nkb - 16:
                            mi = kb - (nkb - 16)
                            for c in range(2):
                                A("pool" if c == 0 else "dve", lambda e, c=c, ps3=ps3, mi=mi: e.tensor_tensor(
                                    out=pt[c][ps3], in0=pt[c][ps3], in1=msk[:, mi, :], op=ALU.mult),
                                  [b_pt[c][ps3], b_msk], [b_pt[c][ps3]])
                        for c in range(2):
                            A("pe", lambda e, c=c, kb=kb, ps3=ps3, nkb=nkb: e.matmul(
                                pO[c], lhsT=v_sb[:, kb, :], rhs=pt[c][ps3], start=(kb == 0), stop=(kb == nkb - 1)),
                              [b_v[sg], b_pt[c][ps3]], [b_pO[c]])
                            A("pe", lambda e, c=c, kb=kb, ps3=ps3, nkb=nkb: e.matmul(
                                pL[c], lhsT=ones, rhs=pt[c][ps3], start=(kb == 0), stop=(kb == nkb - 1)),
                              [b_cst, b_pt[c][ps3]], [b_pL[c]])
                    es = ecnt % 2
                    ecnt += 1
                    for c in range(2):
                        A("act", lambda e, c=c: e.activation(out=rr[c], in_=pL[c], func=ACTF.Copy), [b_pL[c]], [b_rr[c]])
                        A("act", lambda e, c=c: e.activation(out=oo[c], in_=pO[c], func=ACTF.Copy), [b_pO[c]], [b_oo[c]])
                        A("dve", lambda e, c=c: e.reciprocal(out=rr[c], in_=rr[c]), [b_rr[c]], [b_rr[c]])
                        A("dve", lambda e, c=c: e.tensor_tensor(out=oo[c], in0=oo[c], in1=rr[c], op=ALU.mult),
                          [b_oo[c], b_rr[c]], [b_oo[c]])
                    A("dve", lambda e: e.scalar_tensor_tensor(out=oo[0], in0=oo[1], scalar=neglam[:, 0:1], in1=oo[0],
                                                              op0=ALU.mult, op1=ALU.add), [b_oo[0], b_oo[1], b_neglam], [b_oo[0]])
                    A("act", lambda e: e.activation(out=sq, in_=oo[0], func=ACTF.Square), [b_oo[0]], [b_sq])
                    A("pe", lambda e: e.matmul(pL[0], lhsT=ones, rhs=sq, start=True, stop=True), [b_cst, b_sq], [b_pL[0]])
                    A("act", lambda e: e.activation(out=rs, in_=pL[0], func=ACTF.Sqrt, scale=1.0 / 128, bias=EPS),
                      [b_pL[0]], [b_rs])
                    A("dve", lambda e: e.reciprocal(out=rs, in_=rs), [b_rs], [b_rs])
                    A("dve", lambda e, es=es: e.scalar_tensor_tensor(out=at[es], in0=oo[0], scalar=gsc[:, 0:1], in1=rs,
                                                                   op0=ALU.mult, op1=ALU.mult),
                      [b_oo[0], b_rs, b_gsc], [b_at[es]])
                    DMA("sp", MIXT_d[8 + h, :, i * 512:(i + 1) * 512], at[es], [b_at[es]], [b_MIXd])

        if cfg.get('maxph', 8) >= 3:
            ph3()
        def ph4():
            new_phase()
            wo = carve([128, KC, D], BF16); b_wo = Buf()
            for k in range(KC):
                S.add("pool", lambda e, k=k: e.dma_start(out=wo[:, k, :], in_=w_out[k * 128:(k + 1) * 128, :]),
                      (), [b_wo], dma=True)
            wr = carve([128, KC, NR], BF16); b_wr = Buf()
            S.add("pool", lambda e: e.dma_start(out=wr, in_=w_r.rearrange("(k p) n -> p k n", p=128)), (), [b_wr], dma=True)
            br = carve([128, NR], F32); b_br = Buf()
            DMA("sp", br, b_r.partition_broadcast(128).rearrange("p o d -> p (o d)"), w=[b_br])
            gffn_bc = carve([128, D], F32); b_gffn = Buf()
            DMA("sp", gffn_bc, g_ffn.partition_broadcast(128).rearrange("p o d -> p (o d)"), w=[b_gffn])
            mT = [carve([128, KC, 128], BF16) for _ in range(2)]; b_mT = [Buf() for _ in range(2)]
            xt = [carve([128, D], F32) for _ in range(2)]; b_xt = [Buf() for _ in range(2)]
            hsb = [carve([128, D], F32) for _ in range(2)]; b_hsb = [Buf() for _ in range(2)]
            junk = carve([128, D], BF16); b_junk = Buf()
            ssb = [carve([128, 1], F32) for _ in range(2)]; b_ss = [Buf() for _ in range(2)]
            hn = [carve([128, D], BF16) for _ in range(2)]; b_hn = [Buf() for _ in range(2)]
            hnT = [carve([128, KC, 128], BF16) for _ in range(2)]; b_hnT = [Buf() for _ in range(2)]
            pH = [pbank(b) for b in range(4)]; b_pH = [Buf() for _ in range(4)]
            pT = pbank(4, 2, BF16); b_pT = Buf()
            pR = pbank(6); b_pR = Buf()
            for t in range(NTQ):
                s2 = t % 2
                i, r = t // 4, t % 4
                DMA("sp", mT[s2], MIXT_d[:, :, t * 128:(t + 1) * 128].rearrange("f p c -> p f c"), [b_MIXd], [b_mT[s2]])
                DMA("sp", xt[s2], xq[i, (r + 1) * 128:(r + 2) * 128, :], w=[b_xt[s2]])
                for cg in range(4):
                    for k in range(KC):
                        A("pe", lambda e, k=k, cg=cg, s2=s2: e.matmul(pH[cg], lhsT=mT[s2][:, k, :],
                                                                      rhs=wo[:, k, cg * 512:(cg + 1) * 512],
                                                                      start=(k == 0), stop=(k == KC - 1)),
                          [b_mT[s2], b_wo], [b_pH[cg]])
                    A("act", lambda e, cg=cg, s2=s2: e.activation(out=hsb[s2][:, cg * 512:(cg + 1) * 512], in_=pH[cg],
                                                                  func=ACTF.Copy), [b_pH[cg]], [b_hsb[s2]])
                    A("dve", lambda e, cg=cg, s2=s2: e.tensor_tensor(out=hsb[s2][:, cg * 512:(cg + 1) * 512],
                                                                     in0=hsb[s2][:, cg * 512:(cg + 1) * 512],
                                                                     in1=xt[s2][:, cg * 512:(cg + 1) * 512], op=ALU.add),
                      [b_hsb[s2], b_xt[s2]], [b_hsb[s2]])
                DMA("sp", H_d[t * 128:(t + 1) * 128, :], hsb[s2], [b_hsb[s2]], [b_Hd])
                A("act", lambda e, s2=s2: e.activation(out=junk, in_=hsb[s2], func=ACTF.Square, accum_out=ssb[s2]),
                  [b_hsb[s2]], [b_junk, b_ss[s2]])
                A("act", lambda e, s2=s2: e.activation(out=ssb[s2], in_=ssb[s2], func=ACTF.Sqrt, scale=1.0 / D, bias=EPS),
                  [b_ss[s2]], [b_ss[s2]])
                A("dve", lambda e, s2=s2: e.reciprocal(out=ssb[s2], in_=ssb[s2]), [b_ss[s2]], [b_ss[s2]])
                A("dve", lambda e, s2=s2: e.scalar_tensor_tensor(out=hn[s2], in0=hsb[s2], scalar=ssb[s2][:, 0:1], in1=gffn_bc,
                                                               op0=ALU.mult, op1=ALU.mult),
                  [b_hsb[s2], b_ss[s2], b_gffn], [b_hn[s2]])
                DMA("sp", HN_d[t * 128:(t + 1) * 128, :], hn[s2], [b_hn[s2]], [b_HNd])
                for k in range(KC):
                    A("pe", lambda e, k=k, s2=s2: e.transpose(out=pT[:, k * 128:(k + 1) * 128],
                                                              in_=hn[s2][:, k * 128:(k + 1) * 128], identity=ident),
                      [b_hn[s2], b_cst], [b_pT])
                A("act", lambda e, s2=s2: e.activation(out=hnT[s2].rearrange("p a b -> p (a b)"), in_=pT, func=ACTF.Copy),
                  [b_pT], [b_hnT[s2]])
                for k in range(KC):
                    A("pe", lambda e, k=k, s2=s2: e.matmul(pR[:, 0:NR], lhsT=hnT[s2][:, k, :], rhs=wr[:, k, :],
                                                           start=(k == 0), stop=(k == KC - 1)), [b_hnT[s2], b_wr], [b_pR])
                A("act", lambda e, t=t: e.activation(out=lall[:, t, :], in_=pR[:, 0:NR], func=ACTF.Copy), [b_pR], [b_lall])
                A("dve", lambda e, t=t: e.tensor_tensor(out=lall[:, t, :], in0=lall[:, t, :], in1=br, op=ALU.add),
                  [b_lall, b_br], [b_lall])

        if cfg.get('maxph', 8) >= 4:
            ph4()
        def ph5():
            new_phase()
            T_ = NTQ
            V = lambda shape, dt=F32: carve(shape, dt)
            mg = V([128, T_]); bm = Buf()
            maskg = V([128, T_, 8])
            eg = V([128, T_, 8])
            sume = V([128, T_])
            prod = V([128, T_, 8, 8])
            sel = V([128, T_, 8])
            top8 = V([128, T_, 8])
            m1 = V([128, T_, 8]); m2 = V([128, T_, 8])
            dm = V([128, T_]); g1 = V([128, T_])
            E = [V([128, T_, NE], BF16) for _ in range(2)]
            E32 = [V([128, T_, NE]) for _ in range(2)]
            Mt = V([128, T_, NE], BF16)
            Mc = V([128, T_ + 1, NE], BF16)
            rank = V([128, T_, NE])
            cnts = V([128, NE]); nblk = V([128, NE]); pend = V([128, NE]); pst = V([128, NE])
            thr = V([128, NE, NBMAX]); cmp1 = V([128, NE, NBMAX])
            bst = V([128, NB, NE]); cmp2 = V([128, NB, NE]); bexp = V([128, NB])
            onesf = V([128, NE])
            dst_f = V([128, 2, T_])
            bE = Buf()
            DMA("sp", thr, thr_d, w=[bE])
            DMA("sp", bst, bst_d, w=[bE])
            lg = lall[:, :, 0:8]
            le = lall[:, :, 8:NR]
            RW = ([b_lall, bE], [bE])

            def dv(fn, r=RW[0], w=RW[1], eng="dve"):
                A(eng, fn, r, w)
            dv(lambda e: e.tensor_reduce(out=mg, in_=lg, axis=AX.X, op=ALU.max))
            dv(lambda e: e.tensor_tensor(out=maskg, in0=lg, in1=mg.unsqueeze(2).to_broadcast([128, T_, 8]), op=ALU.is_equal))
            dv(lambda e: e.tensor_tensor(out=eg, in0=lg, in1=mg.unsqueeze(2).to_broadcast([128, T_, 8]), op=ALU.subtract))
            dv(lambda e: e.activation(out=eg, in_=eg, func=ACTF.Exp), eng="act")
            dv(lambda e: e.tensor_reduce(out=sume, in_=eg, axis=AX.X, op=ALU.add))
            dv(lambda e: e.reciprocal(out=sume, in_=sume))
            if NG == 8:
                lev = le.rearrange("p t (g i) -> p t g i", i=8)
            else:
                lev = le.rearrange("p t (g i) -> p t g i", i=8)
            for g in range(NG):
                dv(lambda e, g=g: e.tensor_tensor(out=prod[:, :, g, :], in0=lev[:, :, g, :],
                                                  in1=maskg[:, :, g:g + 1].to_broadcast([128, T_, 8]), op=ALU.mult))
            dv(lambda e: e.tensor_copy(out=sel, in_=prod[:, :, 0, :]))
            for g in range(1, NG):
                dv(lambda e, g=g: e.tensor_tensor(out=sel, in0=sel, in1=prod[:, :, g, :], op=ALU.add))
            for t in range(T_):
                dv(lambda e, t=t: e.max(out=top8[:, t, :], in_=sel[:, t, :]))
            dv(lambda e: e.tensor_tensor(out=m1, in0=sel, in1=top8[:, :, 0:1].to_broadcast([128, T_, 8]), op=ALU.is_equal))
            dv(lambda e: e.tensor_tensor(out=m2, in0=sel, in1=top8[:, :, 1:2].to_broadcast([128, T_, 8]), op=ALU.is_equal))
            dv(lambda e: e.tensor_tensor(out=dm, in0=top8[:, :, 1], in1=top8[:, :, 0], op=ALU.subtract))
            dv(lambda e: e.activation(out=dm, in_=dm, func=ACTF.Exp), eng="act")
            dv(lambda e: e.tensor_scalar(out=dm, in0=dm, scalar1=1.0, scalar2=None, op0=ALU.add))
            dv(lambda e: e.reciprocal(out=g1, in_=dm))
            dv(lambda e: e.tensor_tensor(out=gates[:, 0, :], in0=g1, in1=sume, op=ALU.mult), w=[bE, b_gates])
            dv(lambda e: e.tensor_tensor(out=gates[:, 1, :], in0=sume, in1=gates[:, 0, :], op=ALU.subtract), w=[bE, b_gates])
            for s_, mm in ((0, m1), (1, m2)):
                for g in range(NG):
                    dv(lambda e, s_=s_, mm=mm, g=g: e.tensor_tensor(
                        out=E32[s_][:, :, g * 8:(g + 1) * 8], in0=mm,
                        in1=maskg[:, :, g:g + 1].to_broadcast([128, T_, 8]), op=ALU.mult))
                dv(lambda e, s_=s_: e.tensor_copy(out=E[s_], in_=E32[s_]))
            dv(lambda e: e.tensor_tensor(out=Mt, in0=E[0], in1=E[1], op=ALU.add))
            dv(lambda e: e.memset(Mc[:, 0, :], 0.0))
            for t in range(T_):
                dv(lambda e, t=t: e.tensor_tensor(out=Mc[:, t + 1, :], in0=Mc[:, t, :], in1=Mt[:, t, :], op=ALU.add))
            pRk = psum[:, 0:4, :].rearrange("p a b -> p (a b)")
            b_pRk = Buf()
            PER = 512 // NE
            for t in range(T_):
                o_ = pRk[:, (t // PER) * 512 + (t % PER) * NE:(t // PER) * 512 + (t % PER + 1) * NE]
                A("pe", lambda e, t=t, o_=o_: e.matmul(o_, lhsT=tri, rhs=Mt[:, t, :], start=True, stop=False),
                  [bE, b_cst], [b_pRk])
                A("pe", lambda e, t=t, o_=o_: e.matmul(o_, lhsT=ones, rhs=Mc[:, t, :], start=False, stop=True),
                  [bE, b_cst], [b_pRk])
            pC = pbank(4); b_pC = Buf()
            A("pe", lambda e: e.matmul(pC[:, 0:NE], lhsT=ones, rhs=Mc[:, T_, :], start=True, stop=True), [bE, b_cst], [b_pC])
            for t in range(T_):
                o_ = pRk[:, (t // PER) * 512 + (t % PER) * NE:(t // PER) * 512 + (t % PER + 1) * NE]
                dv(lambda e, t=t, o_=o_: e.tensor_copy(out=rank[:, t, :], in_=o_), r=[b_pRk, bE])
            dv(lambda e: e.tensor_copy(out=cnts, in_=pC[:, 0:NE]), r=[b_pC, bE])
            dv(lambda e: e.tensor_tensor(out=cmp1, in0=thr, in1=cnts.unsqueeze(2).to_broadcast([128, NE, NBMAX]), op=ALU.is_lt))
            dv(lambda e: e.tensor_reduce(out=nblk, in_=cmp1, axis=AX.X, op=ALU.add))
            dv(lambda e: e.memset(onesf, 1.0))
            dv(lambda e: e.tensor_tensor_scan(out=pend, data0=onesf, data1=nblk, initial=0.0, op0=ALU.mult, op1=ALU.add))
            dv(lambda e: e.tensor_tensor(out=pst, in0=pend, in1=nblk, op=ALU.subtract))
            dv(lambda e: e.tensor_scalar(out=pst, in0=pst, scalar1=float(MB), scalar2=None, op0=ALU.mult))
            dv(lambda e: e.tensor_scalar(out=pend, in0=pend, scalar1=float(MB), scalar2=None, op0=ALU.mult))
            dv(lambda e: e.tensor_tensor(out=rank, in0=rank, in1=pst.unsqueeze(1).to_broadcast([128, T_, NE]), op=ALU.add))
            for s_ in range(2):
                dv(lambda e, s_=s_: e.tensor_tensor(out=E32[s_], in0=E32[s_], in1=rank, op=ALU.mult))
                dv(lambda e, s_=s_: e.tensor_reduce(out=dst_f[:, s_, :], in_=E32[s_], axis=AX.X, op=ALU.add))
            dv(lambda e: e.tensor_copy(out=dest_i, in_=dst_f), w=[bE, b_dest])
            dv(lambda e: e.tensor_tensor(out=cmp2, in0=bst, in1=pend.unsqueeze(1).to_broadcast([128, NB, NE]), op=ALU.is_ge))
            dv(lambda e: e.tensor_reduce(out=bexp, in_=cmp2, axis=AX.X, op=ALU.add))
            dv(lambda e: e.tensor_scalar(out=bexp, in0=bexp, scalar1=float(NE - 1), scalar2=128.0, op0=ALU.min, op1=ALU.mult))
            dv(lambda e: e.tensor_scalar(out=bexp, in0=bexp, scalar1=iop[:, 0:1], scalar2=None, op0=ALU.add), r=[bE, b_iop])
            dv(lambda e: e.tensor_copy(out=widx, in_=bexp), w=[bE, b_widx])
            if debug:
                DMA("sp", dbg["lall"], lall, [b_lall], [Buf()])
                DMA("sp", dbg["dest"], dest_i, [b_dest], [Buf()])
                DMA("sp", dbg["gate"], gates, [b_gates], [Buf()])
                DMA("sp", dbg["bexp"], widx, [b_widx], [Buf()])

        if cfg.get('maxph', 8) >= 5:
            ph5()
        def ph6():
            new_phase()
            hn = [carve([128, D], BF16) for _ in range(3)]; b_hn = [Buf() for _ in range(3)]
            for t in range(NTQ):
                s3 = t % 3
                DMA("sp", hn[s3], HN_d[t * 128:(t + 1) * 128, :], [b_HNd], [b_hn[s3]])
                for s_ in range(2):
                    S.add("pool", lambda e, t=t, s_=s_, s3=s3: e.indirect_dma_start(
                        out=XS_d, out_offset=bass.IndirectOffsetOnAxis(ap=dest_i[:, s_, t:t + 1], axis=0),
                        in_=hn[s3], in_offset=None), [b_hn[s3], b_dest], [b_XSd], dma=True)

        if cfg.get('maxph', 8) >= 6:
            ph6()
        def ph7():
            new_phase()
            wst = [carve([128, 8192], F32) for _ in range(2)]; b_wst = [Buf() for _ in range(2)]
            wg = carve([128, KC, 512], BF16); b_wg = Buf()
            wu = carve([128, KC, 512], BF16); b_wu = Buf()
            wd = carve([128, 4, D], BF16); b_wd = Buf()
            xs = [carve([128, D], BF16) for _ in range(2)]; b_xs = [Buf() for _ in range(2)]
            xsT = carve([128, KC, MB], BF16); b_xsT = Buf()
            sg_ = [carve([128, MB], F32) for _ in range(2)]; b_sg = [Buf() for _ in range(2)]
            hT = carve([128, 4, MB], BF16); b_hT = Buf()
            su_ = [carve([128, MB], F32) for _ in range(2)]; b_su = [Buf() for _ in range(2)]
            ysb = [carve([128, D], BF16) for _ in range(2)]; b_ysb = [Buf() for _ in range(2)]
            pT = pbank(0, 2, BF16); b_pT = Buf()
            pG = [pbank(2), pbank(3)]; b_pG = [Buf(), Buf()]
            pY = [pbank(4 + q) for q in range(4)]; b_pY = [Buf() for _ in range(4)]
            wsrc = [w_gate.rearrange("e (p k) n -> (e p) (k n)", k=KC), w_up.rearrange("e (p k) n -> (e p) (k n)", k=KC),
                    w_down.rearrange("e (p k) n -> (e p) (k n)", k=4)]
            wdst = [(wg, b_wg), (wu, b_wu), (wd, b_wd)]
            wc = 0
            for b in range(NB):
                for m in range(3):
                    ws = wc % 2
                    wc += 1
                    S.add("pool", lambda e, b=b, m=m, ws=ws: e.indirect_dma_start(
                        out=wst[ws], out_offset=None, in_=wsrc[m],
                        in_offset=bass.IndirectOffsetOnAxis(ap=widx[:, b:b + 1], axis=0)), [b_widx], [b_wst[ws]], dma=True)
                    dflat = wdst[m][0].rearrange("p a b -> p (a b)")
                    A("pool", lambda e, ws=ws, dflat=dflat: e.tensor_copy(out=dflat[:, 0:4096], in_=wst[ws][:, 0:4096]),
                      [b_wst[ws]], [wdst[m][1]])
                    A("dve", lambda e, ws=ws, dflat=dflat: e.tensor_copy(out=dflat[:, 4096:8192], in_=wst[ws][:, 4096:8192]),
                      [b_wst[ws]], [wdst[m][1]])
                for half in range(2):
                    DMA("sp", xs[half], XS_d[b * MB + half * 128:b * MB + (half + 1) * 128, :], [b_XSd], [b_xs[half]])
                    xv = xs[half].rearrange("p (q k) -> p k q", k=KC)
                    for k in range(KC):
                        A("pe", lambda e, k=k, xv=xv: e.transpose(out=pT[:, k * 128:(k + 1) * 128], in_=xv[:, k, :],
                                                                  identity=ident), [b_xs[half], b_cst], [b_pT])
                    A("act", lambda e, half=half: e.activation(out=xsT[:, :, half * 128:(half + 1) * 128],
                                                               in_=pT.rearrange("p (k c) -> p k c", c=128), func=ACTF.Copy),
                      [b_pT], [b_xsT])
                for fc in range(4):
                    for m, wt_ in ((0, wg), (1, wu)):
                        wv = wt_.rearrange("p k (q f) -> p k f q", f=4)
                        for k in range(KC):
                            A("pe", lambda e, k=k, m=m, wv=wv, fc=fc: e.matmul(pG[m][:, 0:MB], lhsT=wv[:, k, fc, :],
                                                                             rhs=xsT[:, k, :], start=(k == 0),
                                                                             stop=(k == KC - 1)),
                              [wdst[m][1], b_xsT], [b_pG[m]])
                    s2 = fc % 2
                    A("act", lambda e, s2=s2: e.activation(out=sg_[s2], in_=pG[0][:, 0:MB], func=ACTF.Silu),
                      [b_pG[0]], [b_sg[s2]])
                    A("act", lambda e, s2=s2: e.activation(out=su_[s2], in_=pG[1][:, 0:MB], func=ACTF.Copy),
                      [b_pG[1]], [b_su[s2]])
                    A("dve", lambda e, s2=s2, fc=fc: e.tensor_tensor(out=hT[:, fc, :], in0=su_[s2], in1=sg_[s2],
                                                                    op=ALU.mult), [b_su[s2], b_sg[s2]], [b_hT])
                for half in range(2):
                    for cg in range(4):
                        for fc in range(4):
                            A("pe", lambda e, half=half, cg=cg, fc=fc: e.matmul(
                                pY[cg], lhsT=hT[:, fc, half * 128:(half + 1) * 128], rhs=wd[:, fc, cg * 512:(cg + 1) * 512],
                                start=(fc == 0), stop=(fc == 3)), [b_hT, b_wd], [b_pY[cg]])
                        if cg % 2 == 0:
                            A("act", lambda e, half=half, cg=cg: e.activation(out=ysb[half][:, cg * 512:(cg + 1) * 512],
                                                                              in_=pY[cg], func=ACTF.Copy),
                              [b_pY[cg]], [b_ysb[half]])
                        else:
                            A("dve", lambda e, half=half, cg=cg: e.tensor_copy(out=ysb[half][:, cg * 512:(cg + 1) * 512],
                                                                               in_=pY[cg]), [b_pY[cg]], [b_ysb[half]])
                    DMA("sp", YS_d[b * MB + half * 128:b * MB + (half + 1) * 128, :], ysb[half], [b_ysb[half]], [b_YSd])

        if cfg.get('maxph', 8) >= 7:
            ph7()
        def ph8():
            new_phase()
            gfin_bc = carve([128, D], F32); b_gfin = Buf()
            DMA("sp", gfin_bc, g_fin.partition_broadcast(128).rearrange("p o d -> p (o d)"), w=[b_gfin])
            hh = [carve([128, D], F32) for _ in range(2)]; b_hh = [Buf() for _ in range(2)]
            y1 = [carve([128, D], BF16) for _ in range(2)]; b_y1 = [Buf() for _ in range(2)]
            y2 = [carve([128, D], BF16) for _ in range(2)]; b_y2 = [Buf() for _ in range(2)]
            junk = carve([128, D], BF16); b_junk = Buf()
            ssb = [carve([128, 1], F32) for _ in range(2)]; b_ss = [Buf() for _ in range(2)]
            ob = [carve([128, D], F32) for _ in range(2)]; b_ob = [Buf() for _ in range(2)]
            b_out = Buf()
            for t in range(NTQ):
                s2 = t % 2
                DMA("sp", hh[s2], H_d[t * 128:(t + 1) * 128, :], [b_Hd], [b_hh[s2]])
                for s_, (yy, byy) in enumerate(((y1, b_y1), (y2, b_y2))):
                    S.add("pool", lambda e, t=t, s_=s_, yy=yy, s2=s2: e.indirect_dma_start(
                        out=yy[s2], out_offset=None, in_=YS_d,
                        in_offset=bass.IndirectOffsetOnAxis(ap=dest_i[:, s_, t:t + 1], axis=0)),
                        [b_YSd, b_dest], [byy[s2]], dma=True)
                A("dve", lambda e, t=t, s2=s2: e.scalar_tensor_tensor(out=hh[s2], in0=y1[s2], scalar=gates[:, 0, t:t + 1],
                                                                      in1=hh[s2], op0=ALU.mult, op1=ALU.add),
                  [b_y1[s2], b_hh[s2], b_gates], [b_hh[s2]])
                A("dve", lambda e, t=t, s2=s2: e.scalar_tensor_tensor(out=hh[s2], in0=y2[s2], scalar=gates[:, 1, t:t + 1],
                                                                      in1=hh[s2], op0=ALU.mult, op1=ALU.add),
                  [b_y2[s2], b_hh[s2], b_gates], [b_hh[s2]])
                A("act", lambda e, s2=s2: e.activation(out=junk, in_=hh[s2], func=ACTF.Square, accum_out=ssb[s2]),
                  [b_hh[s2]], [b_junk, b_ss[s2]])
                A("act", lambda e, s2=s2: e.activation(out=ssb[s2], in_=ssb[s2], func=ACTF.Sqrt, scale=1.0 / D, bias=EPS),
                  [b_ss[s2]], [b_ss[s2]])
                A("dve", lambda e, s2=s2: e.reciprocal(out=ssb[s2], in_=ssb[s2]), [b_ss[s2]], [b_ss[s2]])
                A("dve", lambda e, s2=s2: e.scalar_tensor_tensor(out=ob[s2], in0=hh[s2], scalar=ssb[s2][:, 0:1], in1=gfin_bc,
                                                               op0=ALU.mult, op1=ALU.mult),
                  [b_hh[s2], b_ss[s2], b_gfin], [b_ob[s2]])
                DMA("sp", out_d[t * 128:(t + 1) * 128, :], ob[s2], [b_ob[s2]], [b_out])
        if cfg.get('maxph', 8) >= 8:
            ph8()
        S.emit(st)
    return nc


def host_inputs(cfg, x, norm_mix_g, w_in, w_pool, pool_scale, lambda_q1, lambda_k1, lambda_q2, lambda_k2, subln_g,
                w_out, norm_ffn_g, w_grp, b_grp, w_exp, b_exp, w_gate, w_up, w_down, norm_final_g):
    S_ = cfg["S"]; NG = cfg["NG"]; NE = NG * 8
    NCH = S_ // 2048; NQ = NCH * 512; NTQ = NQ // 128; NTKV = S_ // 128
    NB = -(-(2 * NQ + NE * (MB - 1)) // MB); NBMAX = -(-NQ // MB)
    f32 = np.float32
    bf = ml_dtypes.bfloat16
    x = np.asarray(x, f32)
    inv = (500000.0 ** (-np.arange(0, 16, 2, dtype=f32) / f32(16))).astype(f32)
    pos = np.arange(S_, dtype=f32)
    ang = (pos[:, None] * inv[None, :]).astype(f32)
    cos8, sin8 = np.cos(ang).astype(f32), np.sin(ang).astype(f32)
    cs = np.concatenate([cos8, cos8, -sin8, sin8], axis=1).astype(f32)
    ident = np.eye(128, dtype=f32)
    tri = (np.arange(128)[:, None] < np.arange(128)[None, :]).astype(f32)
    cst = np.stack([ident, np.ones((128, 128), f32), tri], axis=1).astype(bf)
    s_i = np.arange(128)[:, None]; t_i = np.arange(128)[None, :]

    def band(w, first):
        cntv = np.minimum(t_i + 1, w) if first else w
        main = ((s_i <= t_i) & (s_i > t_i - w)).astype(f32) / cntv - (s_i == t_i).astype(f32)
        prev = ((s_i - 128 > t_i - w)).astype(f32) / w
        return main, prev
    thr = np.broadcast_to((np.arange(NBMAX, dtype=f32) * MB)[None, None, :], (128, NE, NBMAX)).copy()
    bst = np.broadcast_to((np.arange(NB, dtype=f32) * MB)[None, :, None], (128, NB, NE)).copy()
    iop = np.arange(128, dtype=f32).reshape(128, 1)
    lams = np.concatenate([np.asarray(a, f32).reshape(-1) for a in (lambda_q1, lambda_k1, lambda_q2, lambda_k2)]).reshape(1, 256)
    w_r = np.concatenate([np.asarray(w_grp, f32)[0][:, :NG], np.zeros((D, 8 - NG), f32)] +
                         [np.asarray(w_exp, f32)[0][g] for g in range(NG)], axis=1)
    b_r = np.concatenate([np.asarray(b_grp, f32)[0][:NG], np.full((8 - NG,), -1e30, f32)] +
                         [np.asarray(b_exp, f32)[0][g] for g in range(NG)]).reshape(1, -1).astype(f32)
    common = dict(
        cst=cst, thr=thr, bst=bst, iop=iop, norm_mix_g=np.asarray(norm_mix_g, f32).reshape(1, D),
        w_in=np.asarray(w_in, f32)[0], w_pool=np.asarray(w_pool, f32)[0],
        pool_scale=np.ascontiguousarray(np.asarray(pool_scale, f32).reshape(8, 128).T), lams=lams,
        subln_g=np.asarray(subln_g, f32).reshape(128, 1), w_out=np.asarray(w_out, f32)[0],
        norm_ffn_g=np.asarray(norm_ffn_g, f32).reshape(1, D), w_r=np.ascontiguousarray(w_r), b_r=b_r,
        w_gate=np.asarray(w_gate, f32)[0][:NE], w_up=np.asarray(w_up, f32)[0][:NE], w_down=np.asarray(w_down, f32)[0][:NE],
        norm_final_g=np.asarray(norm_final_g, f32).reshape(1, D))
    maps = []
    for c in range(8):
        b, j = c // 4, c % 4
        xkv = x[b]
        xq = np.zeros((NCH, 640, D), f32)
        qpos = np.zeros((NCH, 512), np.int64)
        for i in range(NCH):
            g0 = (4 * i + j) * 512
            lo = g0 - 128
            if lo >= 0:
                xq[i] = xkv[lo:g0 + 512]
            else:
                xq[i, 128:] = xkv[g0:g0 + 512]
            qpos[i] = np.arange(g0, g0 + 512)
        cs_kv = cs.reshape(NTKV, 128, 32).transpose(1, 0, 2)
        cs_q = cs[qpos.reshape(-1)].reshape(NTQ, 128, 32).transpose(1, 0, 2)
        masks = np.zeros((128, 16, 512), f32)
        for mi in range(16):
            crel, kb4 = mi // 4, mi % 4
            if crel < j:
                masks[:, mi, :] = 1.0
            elif crel == j:
                masks[:, mi, :] = ((kb4 * 128 + np.arange(128))[:, None] <= np.arange(512)[None, :]).astype(f32)
        bands = np.zeros((128, 3, 4, 128), f32)
        for gi, w in enumerate(WINS):
            mn, pv = band(w, False)
            bands[:, 0, gi], bands[:, 1, gi] = mn, pv
            bands[:, 2, gi] = band(w, True)[0] if j == 0 else mn
        m = dict(common)
        m.update(xkv=np.ascontiguousarray(xkv), xq=xq, cs_kv=np.ascontiguousarray(cs_kv), cs_q=np.ascontiguousarray(cs_q),
                 masks=masks.astype(bf), bands=bands.astype(bf))
        maps.append(m)
    return maps


def assemble(cfg, results, key="out"):
    S_ = cfg["S"]; NCH = S_ // 2048
    out = np.zeros((2, S_, D), np.float32)
    for c in range(8):
        b, j = c // 4, c % 4
        o = results[c][key]
        for i in range(NCH):
            g0 = (4 * i + j) * 512
            out[b, g0:g0 + 512] = o[i * 512:(i + 1) * 512]
    return out


def kernel(**inputs):
    cfg = dict(CFG)
    nc = build(cfg)
    maps = host_inputs(cfg, **inputs)
    res = run_bass_kernel_spmd(nc, maps, core_ids=list(range(8)))
    return assemble(cfg, res.results)
```

```python
from contextlib import ExitStack
import math
import numpy as np
import ml_dtypes
import concourse.bass as bass
import concourse.mybir as mybir
from concourse.bass_utils import run_bass_kernel_spmd

F32 = mybir.dt.float32
BF16 = mybir.dt.bfloat16
I32 = mybir.dt.int32
ACTF = mybir.ActivationFunctionType
ALU = mybir.AluOpType
AX = mybir.AxisListType

CFG = dict(S=16384, NG=8)
D = 2048
KC = 16
NH = 8
EPS = 1e-6
LAM_INIT = 0.8 - 0.6 * math.exp(0.0)
WINS = (2, 4, 8, 16)
MB = 256


class Buf:
    __slots__ = ("w", "r")

    def __init__(self):
        self.w = None
        self.r = []


class Op:
    __slots__ = ("eng", "fn", "deps", "signal", "sig", "dma", "dsem", "dval", "ring_wait")

    def __init__(self, eng, fn, dma):
        self.eng = eng
        self.fn = fn
        self.dma = dma
        self.deps = []
        self.signal = False
        self.sig = 0
        self.dsem = None
        self.dval = 0
        self.ring_wait = None


class Sched:
    ENGS = ("pe", "act", "dve", "pool", "sp")
    RING = {"sp": 8, "pool": 8, "act": 2}

    def __init__(self, nc):
        self.nc = nc
        self.q = {e: [] for e in self.ENGS}
        self.ndma = {e: 0 for e in self.ENGS}
        self.dma_since = []

    def add(self, eng, fn, reads=(), writes=(), dma=False, extra=()):
        op = Op(eng, fn, dma)
        raw = set()
        other = set(extra)
        for b in reads:
            if b.w is not None:
                raw.add(b.w)
        for b in writes:
            if b.w is not None:
                other.add(b.w)
            other.update(b.r)
        for b in reads:
            b.r.append(op)
        for b in writes:
            b.w = op
            b.r = []
        deps = []
        for d in raw | other:
            if d is op:
                continue
            if (not d.dma) and d.eng == eng and not dma and d not in extra:
                if eng == "pe" or d not in raw:
                    continue
            deps.append(d)
        op.deps = deps
        if dma:
            n = self.ndma[eng]
            self.ndma[eng] = n + 1
            K = self.RING[eng]
            op.dsem = n % K
            op.dval = 16 * (n // K + 1)
            if n >= K:
                op.ring_wait = (n % K, 16 * (n // K))
            self.dma_since.append(op)
        self.q[eng].append(op)
        return op

    def barrier(self):
        last = [self.q[e][-1] for e in ("pe", "act", "dve", "pool") if self.q[e]]
        last = [o for o in last if o.fn is not None]
        lasts = []
        for e in ("pe", "act", "dve", "pool"):
            for o in reversed(self.q[e]):
                if o.fn is not None and not o.dma:
                    lasts.append(o)
                    break
        dm = list(self.dma_since)
        self.dma_since = []
        for e in self.ENGS:
            op = Op(e, None, False)
            op.deps = [o for o in lasts if o.eng != e] + dm
            self.q[e].append(op)

    def emit(self, stack):
        nc = self.nc
        for e in self.ENGS:
            for op in self.q[e]:
                for d in op.deps:
                    if not d.dma:
                        d.signal = True
        for e in self.ENGS:
            c = 0
            for op in self.q[e]:
                if op.signal and not op.dma:
                    c += 1
                    op.sig = c
        csem = {e: stack.enter_context(nc.semaphore("c_" + e)) for e in ("pe", "act", "dve", "pool")}
        rsem = {e: [stack.enter_context(nc.semaphore("r_%s%d" % (e, i))) for i in range(self.RING[e])]
                for e in ("sp", "pool", "act")}
        block = stack.enter_context(nc.Block())
        engmap = {"pe": block.tensor, "act": block.scalar, "dve": block.vector, "pool": block.gpsimd,
                  "sp": block.sync}

        def mk(e):
            ops = self.q[e]

            def body(eng):
                waited = {}

                def wait(sem, key, val):
                    if waited.get(key, 0) >= val:
                        return
                    waited[key] = val
                    eng.wait_ge(sem, val)

                for op in ops:
                    for d in op.deps:
                        if d.dma:
                            wait(rsem[d.eng][d.dsem], (d.eng, d.dsem), d.dval)
                        else:
                            wait(csem[d.eng], d.eng, d.sig)
                    if op.ring_wait is not None:
                        wait(rsem[e][op.ring_wait[0]], (e, op.ring_wait[0]), op.ring_wait[1])
                    if op.fn is None:
                        continue
                    ins = op.fn(eng)
                    if op.dma:
                        ins.then_inc(rsem[e][op.dsem], 16)
                    elif op.signal:
                        ins.then_inc(csem[e], 1)
                if e in rsem:
                    n = self.ndma[e]
                    K = self.RING[e]
                    for s in range(min(n, K)):
                        wait(rsem[e][s], (e, s), 16 * ((n - 1 - s) // K + 1))
            return body

        for e in self.ENGS:
            engmap[e](mk(e))


def build(cfg, debug=False):
    S_ = cfg["S"]
    NG = cfg["NG"]
    NE = NG * 8
    NCH = S_ // 2048
    NQ = NCH * 512
    NTQ = NQ // 128
    NTKV = S_ // 128
    NR = 8 + NE
    NB = -(-(2 * NQ + NE * (MB - 1)) // MB)
    NBMAX = -(-NQ // MB)

    nc = bass.Bass("TRN2", target_bir_lowering=False)
    din = lambda n, s, dt=F32: nc.dram_tensor(n, list(s), dt, kind="ExternalInput").ap()
    dscr = lambda n, s, dt: nc.dram_tensor(n, list(s), dt, kind="Internal").ap()
    xkv = din("xkv", [S_, D])
    xq = din("xq", [NCH, 640, D])
    cs_kv_d = din("cs_kv", [128, NTKV, 32])
    cs_q_d = din("cs_q", [128, NTQ, 32])
    masks_d = din("masks", [128, 16, 512], BF16)
    bands_d = din("bands", [128, 3, 4, 128], BF16)
    cst_d = din("cst", [128, 3, 128], BF16)
    thr_d = din("thr", [128, NE, NBMAX])
    bst_d = din("bst", [128, NB, NE])
    iop_d = din("iop", [128, 1])
    g_mix = din("norm_mix_g", [1, D])
    w_in = din("w_in", [D, 4096])
    w_pool = din("w_pool", [4, 256, 256])
    pool_scale = din("pool_scale", [128, 8])
    lams = din("lams", [1, 256])
    subln_g = din("subln_g", [128, 1])
    w_out = din("w_out", [D, D])
    g_ffn = din("norm_ffn_g", [1, D])
    w_r = din("w_r", [D, NR])
    b_r = din("b_r", [1, NR])
    w_gate = din("w_gate", [NE, D, 512])
    w_up = din("w_up", [NE, D, 512])
    w_down = din("w_down", [NE, 512, D])
    g_fin = din("norm_final_g", [1, D])
    out_d = nc.dram_tensor("out", [NQ, D], F32, kind="ExternalOutput").ap()
    dbg = {}
    if debug:
        dbg["mixT"] = nc.dram_tensor("dbg_mixT", [16, 128, NQ], BF16, kind="ExternalOutput").ap()
        dbg["h"] = nc.dram_tensor("dbg_h", [NQ, D], F32, kind="ExternalOutput").ap()
        dbg["lall"] = nc.dram_tensor("dbg_lall", [128, NTQ, NR], F32, kind="ExternalOutput").ap()
        dbg["dest"] = nc.dram_tensor("dbg_dest", [128, 2, NTQ], I32, kind="ExternalOutput").ap()
        dbg["gate"] = nc.dram_tensor("dbg_gate", [128, 2, NTQ], F32, kind="ExternalOutput").ap()
        dbg["bexp"] = nc.dram_tensor("dbg_bexp", [128, NB], I32, kind="ExternalOutput").ap()

    KT_d = dscr("KT_d", [NH, 128, S_], BF16)
    V_d = dscr("V_d", [NH, 128, NTKV, 128], BF16)
    QT_d = dscr("QT_d", [NH, 128, NQ], BF16)
    MIXT_d = dbg["mixT"] if debug else dscr("MIXT_d", [16, 128, NQ], BF16)
    H_d = dbg["h"] if debug else dscr("H_d", [NQ, D], F32)
    HN_d = dscr("HN_d", [NQ, D], BF16)
    XS_d = dscr("XS_d", [NB * MB, D], BF16)
    YS_d = dscr("YS_d", [NB * MB, D], BF16)

    S = Sched(nc)
    A = S.add

    def DMA(q, out, in_, r=(), w=()):
        return S.add(q, lambda e: e.dma_start(out=out, in_=in_), r, w, dma=True)

    with ExitStack() as st:
        ARENA = 94000
        arena = st.enter_context(nc.sbuf_tensor("arena", [128, ARENA], BF16))
        psum = st.enter_context(nc.psum_tensor("psum", [128, 8, 512], F32))
        state = {"off": 0, "base": 0}

        def carve(shape, dt):
            n = int(np.prod(shape[1:]))
            nb = n * (2 if dt in (F32, I32) else 1)
            nb = (nb + 15) // 16 * 16
            o = state["off"]
            assert o + nb <= ARENA, ("SBUF arena overflow", o, nb)
            state["off"] = o + nb
            v = arena[:, o:o + nb]
            if dt != BF16:
                v = v.bitcast(dt)
            v = v[:, 0:n]
            if len(shape) == 3:
                v = v.rearrange("p (a b) -> p a b", b=shape[2])
            elif len(shape) == 4:
                v = v.rearrange("p (a b c) -> p a b c", b=shape[2], c=shape[3])
            return v

        def new_phase():
            S.barrier()
            state["off"] = state["base"]

        def pbank(b, n=1, dt=F32):
            v = psum[:, b:b + n, :].rearrange("p a b -> p (a b)")
            if dt != F32:
                v = v.bitcast(dt)
            return v

        cst = carve([128, 3, 128], BF16); b_cst = Buf()
        ident, ones, tri = cst[:, 0, :], cst[:, 1, :], cst[:, 2, :]
        gmix_bc = carve([128, D], F32); b_gmix = Buf()
        lall = carve([128, NTQ, NR], F32); b_lall = Buf()
        neglam = carve([128, 1], F32); b_neglam = Buf()
        gsc = carve([128, 1], F32); b_gsc = Buf()
        iop = carve([128, 1], F32); b_iop = Buf()
        dest_i = carve([128, 2, NTQ], I32); b_dest = Buf()
        gates = carve([128, 2, NTQ], F32); b_gates = Buf()
        widx = carve([128, NB], I32); b_widx = Buf()
        small = carve([128, 8], F32)
        epsb = carve([128, 1], F32); b_epsb = Buf()
        A("dve", lambda e: e.memset(epsb, EPS), (), [b_epsb])
        DMA("sp", cst, cst_d, w=[b_cst])
        DMA("sp", gmix_bc, g_mix.partition_broadcast(128).rearrange("p o d -> p (o d)"), w=[b_gmix])
        DMA("sp", iop, iop_d, w=[b_iop])
        state["base"] = state["off"]

        lam_t = carve([128, 256], F32); b_lamt = Buf()
        lam_p = carve([128, 128], F32); b_lamp = Buf()
        lam_s = carve([128, 2], F32); b_lams = Buf()
        DMA("sp", lam_t, lams.partition_broadcast(128).rearrange("p o d -> p (o d)"), w=[b_lamt])
        lv = lam_t.rearrange("p (a b c) -> p a b c", a=2, b=2)
        A("dve", lambda e: e.tensor_tensor(out=lam_p.rearrange("p (a c) -> p a c", a=2), in0=lv[:, :, 0, :],
                                           in1=lv[:, :, 1, :], op=ALU.mult), [b_lamt], [b_lamp])
        A("dve", lambda e: e.tensor_reduce(out=lam_s, in_=lam_p.rearrange("p (a c) -> p a c", a=2), axis=AX.X,
                                           op=ALU.add), [b_lamp], [b_lams])
        A("act", lambda e: e.activation(out=lam_s, in_=lam_s, func=ACTF.Exp), [b_lams], [b_lams])
        A("dve", lambda e: e.scalar_tensor_tensor(out=neglam, in0=lam_s[:, 1:2], scalar=-LAM_INIT, in1=lam_s[:, 0:1],
                                                  op0=ALU.add, op1=ALU.subtract), [b_lams], [b_neglam])
        DMA("sp", gsc, subln_g, w=[b_gsc])
        A("dve", lambda e: e.tensor_scalar(out=gsc, in0=gsc, scalar1=1.0 - LAM_INIT, scalar2=None, op0=ALU.mult),
          [b_gsc], [b_gsc])

        def norm_transpose(src, xt, b_xt, junk, b_junk, ss, b_ss, xb, b_xb, xT, b_xT, gbc, b_gbc, pT, b_pT):
            DMA("sp", xt, src, w=[b_xt])
            A("act", lambda e: e.activation(out=junk, in_=xt, func=ACTF.Square, accum_out=ss), [b_xt], [b_junk, b_ss])
            A("act", lambda e: e.activation(out=ss, in_=ss, func=ACTF.Sqrt, scale=1.0 / D, bias=EPS), [b_ss], [b_ss])
            A("dve", lambda e: e.reciprocal(out=ss, in_=ss), [b_ss], [b_ss])
            A("dve", lambda e: e.scalar_tensor_tensor(out=xb, in0=xt, scalar=ss[:, 0:1], in1=gbc, op0=ALU.mult,
                                                      op1=ALU.mult), [b_xt, b_ss, b_gbc], [b_xb])
            if xT is None:
                return
            for k in range(KC):
                A("pe", lambda e, k=k: e.transpose(out=pT[:, k * 128:(k + 1) * 128], in_=xb[:, k * 128:(k + 1) * 128],
                                                   identity=ident), [b_xb, b_cst], [b_pT])
            A("act", lambda e: e.activation(out=xT[:, 0:8, :].rearrange("p a b -> p (a b)"), in_=pT[:, 0:1024],
                                            func=ACTF.Copy), [b_pT], [b_xT])
            A("dve", lambda e: e.tensor_copy(out=xT[:, 8:16, :].rearrange("p a b -> p (a b)"), in_=pT[:, 1024:2048]),
              [b_pT], [b_xT])

        def rope(pk, cs_t, ksb, tmp1, tmp2, rb, wb, b_tmp):
            for hb in range(2):
                pv = pk[:, hb * 512:(hb + 1) * 512].rearrange("p (g d) -> p g d", d=64)
                kv = ksb[:, hb * 512:(hb + 1) * 512].rearrange("p (g d) -> p g d", d=64)
                t1 = tmp1[:, hb * 8:(hb + 1) * 8, :]
                t2 = tmp2[:, hb * 8:(hb + 1) * 8, :]
                cosb = cs_t[:, 0:16].unsqueeze(1).to_broadcast([128, 8, 16])
                s0 = cs_t[:, 16:24].unsqueeze(1).to_broadcast([128, 8, 8])
                s1 = cs_t[:, 24:32].unsqueeze(1).to_broadcast([128, 8, 8])
                import os as _os
                RV = 3
                A("act", lambda e, pv=pv, kv=kv: e.activation(out=kv[:, :, 16:64], in_=pv[:, :, 16:64], func=ACTF.Copy), rb, wb)
                if RV == 1:
                    continue
                if RV == 3:
                    t3 = tmp2[:, hb * 8:(hb + 1) * 8, :]
                    A("act", lambda e, pv=pv, t3=t3: e.activation(out=t3, in_=pv[:, :, 0:16], func=ACTF.Copy), rb, [b_tmp])
                    A("dve", lambda e, t1=t1, t3=t3, cosb=cosb: e.tensor_tensor(out=t1, in0=t3, in1=cosb, op=ALU.mult), rb + [b_tmp], [b_tmp])
                    A("dve", lambda e, kv=kv, t3=t3, s0=s0: e.tensor_tensor(out=kv[:, :, 0:8], in0=t3[:, :, 8:16], in1=s0, op=ALU.mult), rb + [b_tmp], wb)
                    A("dve", lambda e, kv=kv, t3=t3, s1=s1: e.tensor_tensor(out=kv[:, :, 8:16], in0=t3[:, :, 0:8], in1=s1, op=ALU.mult), rb + [b_tmp], wb)
                    A("dve", lambda e, kv=kv, t1=t1: e.tensor_tensor(out=kv[:, :, 0:16], in0=kv[:, :, 0:16], in1=t1, op=ALU.add), [b_tmp] + wb, wb)
                    continue
                if RV == 2:
                    A("dve", lambda e, pv=pv, t1=t1, t2=t2: e.tensor_tensor(out=t1, in0=pv[:, :, 0:16], in1=t2, op=ALU.mult), rb, [b_tmp])
                    A("dve", lambda e, kv=kv, t1=t1, t2=t2: e.tensor_tensor(out=kv[:, :, 0:16], in0=t1, in1=t2, op=ALU.add), [b_tmp], wb)
                    continue
                A("dve", lambda e, pv=pv, t1=t1, cosb=cosb: e.tensor_tensor(out=t1, in0=pv[:, :, 0:16], in1=cosb, op=ALU.mult), rb, [b_tmp])
                A("dve", lambda e, pv=pv, t2=t2, s0=s0: e.tensor_tensor(out=t2[:, :, 0:8], in0=pv[:, :, 8:16], in1=s0, op=ALU.mult), rb, [b_tmp])
                A("dve", lambda e, pv=pv, t2=t2, s1=s1: e.tensor_tensor(out=t2[:, :, 8:16], in0=pv[:, :, 0:8], in1=s1, op=ALU.mult), rb, [b_tmp])
                A("dve", lambda e, kv=kv, t1=t1, t2=t2: e.tensor_tensor(out=kv[:, :, 0:16], in0=t1, in1=t2, op=ALU.add), [b_tmp], wb)

        b_KTd = Buf(); b_Vd = Buf(); b_QTd = Buf(); b_MIXd = Buf(); b_Hd = Buf(); b_HNd = Buf(); b_XSd = Buf(); b_YSd = Buf()
        def ph1():
            wkv = carve([128, KC, 2048], BF16); b_wkv = Buf()
            for k in range(KC):
                S.add("pool", lambda e, k=k: e.dma_start(out=wkv[:, k, :], in_=w_in[k * 128:(k + 1) * 128, 2048:4096]),
                      (), [b_wkv], dma=True)
            cs_kv = carve([128, NTKV, 32], F32); b_cskv = Buf()
            DMA("sp", cs_kv, cs_kv_d, w=[b_cskv])
            xt = [carve([128, D], F32) for _ in range(2)]; b_xt = [Buf() for _ in range(2)]
            junk = carve([128, D], BF16); b_junk = Buf()
            ssb = [carve([128, 1], F32) for _ in range(2)]; b_ss = [Buf() for _ in range(2)]
            xb = [carve([128, D], BF16) for _ in range(2)]; b_xb = [Buf() for _ in range(2)]
            xT = [carve([128, KC, 128], BF16) for _ in range(2)]; b_xT = [Buf() for _ in range(2)]
            ksb = [carve([128, 1024], BF16) for _ in range(2)]; b_ksb = [Buf() for _ in range(2)]
            vsb = [carve([128, 4, 1024], BF16) for _ in range(2)]; b_vsb = [Buf() for _ in range(2)]
            kTs = [carve([128, NH, 512], BF16) for _ in range(2)]; b_kTs = [Buf() for _ in range(2)]
            tmp1 = carve([128, 16, 16], F32); tmp2 = carve([128, 16, 16], F32); b_tmp = Buf()
            pT = pbank(0, 2, BF16); b_pT = Buf()
            pK = pbank(2, 2); b_pK = Buf()
            pV = pbank(4, 2); b_pV = Buf()
            pKT = pbank(6, 1, BF16); b_pKT = Buf()
            def frontA(t):
                s2 = t % 2
                norm_transpose(xkv[t * 128:(t + 1) * 128, :], xt[s2], b_xt[s2], junk, b_junk, ssb[s2], b_ss[s2], xb[s2],
                               b_xb[s2], xT[s2], b_xT[s2], gmix_bc, b_gmix, pT, b_pT)
            frontA(0)
            for t in range(NTKV):
                s2 = t % 2
                g4 = (t // 4) % 2
                if t + 1 < NTKV:
                    frontA(t + 1)
                LV = cfg.get("lv", 9)
                if LV < 1:
                    continue
                for cg in range(4):
                    dst, bd = (pK, b_pK) if cg < 2 else (pV, b_pV)
                    for k in range(KC):
                        import os as _os
                        _N = int(_os.environ.get("EXPN", 512))
                        if _os.environ.get("WSRC"):
                            A("pe", lambda e, k=k, cg=cg, dst=dst, s2=s2: e.matmul(
                                dst[:, (cg % 2) * 512:(cg % 2) * 512 + _N], lhsT=xT[s2][:, k, :],
                                rhs=xb[s2][:, 0:_N], start=(k == 0), stop=(k == KC - 1)),
                              [b_xT[s2], b_xb[s2]], [bd])
                        else:
                            A("pe", lambda e, k=k, cg=cg, dst=dst, s2=s2: e.matmul(
                                dst[:, (cg % 2) * 512:(cg % 2) * 512 + _N], lhsT=xT[s2][:, k, :],
                                rhs=wkv[:, k, cg * 512:cg * 512 + _N], start=(k == 0), stop=(k == KC - 1)),
                              [b_xT[s2], b_wkv], [bd])
                if LV < 2:
                    continue
                rope(pK, cs_kv[:, t, :], ksb[s2], tmp1, tmp2, [b_pK, b_cskv], [b_ksb[s2]], b_tmp)
                if LV < 3:
                    continue
                A("act", lambda e, t=t, g4=g4: e.activation(out=vsb[g4][:, t % 4, :], in_=pV, func=ACTF.Copy),
                  [b_pV], [b_vsb[g4]])
                if LV < 4:
                    continue
                for h in range(NH):
                    A("pe", lambda e, h=h, s2=s2: e.transpose(out=pKT[:, h * 128:(h + 1) * 128],
                                                              in_=ksb[s2][:, h * 128:(h + 1) * 128], identity=ident),
                      [b_ksb[s2], b_cst], [b_pKT])
                A("dve", lambda e, t=t, g4=g4: e.tensor_copy(out=kTs[g4][:, :, (t % 4) * 128:(t % 4 + 1) * 128],
                                                             in_=pKT.rearrange("p (h c) -> p h c", c=128)),
                  [b_pKT], [b_kTs[g4]])
                if t % 4 == 3 and LV >= 5:
                    t0 = (t // 4) * 4
                    DMA("sp", KT_d[:, :, t0 * 128:(t0 + 4) * 128].rearrange("h p c -> p h c"), kTs[g4], [b_kTs[g4]], [b_KTd])
                    for tt in range(4):
                        DMA("sp", V_d[:, :, t0 + tt, :].rearrange("h p e -> p h e"),
                            vsb[g4][:, tt, :].rearrange("p (h e) -> p h e", e=128), [b_vsb[g4]], [b_Vd])

        if cfg.get('maxph', 8) >= 1:
            ph1()
        def ph2():
            new_phase()
            wq = carve([128, KC, 2048], BF16); b_wq = Buf()
            for k in range(KC):
                S.add("pool", lambda e, k=k: e.dma_start(out=wq[:, k, :], in_=w_in[k * 128:(k + 1) * 128, 0:2048]),
                      (), [b_wq], dma=True)
            wp = carve([128, 8, 256], BF16); b_wp = Buf()
            S.add("pool", lambda e: e.dma_start(out=wp, in_=w_pool.rearrange("g (cc p) d -> p (g cc) d", p=128)),
                  (), [b_wp], dma=True)
            psc = carve([128, 8], F32); b_psc = Buf()
            DMA("sp", psc, pool_scale, w=[b_psc])
            bands = carve([128, 3, 4, 128], BF16); b_bands = Buf()
            DMA("sp", bands, bands_d, w=[b_bands])
            cs_q = carve([128, NTQ, 32], F32); b_csq = Buf()
            DMA("sp", cs_q, cs_q_d, w=[b_csq])
            xt = [carve([128, D], F32) for _ in range(2)]; b_xt = [Buf() for _ in range(2)]
            junk = carve([128, D], BF16); b_junk = Buf()
            ssb = [carve([128, 1], F32) for _ in range(2)]; b_ss = [Buf() for _ in range(2)]
            xb = [carve([128, D], BF16) for _ in range(2)]; b_xb = [Buf() for _ in range(2)]
            xT = [carve([128, KC, 128], BF16) for _ in range(2)]; b_xT = [Buf() for _ in range(2)]
            qsb = [carve([128, 1024], BF16) for _ in range(2)]; b_qsb = [Buf() for _ in range(2)]
            pin = [carve([128, 1024], BF16) for _ in range(3)]; b_pin = [Buf() for _ in range(3)]
            qTs = [carve([128, NH, 128], BF16) for _ in range(2)]; b_qTs = [Buf() for _ in range(2)]
            pldT = [carve([128, 8, 128], BF16) for _ in range(2)]; b_pldT = [Buf() for _ in range(2)]
            mxT = [carve([128, 8, 128], BF16) for _ in range(2)]; b_mxT = [Buf() for _ in range(2)]
            tmp1 = carve([128, 16, 16], F32); tmp2 = carve([128, 16, 16], F32); b_tmp = Buf()
            pT = pbank(0, 2, BF16); b_pT = Buf()
            pP = pbank(2, 2); b_pP = Buf()
            pQ = pbank(4, 2); b_pQ = Buf()
            pQT = pbank(6, 1, BF16); b_pQT = Buf()
            pM = pbank(7, 1); b_pM = Buf()
            tilesB = [(i, r) for i in range(NCH) for r in range(5)]

            def frontB(cn):
                i, r = tilesB[cn]
                s2 = cn % 2
                norm_transpose(xq[i, r * 128:(r + 1) * 128, :], xt[s2], b_xt[s2], junk, b_junk, ssb[s2], b_ss[s2],
                               xb[s2], b_xb[s2], xT[s2], b_xT[s2], gmix_bc, b_gmix, pT, b_pT)
            frontB(0)
            cnt = 0
            for i in range(NCH):
                for r in range(5):
                    s2 = cnt % 2
                    s3 = cnt % 3
                    sp3 = (cnt - 1) % 3
                    cnt += 1
                    if cnt < len(tilesB):
                        frontB(cnt)
                    for cg in range(2 if r == 0 else 4):
                        dst, bd = (pP, b_pP) if cg < 2 else (pQ, b_pQ)
                        for k in range(KC):
                            A("pe", lambda e, k=k, cg=cg, dst=dst, s2=s2: e.matmul(
                                dst[:, (cg % 2) * 512:(cg % 2 + 1) * 512], lhsT=xT[s2][:, k, :],
                                rhs=wq[:, k, cg * 512:(cg + 1) * 512], start=(k == 0), stop=(k == KC - 1)),
                              [b_xT[s2], b_wq], [bd])
                    A("act", lambda e, s3=s3: e.activation(out=pin[s3], in_=pP, func=ACTF.Copy), [b_pP], [b_pin[s3]])
                    if r == 0:
                        continue
                    tq = i * 4 + (r - 1)
                    rope(pQ, cs_q[:, tq, :], qsb[s2], tmp1, tmp2, [b_pQ, b_csq], [b_qsb[s2]], b_tmp)
                    for h in range(NH):
                        A("pe", lambda e, h=h, s2=s2: e.transpose(out=pQT[:, h * 128:(h + 1) * 128],
                                                                  in_=qsb[s2][:, h * 128:(h + 1) * 128], identity=ident),
                          [b_qsb[s2], b_cst], [b_pQT])
                    A("dve", lambda e, s2=s2: e.tensor_copy(out=qTs[s2].rearrange("p h c -> p (h c)"), in_=pQT),
                      [b_pQT], [b_qTs[s2]])
                    DMA("sp", QT_d[:, :, tq * 128:(tq + 1) * 128].rearrange("h p c -> p h c"), qTs[s2], [b_qTs[s2]], [b_QTd])
                    bsel = 2 if (i == 0 and r == 1) else 0
                    for half in range(2):
                        for u in range(4):
                            gc = half * 4 + u
                            g = gc // 2
                            o_ = pM[:, u * 128:(u + 1) * 128]
                            A("pe", lambda e, gc=gc, g=g, o_=o_, s3=s3, bsel=bsel: e.matmul(
                                o_, lhsT=pin[s3][:, gc * 128:(gc + 1) * 128], rhs=bands[:, bsel, g, :], start=True, stop=False),
                              [b_pin[s3], b_bands], [b_pM])
                            A("pe", lambda e, gc=gc, g=g, o_=o_, sp3=sp3: e.matmul(
                                o_, lhsT=pin[sp3][:, gc * 128:(gc + 1) * 128], rhs=bands[:, 1, g, :], start=False, stop=True),
                              [b_pin[sp3], b_bands], [b_pM])
                        A("dve", lambda e, half=half, s2=s2: e.tensor_copy(
                            out=pldT[s2][:, half * 4:(half + 1) * 4, :].rearrange("p a b -> p (a b)"), in_=pM),
                          [b_pM], [b_pldT[s2]])
                    for half in range(2):
                        for u in range(4):
                            gd = half * 4 + u
                            g, dd = gd // 2, gd % 2
                            o_ = pM[:, u * 128:(u + 1) * 128]
                            for cc in range(2):
                                A("pe", lambda e, g=g, dd=dd, cc=cc, o_=o_, s2=s2: e.matmul(
                                    o_, lhsT=wp[:, g * 2 + cc, dd * 128:(dd + 1) * 128], rhs=pldT[s2][:, g * 2 + cc, :],
                                    start=(cc == 0), stop=(cc == 1)), [b_wp, b_pldT[s2]], [b_pM])
                        for u in range(4):
                            gd = half * 4 + u
                            A("act", lambda e, gd=gd, u=u, s2=s2: e.activation(
                                out=mxT[s2][:, gd, :], in_=pM[:, u * 128:(u + 1) * 128], func=ACTF.Identity,
                                scale=psc[:, gd:gd + 1]), [b_pM, b_psc], [b_mxT[s2]])
                    DMA("sp", MIXT_d[0:8, :, tq * 128:(tq + 1) * 128].rearrange("f p c -> p f c"), mxT[s2], [b_mxT[s2]], [b_MIXd])

        if cfg.get('maxph', 8) >= 2:
            ph2()
        def ph3():
            new_phase()
            NSEG = 4
            SEG = S_ // NSEG
            kt_sb = carve([128, S_], BF16); b_kt = [Buf() for _ in range(NSEG)]
            v_sb = carve([128, NTKV, 128], BF16); b_v = [Buf() for _ in range(NSEG)]
            qt_sb = [carve([128, NQ], BF16) for _ in range(2)]; b_qt = [Buf() for _ in range(2)]
            msk = carve([128, 16, 512], BF16); b_msk = Buf()
            DMA("sp", msk, masks_d, w=[b_msk])
            pt = [[carve([128, 512], BF16) for _ in range(3)] for _ in range(2)]
            b_pt = [[Buf() for _ in range(3)] for _ in range(2)]
            rr = [carve([128, 512], F32) for _ in range(2)]; b_rr = [Buf() for _ in range(2)]
            oo = [carve([128, 512], F32) for _ in range(2)]; b_oo = [Buf() for _ in range(2)]
            sq = carve([128, 512], BF16); b_sq = Buf()
            rs = carve([128, 512], F32); b_rs = Buf()
            at = [carve([128, 512], BF16) for _ in range(2)]; b_at = [Buf() for _ in range(2)]
            pS = [[pbank(c * 2 + s) for s in range(2)] for c in range(2)]
            b_pS = [[Buf() for _ in range(2)] for _ in range(2)]
            pO = [pbank(4 + c) for c in range(2)]; b_pO = [Buf() for _ in range(2)]
            pL = [pbank(6 + c) for c in range(2)]; b_pL = [Buf() for _ in range(2)]
            st3 = {"ecnt": 0}

            def head_load(h):
                hs = h % 2
                for sg in range(NSEG):
                    DMA("sp", kt_sb[:, sg * SEG:(sg + 1) * SEG], KT_d[h, :, sg * SEG:(sg + 1) * SEG], [b_KTd], [b_kt[sg]])
                    DMA("sp", v_sb[:, sg * SEG // 128:(sg + 1) * SEG // 128, :],
                        V_d[h, :, sg * SEG // 128:(sg + 1) * SEG // 128, :], [b_Vd], [b_v[sg]])
                DMA("sp", qt_sb[hs], QT_d[h], [b_QTd], [b_qt[hs]])

            def qk(n, u):
                h, i, kb, nkb = u
                hs = h % 2
                sg = (kb * 128) // SEG
                sl = n % 2
                for c in range(2):
                    A("pe", lambda e, c=c, kb=kb, i=i, sl=sl, hs=hs: e.matmul(
                        pS[c][sl], lhsT=kt_sb[c * 64:(c + 1) * 64, kb * 128:(kb + 1) * 128],
                        rhs=qt_sb[hs][c * 64:(c + 1) * 64, i * 512:(i + 1) * 512], start=True, stop=True),
                      [b_kt[sg], b_qt[hs]], [b_pS[c][sl]])

            def ex(n, u):
                h, i, kb, nkb = u
                sl = n % 2
                ps3 = n % 3
                for c in range(2):
                    A("act", lambda e, c=c, sl=sl, ps3=ps3: e.activation(out=pt[c][ps3], in_=pS[c][sl], func=ACTF.Exp,
                                                                       scale=0.125),
                      [b_pS[c][sl]], [b_pt[c][ps3]])
                if kb >= nkb - 16:
                    mi = kb - (nkb - 16)
                    for c in range(2):
                        A("pool" if c == 0 else "dve", lambda e, c=c, ps3=ps3, mi=mi: e.tensor_tensor(
                            out=pt[c][ps3], in0=pt[c][ps3], in1=msk[:, mi, :], op=ALU.mult),
                          [b_pt[c][ps3], b_msk], [b_pt[c][ps3]])

            def pv(n, u):
                h, i, kb, nkb = u
                sg = (kb * 128) // SEG
                ps3 = n % 3
                for c in range(2):
                    A("pe", lambda e, c=c, kb=kb, ps3=ps3, nkb=nkb: e.matmul(
                        pO[c], lhsT=v_sb[:, kb, :], rhs=pt[c][ps3], start=(kb == 0), stop=(kb == nkb - 1)),
                      [b_v[sg], b_pt[c][ps3]], [b_pO[c]])
                    A("pe", lambda e, c=c, kb=kb, ps3=ps3, nkb=nkb: e.matmul(
                        pL[c], lhsT=ones, rhs=pt[c][ps3], start=(kb == 0), stop=(kb == nkb - 1)),
                      [b_cst, b_pt[c][ps3]], [b_pL[c]])

            def epi_a():
                for c in range(2):
                    A("act", lambda e, c=c: e.activation(out=rr[c], in_=pL[c], func=ACTF.Copy), [b_pL[c]], [b_rr[c]])
                    A("act", lambda e, c=c: e.activation(out=oo[c], in_=pO[c], func=ACTF.Copy), [b_pO[c]], [b_oo[c]])
                for c in range(2):
                    A("dve", lambda e, c=c: e.reciprocal(out=rr[c], in_=rr[c]), [b_rr[c]], [b_rr[c]])
                    A("dve", lambda e, c=c: e.tensor_tensor(out=oo[c], in0=oo[c], in1=rr[c], op=ALU.mult),
                      [b_oo[c], b_rr[c]], [b_oo[c]])
                A("dve", lambda e: e.scalar_tensor_tensor(out=oo[0], in0=oo[1], scalar=neglam[:, 0:1], in1=oo[0],
                                                          op0=ALU.mult, op1=ALU.add), [b_oo[0], b_oo[1], b_neglam], [b_oo[0]])
                A("dve", lambda e: e.tensor_tensor(out=sq, in0=oo[0], in1=oo[0], op=ALU.mult), [b_oo[0]], [b_sq])

            def epi_b(h, i, n):
                es = st3["ecnt"] % 2
                st3["ecnt"] += 1
                sl = (n + 1) % 2
                A("pe", lambda e, sl=sl: e.matmul(pS[0][sl], lhsT=ones, rhs=sq, start=True, stop=True),
                  [b_cst, b_sq], [b_pS[0][sl]])
                A("act", lambda e, sl=sl: e.activation(out=rs, in_=pS[0][sl], func=ACTF.Ln, scale=1.0 / 128, bias=epsb[:, 0:1]),
                  [b_pS[0][sl], b_epsb], [b_rs])
                A("act", lambda e: e.activation(out=rs, in_=rs, func=ACTF.Exp, scale=-0.5), [b_rs], [b_rs])
                A("dve", lambda e, es=es: e.scalar_tensor_tensor(out=at[es], in0=oo[0], scalar=gsc[:, 0:1], in1=rs,
                                                               op0=ALU.mult, op1=ALU.mult),
                  [b_oo[0], b_rs, b_gsc], [b_at[es]])
                DMA("sp", MIXT_d[8 + h, :, i * 512:(i + 1) * 512], at[es], [b_at[es]], [b_MIXd])

            gn = 0
            for h in range(NH):
                units = [(h, i, kb, 16 * (i + 1)) for i in range(NCH) for kb in range(16 * (i + 1))]
                N = len(units)
                head_load(h)
                pend = []
                for n in range(N + 2):
                    if n < N:
                        qk(gn + n, units[n])
                        ex(gn + n, units[n])
                    m = n - 2
                    if m >= 0:
                        pv(gn + m, units[m])
                        _, i_, kb_, nkb_ = units[m]
                        if kb_ == nkb_ - 1:
                            epi_a()
                            pend.append((n + 4, h, i_))
                    while pend and (pend[0][0] <= n or n == N + 1):
                        _, hh_, ii_ = pend.pop(0)
                        epi_b(hh_, ii_, gn + n)
                gn += N

        if cfg.get('maxph', 8) >= 3:
            ph3()
        def ph4():
            new_phase()
            wo = carve([128, KC, D], BF16); b_wo = Buf()
            for k in range(KC):
                S.add("pool", lambda e, k=k: e.dma_start(out=wo[:, k, :], in_=w_out[k * 128:(k + 1) * 128, :]),
                      (), [b_wo], dma=True)
            wr = carve([128, KC, NR], BF16); b_wr = Buf()
            S.add("pool", lambda e: e.dma_start(out=wr, in_=w_r.rearrange("(k p) n -> p k n", p=128)), (), [b_wr], dma=True)
            br = carve([128, NR], F32); b_br = Buf()
            DMA("sp", br, b_r.partition_broadcast(128).rearrange("p o d -> p (o d)"), w=[b_br])
            gffn_bc = carve([128, D], F32); b_gffn = Buf()
            DMA("sp", gffn_bc, g_ffn.partition_broadcast(128).rearrange("p o d -> p (o d)"), w=[b_gffn])
            mT = [carve([128, KC, 128], BF16) for _ in range(2)]; b_mT = [Buf() for _ in range(2)]
            xt = [carve([128, D], F32) for _ in range(2)]; b_xt = [Buf() for _ in range(2)]
            hsb = [carve([128, D], F32) for _ in range(2)]; b_hsb = [Buf() for _ in range(2)]
            junk = carve([128, D], BF16); b_junk = Buf()
            ssb = [carve([128, 1], F32) for _ in range(2)]; b_ss = [Buf() for _ in range(2)]
            hn = [carve([128, D], BF16) for _ in range(2)]; b_hn = [Buf() for _ in range(2)]
            hnT = [carve([128, KC, 128], BF16) for _ in range(2)]; b_hnT = [Buf() for _ in range(2)]
            pH = [pbank(b) for b in range(4)]; b_pH = [Buf() for _ in range(4)]
            pT = pbank(4, 2, BF16); b_pT = Buf()
            pR = pbank(6); b_pR = Buf()
            def frontD(t):
                s2 = t % 2
                i, r = t // 4, t % 4
                DMA("sp", mT[s2], MIXT_d[:, :, t * 128:(t + 1) * 128].rearrange("f p c -> p f c"), [b_MIXd], [b_mT[s2]])
                DMA("sp", xt[s2], xq[i, (r + 1) * 128:(r + 2) * 128, :], w=[b_xt[s2]])
                for cg in range(4):
                    for k in range(KC):
                        A("pe", lambda e, k=k, cg=cg, s2=s2: e.matmul(pH[cg], lhsT=mT[s2][:, k, :],
                                                                      rhs=wo[:, k, cg * 512:(cg + 1) * 512],
                                                                      start=(k == 0), stop=(k == KC - 1)),
                          [b_mT[s2], b_wo], [b_pH[cg]])
                    A("act", lambda e, cg=cg, s2=s2: e.activation(out=hsb[s2][:, cg * 512:(cg + 1) * 512], in_=pH[cg],
                                                                  func=ACTF.Copy), [b_pH[cg]], [b_hsb[s2]])
                    A("dve", lambda e, cg=cg, s2=s2: e.tensor_tensor(out=hsb[s2][:, cg * 512:(cg + 1) * 512],
                                                                     in0=hsb[s2][:, cg * 512:(cg + 1) * 512],
                                                                     in1=xt[s2][:, cg * 512:(cg + 1) * 512], op=ALU.add),
                      [b_hsb[s2], b_xt[s2]], [b_hsb[s2]])
                DMA("sp", H_d[t * 128:(t + 1) * 128, :], hsb[s2], [b_hsb[s2]], [b_Hd])
                A("act", lambda e, s2=s2: e.activation(out=junk, in_=hsb[s2], func=ACTF.Square, accum_out=ssb[s2]),
                  [b_hsb[s2]], [b_junk, b_ss[s2]])
                A("act", lambda e, s2=s2: e.activation(out=ssb[s2], in_=ssb[s2], func=ACTF.Sqrt, scale=1.0 / D, bias=EPS),
                  [b_ss[s2]], [b_ss[s2]])
                A("dve", lambda e, s2=s2: e.reciprocal(out=ssb[s2], in_=ssb[s2]), [b_ss[s2]], [b_ss[s2]])
                A("dve", lambda e, s2=s2: e.scalar_tensor_tensor(out=hn[s2], in0=hsb[s2], scalar=ssb[s2][:, 0:1], in1=gffn_bc,
                                                               op0=ALU.mult, op1=ALU.mult),
                  [b_hsb[s2], b_ss[s2], b_gffn], [b_hn[s2]])
                DMA("sp", HN_d[t * 128:(t + 1) * 128, :], hn[s2], [b_hn[s2]], [b_HNd])

            def backD(t):
                s2 = t % 2
                for k in range(KC):
                    A("pe", lambda e, k=k, s2=s2: e.transpose(out=pT[:, k * 128:(k + 1) * 128],
                                                              in_=hn[s2][:, k * 128:(k + 1) * 128], identity=ident),
                      [b_hn[s2], b_cst], [b_pT])
                A("act", lambda e, s2=s2: e.activation(out=hnT[s2].rearrange("p a b -> p (a b)"), in_=pT, func=ACTF.Copy),
                  [b_pT], [b_hnT[s2]])
                for k in range(KC):
                    A("pe", lambda e, k=k, s2=s2: e.matmul(pR[:, 0:NR], lhsT=hnT[s2][:, k, :], rhs=wr[:, k, :],
                                                           start=(k == 0), stop=(k == KC - 1)), [b_hnT[s2], b_wr], [b_pR])
                A("act", lambda e, t=t: e.activation(out=lall[:, t, :], in_=pR[:, 0:NR], func=ACTF.Copy), [b_pR], [b_lall])
                A("dve", lambda e, t=t: e.tensor_tensor(out=lall[:, t, :], in0=lall[:, t, :], in1=br, op=ALU.add),
                  [b_lall, b_br], [b_lall])


            frontD(0)
            for t in range(NTQ):
                if t + 1 < NTQ:
                    frontD(t + 1)
                backD(t)

        if cfg.get('maxph', 8) >= 4:
            ph4()
        def ph5():
            new_phase()
            T_ = NTQ
            V = lambda shape, dt=F32: carve(shape, dt)
            mg = V([128, T_]); bm = Buf()
            maskg = V([128, T_, 8])
            eg = V([128, T_, 8])
            sume = V([128, T_])
            prod = V([128, T_, 8, 8])
            sel = V([128, T_, 8])
            top8 = V([128, T_, 8])
            m1 = V([128, T_, 8]); m2 = V([128, T_, 8])
            dm = V([128, T_]); g1 = V([128, T_])
            E = [V([128, T_, NE], BF16) for _ in range(2)]
            E32 = [V([128, T_, NE]) for _ in range(2)]
            Mt = V([128, T_, NE], BF16)
            Mc = V([128, T_ + 1, NE], BF16)
            rank = V([128, T_, NE])
            cnts = V([128, NE]); nblk = V([128, NE]); pend = V([128, NE]); pst = V([128, NE])
            thr = V([128, NE, NBMAX]); cmp1 = V([128, NE, NBMAX])
            bst = V([128, NB, NE]); cmp2 = V([128, NB, NE]); bexp = V([128, NB])
            onesf = V([128, NE])
            dst_f = V([128, 2, T_])
            bE = Buf()
            DMA("sp", thr, thr_d, w=[bE])
            DMA("sp", bst, bst_d, w=[bE])
            lg = lall[:, :, 0:8]
            le = lall[:, :, 8:NR]
            RW = ([b_lall, bE], [bE])

            def dv(fn, r=RW[0], w=RW[1], eng="dve"):
                A(eng, fn, r, w)
            dv(lambda e: e.tensor_reduce(out=mg, in_=lg, axis=AX.X, op=ALU.max))
            dv(lambda e: e.tensor_tensor(out=maskg, in0=lg, in1=mg.unsqueeze(2).to_broadcast([128, T_, 8]), op=ALU.is_equal))
            dv(lambda e: e.tensor_tensor(out=eg, in0=lg, in1=mg.unsqueeze(2).to_broadcast([128, T_, 8]), op=ALU.subtract))
            dv(lambda e: e.activation(out=eg, in_=eg, func=ACTF.Exp), eng="act")
            dv(lambda e: e.tensor_reduce(out=sume, in_=eg, axis=AX.X, op=ALU.add))
            dv(lambda e: e.reciprocal(out=sume, in_=sume))
            if NG == 8:
                lev = le.rearrange("p t (g i) -> p t g i", i=8)
            else:
                lev = le.rearrange("p t (g i) -> p t g i", i=8)
            for g in range(NG):
                dv(lambda e, g=g: e.tensor_tensor(out=prod[:, :, g, :], in0=lev[:, :, g, :],
                                                  in1=maskg[:, :, g:g + 1].to_broadcast([128, T_, 8]), op=ALU.mult))
            dv(lambda e: e.tensor_copy(out=sel, in_=prod[:, :, 0, :]))
            for g in range(1, NG):
                dv(lambda e, g=g: e.tensor_tensor(out=sel, in0=sel, in1=prod[:, :, g, :], op=ALU.add))
            for t in range(T_):
                dv(lambda e, t=t: e.max(out=top8[:, t, :], in_=sel[:, t, :]))
            dv(lambda e: e.tensor_tensor(out=m1, in0=sel, in1=top8[:, :, 0:1].to_broadcast([128, T_, 8]), op=ALU.is_equal))
            dv(lambda e: e.tensor_tensor(out=m2, in0=sel, in1=top8[:, :, 1:2].to_broadcast([128, T_, 8]), op=ALU.is_equal))
            dv(lambda e: e.tensor_tensor(out=dm, in0=top8[:, :, 1], in1=top8[:, :, 0], op=ALU.subtract))
            dv(lambda e: e.activation(out=dm, in_=dm, func=ACTF.Exp), eng="act")
            dv(lambda e: e.tensor_scalar(out=dm, in0=dm, scalar1=1.0, scalar2=None, op0=ALU.add))
            dv(lambda e: e.reciprocal(out=g1, in_=dm))
            dv(lambda e: e.tensor_tensor(out=gates[:, 0, :], in0=g1, in1=sume, op=ALU.mult), w=[bE, b_gates])
            dv(lambda e: e.tensor_tensor(out=gates[:, 1, :], in0=sume, in1=gates[:, 0, :], op=ALU.subtract), w=[bE, b_gates])
            for s_, mm in ((0, m1), (1, m2)):
                for g in range(NG):
                    dv(lambda e, s_=s_, mm=mm, g=g: e.tensor_tensor(
                        out=E32[s_][:, :, g * 8:(g + 1) * 8], in0=mm,
                        in1=maskg[:, :, g:g + 1].to_broadcast([128, T_, 8]), op=ALU.mult))
                dv(lambda e, s_=s_: e.tensor_copy(out=E[s_], in_=E32[s_]))
            dv(lambda e: e.tensor_tensor(out=Mt, in0=E[0], in1=E[1], op=ALU.add))
            dv(lambda e: e.memset(Mc[:, 0, :], 0.0))
            for t in range(T_):
                dv(lambda e, t=t: e.tensor_tensor(out=Mc[:, t + 1, :], in0=Mc[:, t, :], in1=Mt[:, t, :], op=ALU.add))
            pRk = psum[:, 0:4, :].rearrange("p a b -> p (a b)")
            b_pRk = Buf()
            PER = 512 // NE
            for t in range(T_):
                o_ = pRk[:, (t // PER) * 512 + (t % PER) * NE:(t // PER) * 512 + (t % PER + 1) * NE]
                A("pe", lambda e, t=t, o_=o_: e.matmul(o_, lhsT=tri, rhs=Mt[:, t, :], start=True, stop=False),
                  [bE, b_cst], [b_pRk])
                A("pe", lambda e, t=t, o_=o_: e.matmul(o_, lhsT=ones, rhs=Mc[:, t, :], start=False, stop=True),
                  [bE, b_cst], [b_pRk])
            pC = pbank(4); b_pC = Buf()
            A("pe", lambda e: e.matmul(pC[:, 0:NE], lhsT=ones, rhs=Mc[:, T_, :], start=True, stop=True), [bE, b_cst], [b_pC])
            for t in range(T_):
                o_ = pRk[:, (t // PER) * 512 + (t % PER) * NE:(t // PER) * 512 + (t % PER + 1) * NE]
                dv(lambda e, t=t, o_=o_: e.tensor_copy(out=rank[:, t, :], in_=o_), r=[b_pRk, bE])
            dv(lambda e: e.tensor_copy(out=cnts, in_=pC[:, 0:NE]), r=[b_pC, bE])
            dv(lambda e: e.tensor_tensor(out=cmp1, in0=thr, in1=cnts.unsqueeze(2).to_broadcast([128, NE, NBMAX]), op=ALU.is_lt))
            dv(lambda e: e.tensor_reduce(out=nblk, in_=cmp1, axis=AX.X, op=ALU.add))
            dv(lambda e: e.memset(onesf, 1.0))
            dv(lambda e: e.tensor_tensor_scan(out=pend, data0=onesf, data1=nblk, initial=0.0, op0=ALU.mult, op1=ALU.add))
            dv(lambda e: e.tensor_tensor(out=pst, in0=pend, in1=nblk, op=ALU.subtract))
            dv(lambda e: e.tensor_scalar(out=pst, in0=pst, scalar1=float(MB), scalar2=None, op0=ALU.mult))
            dv(lambda e: e.tensor_scalar(out=pend, in0=pend, scalar1=float(MB), scalar2=None, op0=ALU.mult))
            dv(lambda e: e.tensor_tensor(out=rank, in0=rank, in1=pst.unsqueeze(1).to_broadcast([128, T_, NE]), op=ALU.add))
            for s_ in range(2):
                dv(lambda e, s_=s_: e.tensor_tensor(out=E32[s_], in0=E32[s_], in1=rank, op=ALU.mult))
                dv(lambda e, s_=s_: e.tensor_reduce(out=dst_f[:, s_, :], in_=E32[s_], axis=AX.X, op=ALU.add))
            dv(lambda e: e.tensor_copy(out=dest_i, in_=dst_f), w=[bE, b_dest])
            dv(lambda e: e.tensor_tensor(out=cmp2, in0=bst, in1=pend.unsqueeze(1).to_broadcast([128, NB, NE]), op=ALU.is_ge))
            dv(lambda e: e.tensor_reduce(out=bexp, in_=cmp2, axis=AX.X, op=ALU.add))
            dv(lambda e: e.tensor_scalar(out=bexp, in0=bexp, scalar1=float(NE - 1), scalar2=128.0, op0=ALU.min, op1=ALU.mult))
            dv(lambda e: e.tensor_scalar(out=bexp, in0=bexp, scalar1=iop[:, 0:1], scalar2=None, op0=ALU.add), r=[bE, b_iop])
            dv(lambda e: e.tensor_copy(out=widx, in_=bexp), w=[bE, b_widx])
            if debug:
                DMA("sp", dbg["lall"], lall, [b_lall], [Buf()])
                DMA("sp", dbg["dest"], dest_i, [b_dest], [Buf()])
                DMA("sp", dbg["gate"], gates, [b_gates], [Buf()])
                DMA("sp", dbg["bexp"], widx, [b_widx], [Buf()])

        if cfg.get('maxph', 8) >= 5:
            ph5()
        def ph6():
            new_phase()
            hn = [carve([128, D], BF16) for _ in range(3)]; b_hn = [Buf() for _ in range(3)]
            for t in range(NTQ):
                s3 = t % 3
                DMA("sp", hn[s3], HN_d[t * 128:(t + 1) * 128, :], [b_HNd], [b_hn[s3]])
                for s_ in range(2):
                    S.add("pool", lambda e, t=t, s_=s_, s3=s3: e.indirect_dma_start(
                        out=XS_d, out_offset=bass.IndirectOffsetOnAxis(ap=dest_i[:, s_, t:t + 1], axis=0),
                        in_=hn[s3], in_offset=None), [b_hn[s3], b_dest], [b_XSd], dma=True)

        if cfg.get('maxph', 8) >= 6:
            ph6()
        def ph7():
            new_phase()
            wst = [carve([128, 8192], F32) for _ in range(2)]; b_wst = [Buf() for _ in range(2)]
            wg = carve([128, KC, 512], BF16); b_wg = Buf()
            wu = carve([128, KC, 512], BF16); b_wu = Buf()
            wd = carve([128, 4, D], BF16); b_wd = Buf()
            xs = [carve([128, D], BF16) for _ in range(2)]; b_xs = [Buf() for _ in range(2)]
            xsT = carve([128, KC, MB], BF16); b_xsT = Buf()
            sg_ = [carve([128, MB], F32) for _ in range(2)]; b_sg = [Buf() for _ in range(2)]
            hT = carve([128, 4, MB], BF16); b_hT = Buf()
            su_ = [carve([128, MB], F32) for _ in range(2)]; b_su = [Buf() for _ in range(2)]
            ysb = [carve([128, D], BF16) for _ in range(2)]; b_ysb = [Buf() for _ in range(2)]
            pT = pbank(0, 2, BF16); b_pT = Buf()
            pG = [pbank(2), pbank(3)]; b_pG = [Buf(), Buf()]
            pY = [pbank(4 + q) for q in range(4)]; b_pY = [Buf() for _ in range(4)]
            wsrc = [w_gate.rearrange("e (p k) n -> (e p) (k n)", k=KC), w_up.rearrange("e (p k) n -> (e p) (k n)", k=KC),
                    w_down.rearrange("e (p k) n -> (e p) (k n)", k=4)]
            wdst = [(wg, b_wg), (wu, b_wu), (wd, b_wd)]
            loads = [(b, m) for b in range(NB) for m in range(3)]

            def wdma(j):
                b, m = loads[j]
                ws = j % 2
                S.add("pool", lambda e, b=b, m=m, ws=ws: e.indirect_dma_start(
                    out=wst[ws], out_offset=None, in_=wsrc[m],
                    in_offset=bass.IndirectOffsetOnAxis(ap=widx[:, b:b + 1], axis=0)), [b_widx], [b_wst[ws]], dma=True)

            def wconv(j):
                b, m = loads[j]
                ws = j % 2
                dflat = wdst[m][0].rearrange("p a b -> p (a b)")
                A("act", lambda e, ws=ws, dflat=dflat: e.activation(out=dflat[:, 0:2560], in_=wst[ws][:, 0:2560], func=ACTF.Copy),
                  [b_wst[ws]], [wdst[m][1]])
                A("dve", lambda e, ws=ws, dflat=dflat: e.tensor_copy(out=dflat[:, 2560:5632], in_=wst[ws][:, 2560:5632]),
                  [b_wst[ws]], [wdst[m][1]])
                A("pool", lambda e, ws=ws, dflat=dflat: e.tensor_copy(out=dflat[:, 5632:8192], in_=wst[ws][:, 5632:8192]),
                  [b_wst[ws]], [wdst[m][1]])
            wdma(0)
            wdma(1)
            for b in range(NB):
                for m in range(3):
                    j = b * 3 + m
                    wconv(j)
                    if j + 2 < len(loads):
                        wdma(j + 2)
                for half in range(2):
                    DMA("sp", xs[half], XS_d[b * MB + half * 128:b * MB + (half + 1) * 128, :], [b_XSd], [b_xs[half]])
                    xv = xs[half].rearrange("p (q k) -> p k q", k=KC)
                    for k in range(KC):
                        A("pe", lambda e, k=k, xv=xv: e.transpose(out=pT[:, k * 128:(k + 1) * 128], in_=xv[:, k, :],
                                                                  identity=ident), [b_xs[half], b_cst], [b_pT])
                    A("act", lambda e, half=half: e.activation(out=xsT[:, :, half * 128:(half + 1) * 128],
                                                               in_=pT.rearrange("p (k c) -> p k c", c=128), func=ACTF.Copy),
                      [b_pT], [b_xsT])
                for fc in range(4):
                    for m, wt_ in ((0, wg), (1, wu)):
                        wv = wt_.rearrange("p k (q f) -> p k f q", f=4)
                        for k in range(KC):
                            A("pe", lambda e, k=k, m=m, wv=wv, fc=fc: e.matmul(pG[m][:, 0:MB], lhsT=wv[:, k, fc, :],
                                                                             rhs=xsT[:, k, :], start=(k == 0),
                                                                             stop=(k == KC - 1)),
                              [wdst[m][1], b_xsT], [b_pG[m]])
                    s2 = fc % 2
                    A("act", lambda e, s2=s2: e.activation(out=sg_[s2], in_=pG[0][:, 0:MB], func=ACTF.Silu),
                      [b_pG[0]], [b_sg[s2]])
                    A("act", lambda e, s2=s2: e.activation(out=su_[s2], in_=pG[1][:, 0:MB], func=ACTF.Copy),
                      [b_pG[1]], [b_su[s2]])
                    A("dve", lambda e, s2=s2, fc=fc: e.tensor_tensor(out=hT[:, fc, :], in0=su_[s2], in1=sg_[s2],
                                                                    op=ALU.mult), [b_su[s2], b_sg[s2]], [b_hT])
                for half in range(2):
                    for cg in range(4):
                        for fc in range(4):
                            A("pe", lambda e, half=half, cg=cg, fc=fc: e.matmul(
                                pY[cg], lhsT=hT[:, fc, half * 128:(half + 1) * 128], rhs=wd[:, fc, cg * 512:(cg + 1) * 512],
                                start=(fc == 0), stop=(fc == 3)), [b_hT, b_wd], [b_pY[cg]])
                        if cg % 2 == 0:
                            A("act", lambda e, half=half, cg=cg: e.activation(out=ysb[half][:, cg * 512:(cg + 1) * 512],
                                                                              in_=pY[cg], func=ACTF.Copy),
                              [b_pY[cg]], [b_ysb[half]])
                        else:
                            A("dve", lambda e, half=half, cg=cg: e.tensor_copy(out=ysb[half][:, cg * 512:(cg + 1) * 512],
                                                                               in_=pY[cg]), [b_pY[cg]], [b_ysb[half]])
                    DMA("sp", YS_d[b * MB + half * 128:b * MB + (half + 1) * 128, :], ysb[half], [b_ysb[half]], [b_YSd])

        if cfg.get('maxph', 8) >= 7:
            ph7()
        def ph8():
            new_phase()
            gfin_bc = carve([128, D], F32); b_gfin = Buf()
            DMA("sp", gfin_bc, g_fin.partition_broadcast(128).rearrange("p o d -> p (o d)"), w=[b_gfin])
            hh = [carve([128, D], F32) for _ in range(2)]; b_hh = [Buf() for _ in range(2)]
            y1 = [carve([128, D], BF16) for _ in range(2)]; b_y1 = [Buf() for _ in range(2)]
            y2 = [carve([128, D], BF16) for _ in range(2)]; b_y2 = [Buf() for _ in range(2)]
            junk = carve([128, D], BF16); b_junk = Buf()
            ssb = [carve([128, 1], F32) for _ in range(2)]; b_ss = [Buf() for _ in range(2)]
            ob = [carve([128, D], F32) for _ in range(2)]; b_ob = [Buf() for _ in range(2)]
            b_out = Buf()
            for t in range(NTQ):
                s2 = t % 2
                DMA("sp", hh[s2], H_d[t * 128:(t + 1) * 128, :], [b_Hd], [b_hh[s2]])
                for s_, (yy, byy) in enumerate(((y1, b_y1), (y2, b_y2))):
                    S.add("pool", lambda e, t=t, s_=s_, yy=yy, s2=s2: e.indirect_dma_start(
                        out=yy[s2], out_offset=None, in_=YS_d,
                        in_offset=bass.IndirectOffsetOnAxis(ap=dest_i[:, s_, t:t + 1], axis=0)),
                        [b_YSd, b_dest], [byy[s2]], dma=True)
                A("dve", lambda e, t=t, s2=s2: e.scalar_tensor_tensor(out=hh[s2], in0=y1[s2], scalar=gates[:, 0, t:t + 1],
                                                                      in1=hh[s2], op0=ALU.mult, op1=ALU.add),
                  [b_y1[s2], b_hh[s2], b_gates], [b_hh[s2]])
                A("dve", lambda e, t=t, s2=s2: e.scalar_tensor_tensor(out=hh[s2], in0=y2[s2], scalar=gates[:, 1, t:t + 1],
                                                                      in1=hh[s2], op0=ALU.mult, op1=ALU.add),
                  [b_y2[s2], b_hh[s2], b_gates], [b_hh[s2]])
                A("act", lambda e, s2=s2: e.activation(out=junk, in_=hh[s2], func=ACTF.Square, accum_out=ssb[s2]),
                  [b_hh[s2]], [b_junk, b_ss[s2]])
                A("act", lambda e, s2=s2: e.activation(out=ssb[s2], in_=ssb[s2], func=ACTF.Sqrt, scale=1.0 / D, bias=EPS),
                  [b_ss[s2]], [b_ss[s2]])
                A("dve", lambda e, s2=s2: e.reciprocal(out=ssb[s2], in_=ssb[s2]), [b_ss[s2]], [b_ss[s2]])
                A("dve", lambda e, s2=s2: e.scalar_tensor_tensor(out=ob[s2], in0=hh[s2], scalar=ssb[s2][:, 0:1], in1=gfin_bc,
                                                               op0=ALU.mult, op1=ALU.mult),
                  [b_hh[s2], b_ss[s2], b_gfin], [b_ob[s2]])
                DMA("sp", out_d[t * 128:(t + 1) * 128, :], ob[s2], [b_ob[s2]], [b_out])
        if cfg.get('maxph', 8) >= 8:
            ph8()
        S.emit(st)
    return nc


def host_inputs(cfg, x, norm_mix_g, w_in, w_pool, pool_scale, lambda_q1, lambda_k1, lambda_q2, lambda_k2, subln_g,
                w_out, norm_ffn_g, w_grp, b_grp, w_exp, b_exp, w_gate, w_up, w_down, norm_final_g):
    S_ = cfg["S"]; NG = cfg["NG"]; NE = NG * 8
    NCH = S_ // 2048; NQ = NCH * 512; NTQ = NQ // 128; NTKV = S_ // 128
    NB = -(-(2 * NQ + NE * (MB - 1)) // MB); NBMAX = -(-NQ // MB)
    f32 = np.float32
    bf = ml_dtypes.bfloat16
    x = np.asarray(x, f32)
    inv = (500000.0 ** (-np.arange(0, 16, 2, dtype=f32) / f32(16))).astype(f32)
    pos = np.arange(S_, dtype=f32)
    ang = (pos[:, None] * inv[None, :]).astype(f32)
    cos8, sin8 = np.cos(ang).astype(f32), np.sin(ang).astype(f32)
    cs = np.concatenate([cos8, cos8, -sin8, sin8], axis=1).astype(f32)
    ident = np.eye(128, dtype=f32)
    tri = (np.arange(128)[:, None] < np.arange(128)[None, :]).astype(f32)
    cst = np.stack([ident, np.ones((128, 128), f32), tri], axis=1).astype(bf)
    s_i = np.arange(128)[:, None]; t_i = np.arange(128)[None, :]

    def band(w, first):
        cntv = np.minimum(t_i + 1, w) if first else w
        main = ((s_i <= t_i) & (s_i > t_i - w)).astype(f32) / cntv - (s_i == t_i).astype(f32)
        prev = ((s_i - 128 > t_i - w)).astype(f32) / w
        return main, prev
    thr = np.broadcast_to((np.arange(NBMAX, dtype=f32) * MB)[None, None, :], (128, NE, NBMAX)).copy()
    bst = np.broadcast_to((np.arange(NB, dtype=f32) * MB)[None, :, None], (128, NB, NE)).copy()
    iop = np.arange(128, dtype=f32).reshape(128, 1)
    lams = np.concatenate([np.asarray(a, f32).reshape(-1) for a in (lambda_q1, lambda_k1, lambda_q2, lambda_k2)]).reshape(1, 256)
    w_r = np.concatenate([np.asarray(w_grp, f32)[0][:, :NG], np.zeros((D, 8 - NG), f32)] +
                         [np.asarray(w_exp, f32)[0][g] for g in range(NG)], axis=1)
    b_r = np.concatenate([np.asarray(b_grp, f32)[0][:NG], np.full((8 - NG,), -1e30, f32)] +
                         [np.asarray(b_exp, f32)[0][g] for g in range(NG)]).reshape(1, -1).astype(f32)
    common = dict(
        cst=cst, thr=thr, bst=bst, iop=iop, norm_mix_g=np.asarray(norm_mix_g, f32).reshape(1, D),
        w_in=np.asarray(w_in, f32)[0], w_pool=np.asarray(w_pool, f32)[0],
        pool_scale=np.ascontiguousarray(np.asarray(pool_scale, f32).reshape(8, 128).T), lams=lams,
        subln_g=np.asarray(subln_g, f32).reshape(128, 1), w_out=np.asarray(w_out, f32)[0],
        norm_ffn_g=np.asarray(norm_ffn_g, f32).reshape(1, D), w_r=np.ascontiguousarray(w_r), b_r=b_r,
        w_gate=np.asarray(w_gate, f32)[0][:NE], w_up=np.asarray(w_up, f32)[0][:NE], w_down=np.asarray(w_down, f32)[0][:NE],
        norm_final_g=np.asarray(norm_final_g, f32).reshape(1, D))
    maps = []
    for c in range(8):
        b, j = c // 4, c % 4
        xkv = x[b]
        xq = np.zeros((NCH, 640, D), f32)
        qpos = np.zeros((NCH, 512), np.int64)
        for i in range(NCH):
            g0 = (4 * i + j) * 512
            lo = g0 - 128
            if lo >= 0:
                xq[i] = xkv[lo:g0 + 512]
            else:
                xq[i, 128:] = xkv[g0:g0 + 512]
            qpos[i] = np.arange(g0, g0 + 512)
        cs_kv = cs.reshape(NTKV, 128, 32).transpose(1, 0, 2)
        cs_q = cs[qpos.reshape(-1)].reshape(NTQ, 128, 32).transpose(1, 0, 2)
        masks = np.zeros((128, 16, 512), f32)
        for mi in range(16):
            crel, kb4 = mi // 4, mi % 4
            if crel < j:
                masks[:, mi, :] = 1.0
            elif crel == j:
                masks[:, mi, :] = ((kb4 * 128 + np.arange(128))[:, None] <= np.arange(512)[None, :]).astype(f32)
        bands = np.zeros((128, 3, 4, 128), f32)
        for gi, w in enumerate(WINS):
            mn, pv = band(w, False)
            bands[:, 0, gi], bands[:, 1, gi] = mn, pv
            bands[:, 2, gi] = band(w, True)[0] if j == 0 else mn
        m = dict(common)
        m.update(xkv=np.ascontiguousarray(xkv), xq=xq, cs_kv=np.ascontiguousarray(cs_kv), cs_q=np.ascontiguousarray(cs_q),
                 masks=masks.astype(bf), bands=bands.astype(bf))
        maps.append(m)
    return maps


def assemble(cfg, results, key="out"):
    S_ = cfg["S"]; NCH = S_ // 2048
    out = np.zeros((2, S_, D), np.float32)
    for c in range(8):
        b, j = c // 4, c % 4
        o = results[c][key]
        for i in range(NCH):
            g0 = (4 * i + j) * 512
            out[b, g0:g0 + 512] = o[i * 512:(i + 1) * 512]
    return out


def kernel(**inputs):
    cfg = dict(CFG)
    nc = build(cfg)
    maps = host_inputs(cfg, **inputs)
    res = run_bass_kernel_spmd(nc, maps, core_ids=list(range(8)))
    return assemble(cfg, res.results)
```

```python
from contextlib import ExitStack
import math
import numpy as np
import ml_dtypes
import concourse.bass as bass
import concourse.mybir as mybir
from concourse.bass_utils import run_bass_kernel_spmd

F32 = mybir.dt.float32
BF16 = mybir.dt.bfloat16
I32 = mybir.dt.int32
ACTF = mybir.ActivationFunctionType
ALU = mybir.AluOpType
AX = mybir.AxisListType

CFG = dict(S=16384, NG=8)
D = 2048
KC = 16
NH = 8
EPS = 1e-6
LAM_INIT = 0.8 - 0.6 * math.exp(0.0)
WINS = (2, 4, 8, 16)
MB = 256


class Buf:
    __slots__ = ("w", "r")

    def __init__(self):
        self.w = None
        self.r = []


class Op:
    __slots__ = ("eng", "fn", "deps", "signal", "sig", "dma", "dsem", "dval", "ring_wait")

    def __init__(self, eng, fn, dma):
        self.eng = eng
        self.fn = fn
        self.dma = dma
        self.deps = []
        self.signal = False
        self.sig = 0
        self.dsem = None
        self.dval = 0
        self.ring_wait = None


class Sched:
    ENGS = ("pe", "act", "dve", "pool", "sp")
    RING = {"sp": 8, "pool": 8, "act": 2}

    def __init__(self, nc):
        self.nc = nc
        self.q = {e: [] for e in self.ENGS}
        self.ndma = {e: 0 for e in self.ENGS}
        self.dma_since = []

    def add(self, eng, fn, reads=(), writes=(), dma=False, extra=()):
        op = Op(eng, fn, dma)
        raw = set()
        other = set(extra)
        for b in reads:
            if b.w is not None:
                raw.add(b.w)
        for b in writes:
            if b.w is not None:
                other.add(b.w)
            other.update(b.r)
        for b in reads:
            b.r.append(op)
        for b in writes:
            b.w = op
            b.r = []
        deps = []
        for d in raw | other:
            if d is op:
                continue
            if (not d.dma) and d.eng == eng and not dma and d not in extra:
                if eng == "pe" or d not in raw:
                    continue
            deps.append(d)
        op.deps = deps
        if dma:
            n = self.ndma[eng]
            self.ndma[eng] = n + 1
            K = self.RING[eng]
            op.dsem = n % K
            op.dval = 16 * (n // K + 1)
            if n >= K:
                op.ring_wait = (n % K, 16 * (n // K))
            self.dma_since.append(op)
        self.q[eng].append(op)
        return op

    def barrier(self):
        last = [self.q[e][-1] for e in ("pe", "act", "dve", "pool") if self.q[e]]
        last = [o for o in last if o.fn is not None]
        lasts = []
        for e in ("pe", "act", "dve", "pool"):
            for o in reversed(self.q[e]):
                if o.fn is not None and not o.dma:
                    lasts.append(o)
                    break
        dm = list(self.dma_since)
        self.dma_since = []
        for e in self.ENGS:
            op = Op(e, None, False)
            op.deps = [o for o in lasts if o.eng != e] + dm
            self.q[e].append(op)

    def emit(self, stack):
        nc = self.nc
        for e in self.ENGS:
            for op in self.q[e]:
                for d in op.deps:
                    if not d.dma:
                        d.signal = True
        for e in self.ENGS:
            c = 0
            for op in self.q[e]:
                if op.signal and not op.dma:
                    c += 1
                    op.sig = c
        csem = {e: stack.enter_context(nc.semaphore("c_" + e)) for e in ("pe", "act", "dve", "pool")}
        rsem = {e: [stack.enter_context(nc.semaphore("r_%s%d" % (e, i))) for i in range(self.RING[e])]
                for e in ("sp", "pool", "act")}
        block = stack.enter_context(nc.Block())
        engmap = {"pe": block.tensor, "act": block.scalar, "dve": block.vector, "pool": block.gpsimd,
                  "sp": block.sync}

        def mk(e):
            ops = self.q[e]

            def body(eng):
                waited = {}

                def wait(sem, key, val):
                    if waited.get(key, 0) >= val:
                        return
                    waited[key] = val
                    eng.wait_ge(sem, val)

                for op in ops:
                    for d in op.deps:
                        if d.dma:
                            wait(rsem[d.eng][d.dsem], (d.eng, d.dsem), d.dval)
                        else:
                            wait(csem[d.eng], d.eng, d.sig)
                    if op.ring_wait is not None:
                        wait(rsem[e][op.ring_wait[0]], (e, op.ring_wait[0]), op.ring_wait[1])
                    if op.fn is None:
                        continue
                    ins = op.fn(eng)
                    if op.dma:
                        ins.then_inc(rsem[e][op.dsem], 16)
                    elif op.signal:
                        ins.then_inc(csem[e], 1)
                if e in rsem:
                    n = self.ndma[e]
                    K = self.RING[e]
                    for s in range(min(n, K)):
                        wait(rsem[e][s], (e, s), 16 * ((n - 1 - s) // K + 1))
            return body

        for e in self.ENGS:
            engmap[e](mk(e))


def build(cfg, debug=False):
    S_ = cfg["S"]
    NG = cfg["NG"]
    NE = NG * 8
    NCH = S_ // 2048
    NQ = NCH * 512
    NTQ = NQ // 128
    NTKV = S_ // 128
    NR = 8 + NE
    NB = -(-(2 * NQ + NE * (MB - 1)) // MB)
    NBMAX = -(-NQ // MB)

    nc = bass.Bass("TRN2", target_bir_lowering=False)
    din = lambda n, s, dt=F32: nc.dram_tensor(n, list(s), dt, kind="ExternalInput").ap()
    dscr = lambda n, s, dt: nc.dram_tensor(n, list(s), dt, kind="Internal").ap()
    xkv = din("xkv", [S_, D])
    xq = din("xq", [NCH, 640, D])
    cs_kv_d = din("cs_kv", [128, NTKV, 32])
    cs_q_d = din("cs_q", [128, NTQ, 32])
    masks_d = din("masks", [128, 16, 512], BF16)
    bands_d = din("bands", [128, 3, 4, 128], BF16)
    cst_d = din("cst", [128, 3, 128], BF16)
    thr_d = din("thr", [128, NE, NBMAX])
    bst_d = din("bst", [128, NB, NE])
    iop_d = din("iop", [128, 1])
    g_mix = din("norm_mix_g", [1, D])
    w_in = din("w_in", [D, 4096])
    w_pool = din("w_pool", [4, 256, 256])
    pool_scale = din("pool_scale", [128, 8])
    lams = din("lams", [1, 256])
    subln_g = din("subln_g", [128, 1])
    w_out = din("w_out", [D, D])
    g_ffn = din("norm_ffn_g", [1, D])
    w_r = din("w_r", [D, NR])
    b_r = din("b_r", [1, NR])
    w_gate = din("w_gate", [NE, D, 512])
    w_up = din("w_up", [NE, D, 512])
    w_down = din("w_down", [NE, 512, D])
    g_fin = din("norm_final_g", [1, D])
    out_d = nc.dram_tensor("out", [NQ, D], F32, kind="ExternalOutput").ap()
    dbg = {}
    if debug:
        dbg["mixT"] = nc.dram_tensor("dbg_mixT", [16, 128, NQ], BF16, kind="ExternalOutput").ap()
        dbg["h"] = nc.dram_tensor("dbg_h", [NQ, D], F32, kind="ExternalOutput").ap()
        dbg["lall"] = nc.dram_tensor("dbg_lall", [128, NTQ, NR], F32, kind="ExternalOutput").ap()
        dbg["dest"] = nc.dram_tensor("dbg_dest", [128, 2, NTQ], I32, kind="ExternalOutput").ap()
        dbg["gate"] = nc.dram_tensor("dbg_gate", [128, 2, NTQ], F32, kind="ExternalOutput").ap()
        dbg["bexp"] = nc.dram_tensor("dbg_bexp", [128, NB], I32, kind="ExternalOutput").ap()

    KT_d = dscr("KT_d", [NH, 128, S_], BF16)
    V_d = dscr("V_d", [NH, 128, NTKV, 128], BF16)
    QT_d = dscr("QT_d", [NH, 128, NQ], BF16)
    MIXT_d = dbg["mixT"] if debug else dscr("MIXT_d", [16, 128, NQ], BF16)
    H_d = dbg["h"] if debug else dscr("H_d", [NQ, D], F32)
    HN_d = dscr("HN_d", [NQ, D], BF16)
    XS_d = dscr("XS_d", [NB * MB, D], BF16)
    YS_d = dscr("YS_d", [NB * MB, D], BF16)

    S = Sched(nc)
    A = S.add

    def DMA(q, out, in_, r=(), w=()):
        return S.add(q, lambda e: e.dma_start(out=out, in_=in_), r, w, dma=True)

    with ExitStack() as st:
        ARENA = 94000
        arena = st.enter_context(nc.sbuf_tensor("arena", [128, ARENA], BF16))
        psum = st.enter_context(nc.psum_tensor("psum", [128, 8, 512], F32))
        state = {"off": 0, "base": 0}

        def carve(shape, dt):
            n = int(np.prod(shape[1:]))
            nb = n * (2 if dt in (F32, I32) else 1)
            nb = (nb + 15) // 16 * 16
            o = state["off"]
            assert o + nb <= ARENA, ("SBUF arena overflow", o, nb)
            state["off"] = o + nb
            v = arena[:, o:o + nb]
            if dt != BF16:
                v = v.bitcast(dt)
            v = v[:, 0:n]
            if len(shape) == 3:
                v = v.rearrange("p (a b) -> p a b", b=shape[2])
            elif len(shape) == 4:
                v = v.rearrange("p (a b c) -> p a b c", b=shape[2], c=shape[3])
            return v

        def new_phase():
            S.barrier()
            state["off"] = state["base"]

        def pbank(b, n=1, dt=F32):
            v = psum[:, b:b + n, :].rearrange("p a b -> p (a b)")
            if dt != F32:
                v = v.bitcast(dt)
            return v

        cst = carve([128, 3, 128], BF16); b_cst = Buf()
        ident, ones, tri = cst[:, 0, :], cst[:, 1, :], cst[:, 2, :]
        gmix_bc = carve([128, D], F32); b_gmix = Buf()
        lall = carve([128, NTQ, NR], F32); b_lall = Buf()
        neglam = carve([128, 1], F32); b_neglam = Buf()
        gsc = carve([128, 1], F32); b_gsc = Buf()
        iop = carve([128, 1], F32); b_iop = Buf()
        dest_i = carve([128, 2, NTQ], I32); b_dest = Buf()
        gates = carve([128, 2, NTQ], F32); b_gates = Buf()
        widx = carve([128, NB], I32); b_widx = Buf()
        small = carve([128, 8], F32)
        epsb = carve([128, 1], F32); b_epsb = Buf()
        A("dve", lambda e: e.memset(epsb, EPS), (), [b_epsb])
        DMA("sp", cst, cst_d, w=[b_cst])
        DMA("sp", gmix_bc, g_mix.partition_broadcast(128).rearrange("p o d -> p (o d)"), w=[b_gmix])
        DMA("sp", iop, iop_d, w=[b_iop])
        state["base"] = state["off"]

        lam_t = carve([128, 256], F32); b_lamt = Buf()
        lam_p = carve([128, 128], F32); b_lamp = Buf()
        lam_s = carve([128, 2], F32); b_lams = Buf()
        DMA("sp", lam_t, lams.partition_broadcast(128).rearrange("p o d -> p (o d)"), w=[b_lamt])
        lv = lam_t.rearrange("p (a b c) -> p a b c", a=2, b=2)
        A("dve", lambda e: e.tensor_tensor(out=lam_p.rearrange("p (a c) -> p a c", a=2), in0=lv[:, :, 0, :],
                                           in1=lv[:, :, 1, :], op=ALU.mult), [b_lamt], [b_lamp])
        A("dve", lambda e: e.tensor_reduce(out=lam_s, in_=lam_p.rearrange("p (a c) -> p a c", a=2), axis=AX.X,
                                           op=ALU.add), [b_lamp], [b_lams])
        A("act", lambda e: e.activation(out=lam_s, in_=lam_s, func=ACTF.Exp), [b_lams], [b_lams])
        A("dve", lambda e: e.scalar_tensor_tensor(out=neglam, in0=lam_s[:, 1:2], scalar=-LAM_INIT, in1=lam_s[:, 0:1],
                                                  op0=ALU.add, op1=ALU.subtract), [b_lams], [b_neglam])
        DMA("sp", gsc, subln_g, w=[b_gsc])
        A("dve", lambda e: e.tensor_scalar(out=gsc, in0=gsc, scalar1=1.0 - LAM_INIT, scalar2=None, op0=ALU.mult),
          [b_gsc], [b_gsc])

        def norm_transpose(src, xt, b_xt, junk, b_junk, ss, b_ss, xb, b_xb, xT, b_xT, gbc, b_gbc, pT, b_pT):
            if src is not None:
                DMA("sp", xt, src, w=[b_xt])
            A("act", lambda e: e.activation(out=junk, in_=xt, func=ACTF.Square, accum_out=ss), [b_xt], [b_junk, b_ss])
            A("act", lambda e: e.activation(out=ss, in_=ss, func=ACTF.Sqrt, scale=1.0 / D, bias=EPS), [b_ss], [b_ss])
            A("dve", lambda e: e.reciprocal(out=ss, in_=ss), [b_ss], [b_ss])
            A("dve", lambda e: e.scalar_tensor_tensor(out=xb, in0=xt, scalar=ss[:, 0:1], in1=gbc, op0=ALU.mult,
                                                      op1=ALU.mult), [b_xt, b_ss, b_gbc], [b_xb])
            if xT is None:
                return
            for k in range(KC):
                A("pe", lambda e, k=k: e.transpose(out=pT[:, k * 128:(k + 1) * 128], in_=xb[:, k * 128:(k + 1) * 128],
                                                   identity=ident), [b_xb, b_cst], [b_pT])
            A("act", lambda e: e.activation(out=xT[:, 0:8, :].rearrange("p a b -> p (a b)"), in_=pT[:, 0:1024],
                                            func=ACTF.Copy), [b_pT], [b_xT])
            A("dve", lambda e: e.tensor_copy(out=xT[:, 8:16, :].rearrange("p a b -> p (a b)"), in_=pT[:, 1024:2048]),
              [b_pT], [b_xT])

        def rope(pk, cs_t, ksb, tmp1, tmp2, rb, wb, b_tmp):
            for hb in range(2):
                pv = pk[:, hb * 512:(hb + 1) * 512].rearrange("p (g d) -> p g d", d=64)
                kv = ksb[:, hb * 512:(hb + 1) * 512].rearrange("p (g d) -> p g d", d=64)
                t1 = tmp1[:, hb * 8:(hb + 1) * 8, :]
                t2 = tmp2[:, hb * 8:(hb + 1) * 8, :]
                cosb = cs_t[:, 0:16].unsqueeze(1).to_broadcast([128, 8, 16])
                s0 = cs_t[:, 16:24].unsqueeze(1).to_broadcast([128, 8, 8])
                s1 = cs_t[:, 24:32].unsqueeze(1).to_broadcast([128, 8, 8])
                import os as _os
                RV = 3
                A("act", lambda e, pv=pv, kv=kv: e.activation(out=kv[:, :, 16:64], in_=pv[:, :, 16:64], func=ACTF.Copy), rb, wb)
                if RV == 1:
                    continue
                if RV == 3:
                    t3 = tmp2[:, hb * 8:(hb + 1) * 8, :]
                    A("act", lambda e, pv=pv, t3=t3: e.activation(out=t3, in_=pv[:, :, 0:16], func=ACTF.Copy), rb, [b_tmp])
                    A("dve", lambda e, t1=t1, t3=t3, cosb=cosb: e.tensor_tensor(out=t1, in0=t3, in1=cosb, op=ALU.mult), rb + [b_tmp], [b_tmp])
                    A("dve", lambda e, kv=kv, t3=t3, s0=s0: e.tensor_tensor(out=kv[:, :, 0:8], in0=t3[:, :, 8:16], in1=s0, op=ALU.mult), rb + [b_tmp], wb)
                    A("dve", lambda e, kv=kv, t3=t3, s1=s1: e.tensor_tensor(out=kv[:, :, 8:16], in0=t3[:, :, 0:8], in1=s1, op=ALU.mult), rb + [b_tmp], wb)
                    A("dve", lambda e, kv=kv, t1=t1: e.tensor_tensor(out=kv[:, :, 0:16], in0=kv[:, :, 0:16], in1=t1, op=ALU.add), [b_tmp] + wb, wb)
                    continue
                if RV == 2:
                    A("dve", lambda e, pv=pv, t1=t1, t2=t2: e.tensor_tensor(out=t1, in0=pv[:, :, 0:16], in1=t2, op=ALU.mult), rb, [b_tmp])
                    A("dve", lambda e, kv=kv, t1=t1, t2=t2: e.tensor_tensor(out=kv[:, :, 0:16], in0=t1, in1=t2, op=ALU.add), [b_tmp], wb)
                    continue
                A("dve", lambda e, pv=pv, t1=t1, cosb=cosb: e.tensor_tensor(out=t1, in0=pv[:, :, 0:16], in1=cosb, op=ALU.mult), rb, [b_tmp])
                A("dve", lambda e, pv=pv, t2=t2, s0=s0: e.tensor_tensor(out=t2[:, :, 0:8], in0=pv[:, :, 8:16], in1=s0, op=ALU.mult), rb, [b_tmp])
                A("dve", lambda e, pv=pv, t2=t2, s1=s1: e.tensor_tensor(out=t2[:, :, 8:16], in0=pv[:, :, 0:8], in1=s1, op=ALU.mult), rb, [b_tmp])
                A("dve", lambda e, kv=kv, t1=t1, t2=t2: e.tensor_tensor(out=kv[:, :, 0:16], in0=t1, in1=t2, op=ALU.add), [b_tmp], wb)

        b_KTd = Buf(); b_Vd = Buf(); b_QTd = Buf(); b_MIXd = Buf(); b_Hd = Buf(); b_HNd = Buf(); b_XSd = Buf(); b_YSd = Buf()
        def ph1():
            wkv = carve([128, KC, 2048], BF16); b_wkv = [Buf() for _ in range(KC)]
            for k in range(KC):
                S.add("pool", lambda e, k=k: e.dma_start(out=wkv[:, k, :], in_=w_in[k * 128:(k + 1) * 128, 2048:4096]),
                      (), [b_wkv[k]], dma=True)
            cs_kv = carve([128, NTKV, 32], F32); b_cskv = Buf()
            DMA("sp", cs_kv, cs_kv_d, w=[b_cskv])
            xt = [carve([128, D], F32) for _ in range(3)]; b_xt = [Buf() for _ in range(3)]
            junk = carve([128, D], BF16); b_junk = Buf()
            ssb = [carve([128, 1], F32) for _ in range(2)]; b_ss = [Buf() for _ in range(2)]
            xb = [carve([128, D], BF16) for _ in range(2)]; b_xb = [Buf() for _ in range(2)]
            xT = [carve([128, KC, 128], BF16) for _ in range(2)]; b_xT = [Buf() for _ in range(2)]
            ksb = [carve([128, 1024], BF16) for _ in range(2)]; b_ksb = [Buf() for _ in range(2)]
            vsb = [carve([128, 4, 1024], BF16) for _ in range(2)]; b_vsb = [Buf() for _ in range(2)]
            kTs = [carve([128, NH, 512], BF16) for _ in range(2)]; b_kTs = [Buf() for _ in range(2)]
            tmp1 = carve([128, 16, 16], F32); tmp2 = carve([128, 16, 16], F32); b_tmp = Buf()
            pT = pbank(0, 2, BF16); b_pT = Buf()
            pK = pbank(2, 2); b_pK = Buf()
            pV = pbank(4, 2); b_pV = Buf()
            pKT = pbank(6, 1, BF16); b_pKT = Buf()
            def loadA(t):
                DMA("sp", xt[t % 3], xkv[t * 128:(t + 1) * 128, :], w=[b_xt[t % 3]])

            def frontA(t):
                s2 = t % 2
                norm_transpose(None, xt[t % 3], b_xt[t % 3], junk, b_junk, ssb[s2], b_ss[s2], xb[s2],
                               b_xb[s2], xT[s2], b_xT[s2], gmix_bc, b_gmix, pT, b_pT)
            loadA(0)
            loadA(1)
            frontA(0)
            for t in range(NTKV):
                s2 = t % 2
                g4 = (t // 4) % 2
                if t + 2 < NTKV:
                    loadA(t + 2)
                if t + 1 < NTKV:
                    frontA(t + 1)
                LV = cfg.get("lv", 9)
                if LV < 1:
                    continue
                for cg in range(4):
                    dst, bd = (pK, b_pK) if cg < 2 else (pV, b_pV)
                    for k in range(KC):
                        import os as _os
                        _N = int(_os.environ.get("EXPN", 512))
                        if _os.environ.get("WSRC"):
                            A("pe", lambda e, k=k, cg=cg, dst=dst, s2=s2: e.matmul(
                                dst[:, (cg % 2) * 512:(cg % 2) * 512 + _N], lhsT=xT[s2][:, k, :],
                                rhs=xb[s2][:, 0:_N], start=(k == 0), stop=(k == KC - 1)),
                              [b_xT[s2], b_xb[s2]], [bd])
                        else:
                            A("pe", lambda e, k=k, cg=cg, dst=dst, s2=s2: e.matmul(
                                dst[:, (cg % 2) * 512:(cg % 2) * 512 + _N], lhsT=xT[s2][:, k, :],
                                rhs=wkv[:, k, cg * 512:cg * 512 + _N], start=(k == 0), stop=(k == KC - 1)),
                              [b_xT[s2], b_wkv[k]], [bd])
                if LV < 2:
                    continue
                rope(pK, cs_kv[:, t, :], ksb[s2], tmp1, tmp2, [b_pK, b_cskv], [b_ksb[s2]], b_tmp)
                if LV < 3:
                    continue
                A("act", lambda e, t=t, g4=g4: e.activation(out=vsb[g4][:, t % 4, :], in_=pV, func=ACTF.Copy),
                  [b_pV], [b_vsb[g4]])
                if LV < 4:
                    continue
                for h in range(NH):
                    A("pe", lambda e, h=h, s2=s2: e.transpose(out=pKT[:, h * 128:(h + 1) * 128],
                                                              in_=ksb[s2][:, h * 128:(h + 1) * 128], identity=ident),
                      [b_ksb[s2], b_cst], [b_pKT])
                A("dve", lambda e, t=t, g4=g4: e.tensor_copy(out=kTs[g4][:, :, (t % 4) * 128:(t % 4 + 1) * 128],
                                                             in_=pKT.rearrange("p (h c) -> p h c", c=128)),
                  [b_pKT], [b_kTs[g4]])
                if t % 4 == 3 and LV >= 5:
                    t0 = (t // 4) * 4
                    DMA("pool", KT_d[:, :, t0 * 128:(t0 + 4) * 128].rearrange("h p c -> p h c"), kTs[g4], [b_kTs[g4]], [b_KTd])
                    for tt in range(4):
                        DMA("pool", V_d[:, :, t0 + tt, :].rearrange("h p e -> p h e"),
                            vsb[g4][:, tt, :].rearrange("p (h e) -> p h e", e=128), [b_vsb[g4]], [b_Vd])

        if cfg.get('maxph', 8) >= 1:
            ph1()
        def ph2():
            new_phase()
            wq = carve([128, KC, 2048], BF16); b_wq = [Buf() for _ in range(KC)]
            for k in range(KC):
                S.add("pool", lambda e, k=k: e.dma_start(out=wq[:, k, :], in_=w_in[k * 128:(k + 1) * 128, 0:2048]),
                      (), [b_wq[k]], dma=True)
            wp = carve([128, 8, 256], BF16); b_wp = Buf()
            S.add("pool", lambda e: e.dma_start(out=wp, in_=w_pool.rearrange("g (cc p) d -> p (g cc) d", p=128)),
                  (), [b_wp], dma=True)
            psc = carve([128, 8], F32); b_psc = Buf()
            DMA("sp", psc, pool_scale, w=[b_psc])
            bands = carve([128, 3, 4, 128], BF16); b_bands = Buf()
            DMA("sp", bands, bands_d, w=[b_bands])
            cs_q = carve([128, NTQ, 32], F32); b_csq = Buf()
            DMA("sp", cs_q, cs_q_d, w=[b_csq])
            xt = [carve([128, D], F32) for _ in range(2)]; b_xt = [Buf() for _ in range(2)]
            junk = carve([128, D], BF16); b_junk = Buf()
            ssb = [carve([128, 1], F32) for _ in range(2)]; b_ss = [Buf() for _ in range(2)]
            xb = [carve([128, D], BF16) for _ in range(2)]; b_xb = [Buf() for _ in range(2)]
            xT = [carve([128, KC, 128], BF16) for _ in range(2)]; b_xT = [Buf() for _ in range(2)]
            qsb = [carve([128, 1024], BF16) for _ in range(2)]; b_qsb = [Buf() for _ in range(2)]
            pin = [carve([128, 1024], BF16) for _ in range(3)]; b_pin = [Buf() for _ in range(3)]
            qTs = [carve([128, NH, 128], BF16) for _ in range(2)]; b_qTs = [Buf() for _ in range(2)]
            pldT = [carve([128, 8, 128], BF16) for _ in range(2)]; b_pldT = [Buf() for _ in range(2)]
            mxT = [carve([128, 8, 128], BF16) for _ in range(2)]; b_mxT = [Buf() for _ in range(2)]
            tmp1 = carve([128, 16, 16], F32); tmp2 = carve([128, 16, 16], F32); b_tmp = Buf()
            pT = pbank(0, 2, BF16); b_pT = Buf()
            pP = pbank(2, 2); b_pP = Buf()
            pQ = pbank(4, 2); b_pQ = Buf()
            pQT = pbank(6, 1, BF16); b_pQT = Buf()
            pM = pbank(7, 1); b_pM = Buf()
            tilesB = [(i, r) for i in range(NCH) for r in range(5)]

            def frontB(cn):
                i, r = tilesB[cn]
                s2 = cn % 2
                norm_transpose(xq[i, r * 128:(r + 1) * 128, :], xt[s2], b_xt[s2], junk, b_junk, ssb[s2], b_ss[s2],
                               xb[s2], b_xb[s2], xT[s2], b_xT[s2], gmix_bc, b_gmix, pT, b_pT)
            frontB(0)
            cnt = 0
            for i in range(NCH):
                for r in range(5):
                    s2 = cnt % 2
                    s3 = cnt % 3
                    sp3 = (cnt - 1) % 3
                    cnt += 1
                    if cnt < len(tilesB):
                        frontB(cnt)
                    for cg in range(2 if r == 0 else 4):
                        dst, bd = (pP, b_pP) if cg < 2 else (pQ, b_pQ)
                        for k in range(KC):
                            A("pe", lambda e, k=k, cg=cg, dst=dst, s2=s2: e.matmul(
                                dst[:, (cg % 2) * 512:(cg % 2 + 1) * 512], lhsT=xT[s2][:, k, :],
                                rhs=wq[:, k, cg * 512:(cg + 1) * 512], start=(k == 0), stop=(k == KC - 1)),
                              [b_xT[s2], b_wq[k]], [bd])
                    A("act", lambda e, s3=s3: e.activation(out=pin[s3], in_=pP, func=ACTF.Copy), [b_pP], [b_pin[s3]])
                    if r == 0:
                        continue
                    tq = i * 4 + (r - 1)
                    rope(pQ, cs_q[:, tq, :], qsb[s2], tmp1, tmp2, [b_pQ, b_csq], [b_qsb[s2]], b_tmp)
                    for h in range(NH):
                        A("pe", lambda e, h=h, s2=s2: e.transpose(out=pQT[:, h * 128:(h + 1) * 128],
                                                                  in_=qsb[s2][:, h * 128:(h + 1) * 128], identity=ident),
                          [b_qsb[s2], b_cst], [b_pQT])
                    A("dve", lambda e, s2=s2: e.tensor_copy(out=qTs[s2].rearrange("p h c -> p (h c)"), in_=pQT),
                      [b_pQT], [b_qTs[s2]])
                    DMA("pool", QT_d[:, :, tq * 128:(tq + 1) * 128].rearrange("h p c -> p h c"), qTs[s2], [b_qTs[s2]], [b_QTd])
                    bsel = 2 if (i == 0 and r == 1) else 0
                    for half in range(2):
                        for u in range(4):
                            gc = half * 4 + u
                            g = gc // 2
                            o_ = pM[:, u * 128:(u + 1) * 128]
                            A("pe", lambda e, gc=gc, g=g, o_=o_, s3=s3, bsel=bsel: e.matmul(
                                o_, lhsT=pin[s3][:, gc * 128:(gc + 1) * 128], rhs=bands[:, bsel, g, :], start=True, stop=False),
                              [b_pin[s3], b_bands], [b_pM])
                            A("pe", lambda e, gc=gc, g=g, o_=o_, sp3=sp3: e.matmul(
                                o_, lhsT=pin[sp3][:, gc * 128:(gc + 1) * 128], rhs=bands[:, 1, g, :], start=False, stop=True),
                              [b_pin[sp3], b_bands], [b_pM])
                        A("dve", lambda e, half=half, s2=s2: e.tensor_copy(
                            out=pldT[s2][:, half * 4:(half + 1) * 4, :].rearrange("p a b -> p (a b)"), in_=pM),
                          [b_pM], [b_pldT[s2]])
                    for half in range(2):
                        for u in range(4):
                            gd = half * 4 + u
                            g, dd = gd // 2, gd % 2
                            o_ = pM[:, u * 128:(u + 1) * 128]
                            for cc in range(2):
                                A("pe", lambda e, g=g, dd=dd, cc=cc, o_=o_, s2=s2: e.matmul(
                                    o_, lhsT=wp[:, g * 2 + cc, dd * 128:(dd + 1) * 128], rhs=pldT[s2][:, g * 2 + cc, :],
                                    start=(cc == 0), stop=(cc == 1)), [b_wp, b_pldT[s2]], [b_pM])
                        for u in range(4):
                            gd = half * 4 + u
                            A("act", lambda e, gd=gd, u=u, s2=s2: e.activation(
                                out=mxT[s2][:, gd, :], in_=pM[:, u * 128:(u + 1) * 128], func=ACTF.Identity,
                                scale=psc[:, gd:gd + 1]), [b_pM, b_psc], [b_mxT[s2]])
                    DMA("pool", MIXT_d[0:8, :, tq * 128:(tq + 1) * 128].rearrange("f p c -> p f c"), mxT[s2], [b_mxT[s2]], [b_MIXd])

        if cfg.get('maxph', 8) >= 2:
            ph2()
        def ph3():
            new_phase()
            NSEG = 4
            SEG = S_ // NSEG
            kt_sb = carve([128, S_], BF16); b_kt = [Buf() for _ in range(NSEG)]
            v_sb = carve([128, NTKV, 128], BF16); b_v = [Buf() for _ in range(NSEG)]
            qt_sb = [carve([128, NQ], BF16) for _ in range(2)]; b_qt = [Buf() for _ in range(2)]
            msk = carve([128, 16, 512], BF16); b_msk = Buf()
            DMA("sp", msk, masks_d, w=[b_msk])
            NPT = 5
            pt = [[carve([128, 512], BF16) for _ in range(NPT)] for _ in range(2)]
            b_pt = [[Buf() for _ in range(NPT)] for _ in range(2)]
            sacc = [[carve([128, 512], BF16) for _ in range(2)] for _ in range(2)]
            b_sacc = [[Buf() for _ in range(2)] for _ in range(2)]
            rr = [carve([128, 512], F32) for _ in range(2)]; b_rr = [Buf() for _ in range(2)]
            oo = [carve([128, 512], F32) for _ in range(2)]; b_oo = [Buf() for _ in range(2)]
            sq = carve([128, 512], BF16); b_sq = Buf()
            rs = carve([128, 512], F32); b_rs = Buf()
            at = [carve([128, 512], BF16) for _ in range(2)]; b_at = [Buf() for _ in range(2)]
            pS = [[pbank(c * 2 + s) for s in range(2)] for c in range(2)]
            b_pS = [[Buf() for _ in range(2)] for _ in range(2)]
            pO = [pbank(4 + c) for c in range(2)]; b_pO = [Buf() for _ in range(2)]
            pL = [pbank(6 + c) for c in range(2)]; b_pL = [Buf() for _ in range(2)]
            st3 = {"ecnt": 0}

            def head_load(h):
                hs = h % 2
                for sg in range(NSEG):
                    DMA("sp", kt_sb[:, sg * SEG:(sg + 1) * SEG], KT_d[h, :, sg * SEG:(sg + 1) * SEG], [b_KTd], [b_kt[sg]])
                    DMA("sp", v_sb[:, sg * SEG // 128:(sg + 1) * SEG // 128, :],
                        V_d[h, :, sg * SEG // 128:(sg + 1) * SEG // 128, :], [b_Vd], [b_v[sg]])
                DMA("sp", qt_sb[hs], QT_d[h], [b_QTd], [b_qt[hs]])

            def qk(n, u):
                h, i, kb, nkb = u
                hs = h % 2
                sg = (kb * 128) // SEG
                sl = n % 2
                for c in range(2):
                    A("pe", lambda e, c=c, kb=kb, i=i, sl=sl, hs=hs: e.matmul(
                        pS[c][sl], lhsT=kt_sb[c * 64:(c + 1) * 64, kb * 128:(kb + 1) * 128],
                        rhs=qt_sb[hs][c * 64:(c + 1) * 64, i * 512:(i + 1) * 512], start=True, stop=True),
                      [b_kt[sg], b_qt[hs]], [b_pS[c][sl]])

            def ex(n, u):
                h, i, kb, nkb = u
                sl = n % 2
                ps3 = n % NPT
                for c in range(2):
                    A("act", lambda e, c=c, sl=sl, ps3=ps3: e.activation(out=pt[c][ps3], in_=pS[c][sl], func=ACTF.Exp,
                                                                       scale=0.125),
                      [b_pS[c][sl]], [b_pt[c][ps3]])
                if kb >= nkb - 16:
                    mi = kb - (nkb - 16)
                    for c in range(2):
                        A("pool" if c == 0 else "dve", lambda e, c=c, ps3=ps3, mi=mi: e.tensor_tensor(
                            out=pt[c][ps3], in0=pt[c][ps3], in1=msk[:, mi, :], op=ALU.mult),
                          [b_pt[c][ps3], b_msk], [b_pt[c][ps3]])

            def pv(n, u):
                h, i, kb, nkb = u
                sg = (kb * 128) // SEG
                ps3 = n % NPT
                pp3 = (n - 1) % NPT
                g2 = (kb // 4) % 2
                for c in range(2):
                    A("pe", lambda e, c=c, kb=kb, ps3=ps3, nkb=nkb: e.matmul(
                        pO[c], lhsT=v_sb[:, kb, :], rhs=pt[c][ps3], start=(kb == 0), stop=(kb == nkb - 1)),
                      [b_v[sg], b_pt[c][ps3]], [b_pO[c]])
                    if kb % 4 == 1:
                        A("dve", lambda e, c=c, ps3=ps3, pp3=pp3, g2=g2: e.tensor_tensor(
                            out=sacc[c][g2], in0=pt[c][pp3], in1=pt[c][ps3], op=ALU.add),
                          [b_pt[c][pp3], b_pt[c][ps3]], [b_sacc[c][g2]])
                    elif kb % 4 >= 2:
                        A("dve", lambda e, c=c, ps3=ps3, g2=g2: e.tensor_tensor(
                            out=sacc[c][g2], in0=sacc[c][g2], in1=pt[c][ps3], op=ALU.add),
                          [b_sacc[c][g2], b_pt[c][ps3]], [b_sacc[c][g2]])
                    if kb % 4 == 3:
                        A("pe", lambda e, c=c, kb=kb, g2=g2, nkb=nkb: e.matmul(
                            pL[c], lhsT=ones, rhs=sacc[c][g2], start=(kb == 3), stop=(kb == nkb - 1)),
                          [b_cst, b_sacc[c][g2]], [b_pL[c]])

            def epi_a():
                for c in range(2):
                    A("act", lambda e, c=c: e.activation(out=rr[c], in_=pL[c], func=ACTF.Copy), [b_pL[c]], [b_rr[c]])
                    A("act", lambda e, c=c: e.activation(out=oo[c], in_=pO[c], func=ACTF.Copy), [b_pO[c]], [b_oo[c]])
                for c in range(2):
                    A("dve", lambda e, c=c: e.reciprocal(out=rr[c], in_=rr[c]), [b_rr[c]], [b_rr[c]])
                    A("dve", lambda e, c=c: e.tensor_tensor(out=oo[c], in0=oo[c], in1=rr[c], op=ALU.mult),
                      [b_oo[c], b_rr[c]], [b_oo[c]])
                A("dve", lambda e: e.scalar_tensor_tensor(out=oo[0], in0=oo[1], scalar=neglam[:, 0:1], in1=oo[0],
                                                          op0=ALU.mult, op1=ALU.add), [b_oo[0], b_oo[1], b_neglam], [b_oo[0]])
                A("dve", lambda e: e.tensor_tensor(out=sq, in0=oo[0], in1=oo[0], op=ALU.mult), [b_oo[0]], [b_sq])

            def epi_b(h, i, n):
                es = st3["ecnt"] % 2
                st3["ecnt"] += 1
                sl = (n + 1) % 2
                A("pe", lambda e, sl=sl: e.matmul(pS[0][sl], lhsT=ones, rhs=sq, start=True, stop=True),
                  [b_cst, b_sq], [b_pS[0][sl]])
                A("act", lambda e, sl=sl: e.activation(out=rs, in_=pS[0][sl], func=ACTF.Ln, scale=1.0 / 128, bias=epsb[:, 0:1]),
                  [b_pS[0][sl], b_epsb], [b_rs])
                A("act", lambda e: e.activation(out=rs, in_=rs, func=ACTF.Exp, scale=-0.5), [b_rs], [b_rs])
                A("dve", lambda e, es=es: e.scalar_tensor_tensor(out=at[es], in0=oo[0], scalar=gsc[:, 0:1], in1=rs,
                                                               op0=ALU.mult, op1=ALU.mult),
                  [b_oo[0], b_rs, b_gsc], [b_at[es]])
                DMA("sp", MIXT_d[8 + h, :, i * 512:(i + 1) * 512], at[es], [b_at[es]], [b_MIXd])

            gn = 0
            for h in range(NH):
                units = [(h, i, kb, 16 * (i + 1)) for i in range(NCH) for kb in range(16 * (i + 1))]
                N = len(units)
                head_load(h)
                pend = []
                for n in range(N + 2):
                    if n < N:
                        qk(gn + n, units[n])
                        ex(gn + n, units[n])
                    m = n - 2
                    if m >= 0:
                        pv(gn + m, units[m])
                        _, i_, kb_, nkb_ = units[m]
                        if kb_ == nkb_ - 1:
                            epi_a()
                            pend.append((n + 4, h, i_))
                    while pend and (pend[0][0] <= n or n == N + 1):
                        _, hh_, ii_ = pend.pop(0)
                        epi_b(hh_, ii_, gn + n)
                gn += N

        if cfg.get('maxph', 8) >= 3:
            ph3()
        def ph4():
            new_phase()
            wo = carve([128, KC, D], BF16); b_wo = [Buf() for _ in range(KC)]
            for k in range(KC):
                S.add("pool", lambda e, k=k: e.dma_start(out=wo[:, k, :], in_=w_out[k * 128:(k + 1) * 128, :]),
                      (), [b_wo[k]], dma=True)
            wr = carve([128, KC, NR], BF16); b_wr = Buf()
            S.add("pool", lambda e: e.dma_start(out=wr, in_=w_r.rearrange("(k p) n -> p k n", p=128)), (), [b_wr], dma=True)
            br = carve([128, NR], F32); b_br = Buf()
            DMA("sp", br, b_r.partition_broadcast(128).rearrange("p o d -> p (o d)"), w=[b_br])
            gffn_bc = carve([128, D], F32); b_gffn = Buf()
            DMA("sp", gffn_bc, g_ffn.partition_broadcast(128).rearrange("p o d -> p (o d)"), w=[b_gffn])
            mT = [carve([128, KC, 128], BF16) for _ in range(2)]; b_mT = [Buf() for _ in range(2)]
            xt = [carve([128, D], F32) for _ in range(2)]; b_xt = [Buf() for _ in range(2)]
            hsb = [carve([128, D], F32) for _ in range(2)]; b_hsb = [Buf() for _ in range(2)]
            junk = carve([128, D], BF16); b_junk = Buf()
            ssb = [carve([128, 1], F32) for _ in range(2)]; b_ss = [Buf() for _ in range(2)]
            hn = [carve([128, D], BF16) for _ in range(2)]; b_hn = [Buf() for _ in range(2)]
            hnT = [carve([128, KC, 128], BF16) for _ in range(2)]; b_hnT = [Buf() for _ in range(2)]
            pH = [pbank(b) for b in range(4)]; b_pH = [Buf() for _ in range(4)]
            pT = pbank(4, 2, BF16); b_pT = Buf()
            pR = pbank(6); b_pR = Buf()
            def frontD(t):
                s2 = t % 2
                i, r = t // 4, t % 4
                DMA("sp", mT[s2], MIXT_d[:, :, t * 128:(t + 1) * 128].rearrange("f p c -> p f c"), [b_MIXd], [b_mT[s2]])
                DMA("sp", xt[s2], xq[i, (r + 1) * 128:(r + 2) * 128, :], w=[b_xt[s2]])
                for cg in range(4):
                    for k in range(KC):
                        A("pe", lambda e, k=k, cg=cg, s2=s2: e.matmul(pH[cg], lhsT=mT[s2][:, k, :],
                                                                      rhs=wo[:, k, cg * 512:(cg + 1) * 512],
                                                                      start=(k == 0), stop=(k == KC - 1)),
                          [b_mT[s2], b_wo[k]], [b_pH[cg]])
                    A("act", lambda e, cg=cg, s2=s2: e.activation(out=hsb[s2][:, cg * 512:(cg + 1) * 512], in_=pH[cg],
                                                                  func=ACTF.Copy), [b_pH[cg]], [b_hsb[s2]])
                    A("dve", lambda e, cg=cg, s2=s2: e.tensor_tensor(out=hsb[s2][:, cg * 512:(cg + 1) * 512],
                                                                     in0=hsb[s2][:, cg * 512:(cg + 1) * 512],
                                                                     in1=xt[s2][:, cg * 512:(cg + 1) * 512], op=ALU.add),
                      [b_hsb[s2], b_xt[s2]], [b_hsb[s2]])
                DMA("sp", H_d[t * 128:(t + 1) * 128, :], hsb[s2], [b_hsb[s2]], [b_Hd])
                A("act", lambda e, s2=s2: e.activation(out=junk, in_=hsb[s2], func=ACTF.Square, accum_out=ssb[s2]),
                  [b_hsb[s2]], [b_junk, b_ss[s2]])
                A("act", lambda e, s2=s2: e.activation(out=ssb[s2], in_=ssb[s2], func=ACTF.Sqrt, scale=1.0 / D, bias=EPS),
                  [b_ss[s2]], [b_ss[s2]])
                A("dve", lambda e, s2=s2: e.reciprocal(out=ssb[s2], in_=ssb[s2]), [b_ss[s2]], [b_ss[s2]])
                A("dve", lambda e, s2=s2: e.scalar_tensor_tensor(out=hn[s2], in0=hsb[s2], scalar=ssb[s2][:, 0:1], in1=gffn_bc,
                                                               op0=ALU.mult, op1=ALU.mult),
                  [b_hsb[s2], b_ss[s2], b_gffn], [b_hn[s2]])
                DMA("sp", HN_d[t * 128:(t + 1) * 128, :], hn[s2], [b_hn[s2]], [b_HNd])

            def backD(t):
                s2 = t % 2
                for k in range(KC):
                    A("pe", lambda e, k=k, s2=s2: e.transpose(out=pT[:, k * 128:(k + 1) * 128],
                                                              in_=hn[s2][:, k * 128:(k + 1) * 128], identity=ident),
                      [b_hn[s2], b_cst], [b_pT])
                A("act", lambda e, s2=s2: e.activation(out=hnT[s2].rearrange("p a b -> p (a b)"), in_=pT, func=ACTF.Copy),
                  [b_pT], [b_hnT[s2]])
                for k in range(KC):
                    A("pe", lambda e, k=k, s2=s2: e.matmul(pR[:, 0:NR], lhsT=hnT[s2][:, k, :], rhs=wr[:, k, :],
                                                           start=(k == 0), stop=(k == KC - 1)), [b_hnT[s2], b_wr], [b_pR])
                A("act", lambda e, t=t: e.activation(out=lall[:, t, :], in_=pR[:, 0:NR], func=ACTF.Copy), [b_pR], [b_lall])
                A("dve", lambda e, t=t: e.tensor_tensor(out=lall[:, t, :], in0=lall[:, t, :], in1=br, op=ALU.add),
                  [b_lall, b_br], [b_lall])


            frontD(0)
            for t in range(NTQ):
                if t + 1 < NTQ:
                    frontD(t + 1)
                backD(t)

        if cfg.get('maxph', 8) >= 4:
            ph4()
        def ph5():
            new_phase()
            T_ = NTQ
            V = lambda shape, dt=F32: carve(shape, dt)
            mg = V([128, T_]); bm = Buf()
            maskg = V([128, T_, 8])
            eg = V([128, T_, 8])
            sume = V([128, T_])
            prod = V([128, T_, 8, 8])
            sel = V([128, T_, 8])
            top8 = V([128, T_, 8])
            m1 = V([128, T_, 8]); m2 = V([128, T_, 8])
            dm = V([128, T_]); g1 = V([128, T_])
            E = [V([128, T_, NE], BF16) for _ in range(2)]
            E32 = [V([128, T_, NE]) for _ in range(2)]
            Mt = V([128, T_, NE], BF16)
            Mc = V([128, T_ + 1, NE], BF16)
            rank = V([128, T_, NE])
            cnts = V([128, NE]); nblk = V([128, NE]); pend = V([128, NE]); pst = V([128, NE])
            thr = V([128, NE, NBMAX]); cmp1 = V([128, NE, NBMAX])
            bst = V([128, NB, NE]); cmp2 = V([128, NB, NE]); bexp = V([128, NB])
            onesf = V([128, NE])
            dst_f = V([128, 2, T_])
            bE = Buf()
            DMA("sp", thr, thr_d, w=[bE])
            DMA("sp", bst, bst_d, w=[bE])
            lg = lall[:, :, 0:8]
            le = lall[:, :, 8:NR]
            RW = ([b_lall, bE], [bE])

            def dv(fn, r=RW[0], w=RW[1], eng="dve"):
                A(eng, fn, r, w)
            dv(lambda e: e.tensor_reduce(out=mg, in_=lg, axis=AX.X, op=ALU.max))
            dv(lambda e: e.tensor_tensor(out=maskg, in0=lg, in1=mg.unsqueeze(2).to_broadcast([128, T_, 8]), op=ALU.is_equal))
            dv(lambda e: e.tensor_tensor(out=eg, in0=lg, in1=mg.unsqueeze(2).to_broadcast([128, T_, 8]), op=ALU.subtract))
            dv(lambda e: e.activation(out=eg, in_=eg, func=ACTF.Exp), eng="act")
            dv(lambda e: e.tensor_reduce(out=sume, in_=eg, axis=AX.X, op=ALU.add))
            dv(lambda e: e.reciprocal(out=sume, in_=sume))
            if NG == 8:
                lev = le.rearrange("p t (g i) -> p t g i", i=8)
            else:
                lev = le.rearrange("p t (g i) -> p t g i", i=8)
            for g in range(NG):
                dv(lambda e, g=g: e.tensor_tensor(out=prod[:, :, g, :], in0=lev[:, :, g, :],
                                                  in1=maskg[:, :, g:g + 1].to_broadcast([128, T_, 8]), op=ALU.mult))
            dv(lambda e: e.tensor_copy(out=sel, in_=prod[:, :, 0, :]))
            for g in range(1, NG):
                dv(lambda e, g=g: e.tensor_tensor(out=sel, in0=sel, in1=prod[:, :, g, :], op=ALU.add))
            for t in range(T_):
                dv(lambda e, t=t: e.max(out=top8[:, t, :], in_=sel[:, t, :]))
            dv(lambda e: e.tensor_tensor(out=m1, in0=sel, in1=top8[:, :, 0:1].to_broadcast([128, T_, 8]), op=ALU.is_equal))
            dv(lambda e: e.tensor_tensor(out=m2, in0=sel, in1=top8[:, :, 1:2].to_broadcast([128, T_, 8]), op=ALU.is_equal))
            dv(lambda e: e.tensor_tensor(out=dm, in0=top8[:, :, 1], in1=top8[:, :, 0], op=ALU.subtract))
            dv(lambda e: e.activation(out=dm, in_=dm, func=ACTF.Exp), eng="act")
            dv(lambda e: e.tensor_scalar(out=dm, in0=dm, scalar1=1.0, scalar2=None, op0=ALU.add))
            dv(lambda e: e.reciprocal(out=g1, in_=dm))
            dv(lambda e: e.tensor_tensor(out=gates[:, 0, :], in0=g1, in1=sume, op=ALU.mult), w=[bE, b_gates])
            dv(lambda e: e.tensor_tensor(out=gates[:, 1, :], in0=sume, in1=gates[:, 0, :], op=ALU.subtract), w=[bE, b_gates])
            for s_, mm in ((0, m1), (1, m2)):
                for g in range(NG):
                    dv(lambda e, s_=s_, mm=mm, g=g: e.tensor_tensor(
                        out=E32[s_][:, :, g * 8:(g + 1) * 8], in0=mm,
                        in1=maskg[:, :, g:g + 1].to_broadcast([128, T_, 8]), op=ALU.mult))
                dv(lambda e, s_=s_: e.tensor_copy(out=E[s_], in_=E32[s_]))
            dv(lambda e: e.tensor_tensor(out=Mt, in0=E[0], in1=E[1], op=ALU.add))
            dv(lambda e: e.memset(Mc[:, 0, :], 0.0))
            for t in range(T_):
                dv(lambda e, t=t: e.tensor_tensor(out=Mc[:, t + 1, :], in0=Mc[:, t, :], in1=Mt[:, t, :], op=ALU.add))
            pRk = psum[:, 0:4, :].rearrange("p a b -> p (a b)")
            b_pRk = Buf()
            PER = 512 // NE
            for t in range(T_):
                o_ = pRk[:, (t // PER) * 512 + (t % PER) * NE:(t // PER) * 512 + (t % PER + 1) * NE]
                A("pe", lambda e, t=t, o_=o_: e.matmul(o_, lhsT=tri, rhs=Mt[:, t, :], start=True, stop=False),
                  [bE, b_cst], [b_pRk])
                A("pe", lambda e, t=t, o_=o_: e.matmul(o_, lhsT=ones, rhs=Mc[:, t, :], start=False, stop=True),
                  [bE, b_cst], [b_pRk])
            pC = pbank(4); b_pC = Buf()
            A("pe", lambda e: e.matmul(pC[:, 0:NE], lhsT=ones, rhs=Mc[:, T_, :], start=True, stop=True), [bE, b_cst], [b_pC])
            for t in range(T_):
                o_ = pRk[:, (t // PER) * 512 + (t % PER) * NE:(t // PER) * 512 + (t % PER + 1) * NE]
                dv(lambda e, t=t, o_=o_: e.tensor_copy(out=rank[:, t, :], in_=o_), r=[b_pRk, bE])
            dv(lambda e: e.tensor_copy(out=cnts, in_=pC[:, 0:NE]), r=[b_pC, bE])
            dv(lambda e: e.tensor_tensor(out=cmp1, in0=thr, in1=cnts.unsqueeze(2).to_broadcast([128, NE, NBMAX]), op=ALU.is_lt))
            dv(lambda e: e.tensor_reduce(out=nblk, in_=cmp1, axis=AX.X, op=ALU.add))
            dv(lambda e: e.memset(onesf, 1.0))
            dv(lambda e: e.tensor_tensor_scan(out=pend, data0=onesf, data1=nblk, initial=0.0, op0=ALU.mult, op1=ALU.add))
            dv(lambda e: e.tensor_tensor(out=pst, in0=pend, in1=nblk, op=ALU.subtract))
            dv(lambda e: e.tensor_scalar(out=pst, in0=pst, scalar1=float(MB), scalar2=None, op0=ALU.mult))
            dv(lambda e: e.tensor_scalar(out=pend, in0=pend, scalar1=float(MB), scalar2=None, op0=ALU.mult))
            dv(lambda e: e.tensor_tensor(out=rank, in0=rank, in1=pst.unsqueeze(1).to_broadcast([128, T_, NE]), op=ALU.add))
            for s_ in range(2):
                dv(lambda e, s_=s_: e.tensor_tensor(out=E32[s_], in0=E32[s_], in1=rank, op=ALU.mult))
                dv(lambda e, s_=s_: e.tensor_reduce(out=dst_f[:, s_, :], in_=E32[s_], axis=AX.X, op=ALU.add))
            dv(lambda e: e.tensor_copy(out=dest_i, in_=dst_f), w=[bE, b_dest])
            dv(lambda e: e.tensor_tensor(out=cmp2, in0=bst, in1=pend.unsqueeze(1).to_broadcast([128, NB, NE]), op=ALU.is_ge))
            dv(lambda e: e.tensor_reduce(out=bexp, in_=cmp2, axis=AX.X, op=ALU.add))
            dv(lambda e: e.tensor_scalar(out=bexp, in0=bexp, scalar1=float(NE - 1), scalar2=128.0, op0=ALU.min, op1=ALU.mult))
            dv(lambda e: e.tensor_scalar(out=bexp, in0=bexp, scalar1=iop[:, 0:1], scalar2=None, op0=ALU.add), r=[bE, b_iop])
            dv(lambda e: e.tensor_copy(out=widx, in_=bexp), w=[bE, b_widx])
            if debug:
                DMA("sp", dbg["lall"], lall, [b_lall], [Buf()])
                DMA("sp", dbg["dest"], dest_i, [b_dest], [Buf()])
                DMA("sp", dbg["gate"], gates, [b_gates], [Buf()])
                DMA("sp", dbg["bexp"], widx, [b_widx], [Buf()])

        if cfg.get('maxph', 8) >= 5:
            ph5()
        def ph6():
            new_phase()
            hn = [carve([128, D], BF16) for _ in range(3)]; b_hn = [Buf() for _ in range(3)]
            for t in range(NTQ):
                s3 = t % 3
                DMA("sp", hn[s3], HN_d[t * 128:(t + 1) * 128, :], [b_HNd], [b_hn[s3]])
                for s_ in range(2):
                    S.add("pool", lambda e, t=t, s_=s_, s3=s3: e.indirect_dma_start(
                        out=XS_d, out_offset=bass.IndirectOffsetOnAxis(ap=dest_i[:, s_, t:t + 1], axis=0),
                        in_=hn[s3], in_offset=None), [b_hn[s3], b_dest], [b_XSd], dma=True)

        if cfg.get('maxph', 8) >= 6:
            ph6()
        def ph7():
            new_phase()
            wst = [carve([128, 8192], F32) for _ in range(2)]; b_wst = [Buf() for _ in range(2)]
            wg = carve([128, KC, 512], BF16); b_wg = Buf()
            wu = carve([128, KC, 512], BF16); b_wu = Buf()
            wd = carve([128, 4, D], BF16); b_wd = Buf()
            xs = [carve([128, D], BF16) for _ in range(2)]; b_xs = [Buf() for _ in range(2)]
            xsT = carve([128, KC, MB], BF16); b_xsT = Buf()
            sg_ = [carve([128, MB], F32) for _ in range(2)]; b_sg = [Buf() for _ in range(2)]
            hT = carve([128, 4, MB], BF16); b_hT = Buf()
            su_ = [carve([128, MB], F32) for _ in range(2)]; b_su = [Buf() for _ in range(2)]
            ysb = [carve([128, D], BF16) for _ in range(2)]; b_ysb = [Buf() for _ in range(2)]
            pT = pbank(0, 2, BF16); b_pT = Buf()
            pG = [pbank(2), pbank(3)]; b_pG = [Buf(), Buf()]
            pY = [pbank(4 + q) for q in range(4)]; b_pY = [Buf() for _ in range(4)]
            wsrc = [w_gate.rearrange("e (p k) n -> (e p) (k n)", k=KC), w_up.rearrange("e (p k) n -> (e p) (k n)", k=KC),
                    w_down.rearrange("e (p k) n -> (e p) (k n)", k=4)]
            wdst = [(wg, b_wg), (wu, b_wu), (wd, b_wd)]
            loads = [(b, m) for b in range(NB) for m in range(3)]

            def wdma(j):
                b, m = loads[j]
                ws = j % 2
                S.add("pool", lambda e, b=b, m=m, ws=ws: e.indirect_dma_start(
                    out=wst[ws], out_offset=None, in_=wsrc[m],
                    in_offset=bass.IndirectOffsetOnAxis(ap=widx[:, b:b + 1], axis=0)), [b_widx], [b_wst[ws]], dma=True)

            def wconv(j):
                b, m = loads[j]
                ws = j % 2
                dflat = wdst[m][0].rearrange("p a b -> p (a b)")
                A("act", lambda e, ws=ws, dflat=dflat: e.activation(out=dflat[:, 0:2560], in_=wst[ws][:, 0:2560], func=ACTF.Copy),
                  [b_wst[ws]], [wdst[m][1]])
                A("dve", lambda e, ws=ws, dflat=dflat: e.tensor_copy(out=dflat[:, 2560:5632], in_=wst[ws][:, 2560:5632]),
                  [b_wst[ws]], [wdst[m][1]])
                A("pool", lambda e, ws=ws, dflat=dflat: e.tensor_copy(out=dflat[:, 5632:8192], in_=wst[ws][:, 5632:8192]),
                  [b_wst[ws]], [wdst[m][1]])
            wdma(0)
            wdma(1)
            for b in range(NB):
                for m in range(3):
                    j = b * 3 + m
                    wconv(j)
                    if j + 2 < len(loads):
                        wdma(j + 2)
                for half in range(2):
                    DMA("sp", xs[half], XS_d[b * MB + half * 128:b * MB + (half + 1) * 128, :], [b_XSd], [b_xs[half]])
                    xv = xs[half].rearrange("p (q k) -> p k q", k=KC)
                    for k in range(KC):
                        A("pe", lambda e, k=k, xv=xv: e.transpose(out=pT[:, k * 128:(k + 1) * 128], in_=xv[:, k, :],
                                                                  identity=ident), [b_xs[half], b_cst], [b_pT])
                    A("act", lambda e, half=half: e.activation(out=xsT[:, :, half * 128:(half + 1) * 128],
                                                               in_=pT.rearrange("p (k c) -> p k c", c=128), func=ACTF.Copy),
                      [b_pT], [b_xsT])
                for fc in range(4):
                    for m, wt_ in ((0, wg), (1, wu)):
                        wv = wt_.rearrange("p k (q f) -> p k f q", f=4)
                        for k in range(KC):
                            A("pe", lambda e, k=k, m=m, wv=wv, fc=fc: e.matmul(pG[m][:, 0:MB], lhsT=wv[:, k, fc, :],
                                                                             rhs=xsT[:, k, :], start=(k == 0),
                                                                             stop=(k == KC - 1)),
                              [wdst[m][1], b_xsT], [b_pG[m]])
                    s2 = fc % 2
                    A("act", lambda e, s2=s2: e.activation(out=sg_[s2], in_=pG[0][:, 0:MB], func=ACTF.Silu),
                      [b_pG[0]], [b_sg[s2]])
                    A("act", lambda e, s2=s2: e.activation(out=su_[s2], in_=pG[1][:, 0:MB], func=ACTF.Copy),
                      [b_pG[1]], [b_su[s2]])
                    A("dve", lambda e, s2=s2, fc=fc: e.tensor_tensor(out=hT[:, fc, :], in0=su_[s2], in1=sg_[s2],
                                                                    op=ALU.mult), [b_su[s2], b_sg[s2]], [b_hT])
                for half in range(2):
                    for cg in range(4):
                        for fc in range(4):
                            A("pe", lambda e, half=half, cg=cg, fc=fc: e.matmul(
                                pY[cg], lhsT=hT[:, fc, half * 128:(half + 1) * 128], rhs=wd[:, fc, cg * 512:(cg + 1) * 512],
                                start=(fc == 0), stop=(fc == 3)), [b_hT, b_wd], [b_pY[cg]])
                        if cg % 2 == 0:
                            A("act", lambda e, half=half, cg=cg: e.activation(out=ysb[half][:, cg * 512:(cg + 1) * 512],
                                                                              in_=pY[cg], func=ACTF.Copy),
                              [b_pY[cg]], [b_ysb[half]])
                        else:
                            A("dve", lambda e, half=half, cg=cg: e.tensor_copy(out=ysb[half][:, cg * 512:(cg + 1) * 512],
                                                                               in_=pY[cg]), [b_pY[cg]], [b_ysb[half]])
                    DMA("sp", YS_d[b * MB + half * 128:b * MB + (half + 1) * 128, :], ysb[half], [b_ysb[half]], [b_YSd])

        if cfg.get('maxph', 8) >= 7:
            ph7()
        def ph8():
            new_phase()
            gfin_bc = carve([128, D], F32); b_gfin = Buf()
            DMA("sp", gfin_bc, g_fin.partition_broadcast(128).rearrange("p o d -> p (o d)"), w=[b_gfin])
            hh = [carve([128, D], F32) for _ in range(2)]; b_hh = [Buf() for _ in range(2)]
            y1 = [carve([128, D], BF16) for _ in range(2)]; b_y1 = [Buf() for _ in range(2)]
            y2 = [carve([128, D], BF16) for _ in range(2)]; b_y2 = [Buf() for _ in range(2)]
            junk = carve([128, D], BF16); b_junk = Buf()
            ssb = [carve([128, 1], F32) for _ in range(2)]; b_ss = [Buf() for _ in range(2)]
            ob = [carve([128, D], F32) for _ in range(2)]; b_ob = [Buf() for _ in range(2)]
            b_out = Buf()
            for t in range(NTQ):
                s2 = t % 2
                DMA("sp", hh[s2], H_d[t * 128:(t + 1) * 128, :], [b_Hd], [b_hh[s2]])
                for s_, (yy, byy) in enumerate(((y1, b_y1), (y2, b_y2))):
                    S.add("pool", lambda e, t=t, s_=s_, yy=yy, s2=s2: e.indirect_dma_start(
                        out=yy[s2], out_offset=None, in_=YS_d,
                        in_offset=bass.IndirectOffsetOnAxis(ap=dest_i[:, s_, t:t + 1], axis=0)),
                        [b_YSd, b_dest], [byy[s2]], dma=True)
                A("dve", lambda e, t=t, s2=s2: e.scalar_tensor_tensor(out=hh[s2], in0=y1[s2], scalar=gates[:, 0, t:t + 1],
                                                                      in1=hh[s2], op0=ALU.mult, op1=ALU.add),
                  [b_y1[s2], b_hh[s2], b_gates], [b_hh[s2]])
                A("dve", lambda e, t=t, s2=s2: e.scalar_tensor_tensor(out=hh[s2], in0=y2[s2], scalar=gates[:, 1, t:t + 1],
                                                                      in1=hh[s2], op0=ALU.mult, op1=ALU.add),
                  [b_y2[s2], b_hh[s2], b_gates], [b_hh[s2]])
                A("act", lambda e, s2=s2: e.activation(out=junk, in_=hh[s2], func=ACTF.Square, accum_out=ssb[s2]),
                  [b_hh[s2]], [b_junk, b_ss[s2]])
                A("act", lambda e, s2=s2: e.activation(out=ssb[s2], in_=ssb[s2], func=ACTF.Sqrt, scale=1.0 / D, bias=EPS),
                  [b_ss[s2]], [b_ss[s2]])
                A("dve", lambda e, s2=s2: e.reciprocal(out=ssb[s2], in_=ssb[s2]), [b_ss[s2]], [b_ss[s2]])
                A("dve", lambda e, s2=s2: e.scalar_tensor_tensor(out=ob[s2], in0=hh[s2], scalar=ssb[s2][:, 0:1], in1=gfin_bc,
                                                               op0=ALU.mult, op1=ALU.mult),
                  [b_hh[s2], b_ss[s2], b_gfin], [b_ob[s2]])
                DMA("sp", out_d[t * 128:(t + 1) * 128, :], ob[s2], [b_ob[s2]], [b_out])
        if cfg.get('maxph', 8) >= 8:
            ph8()
        S.emit(st)
    return nc


def host_inputs(cfg, x, norm_mix_g, w_in, w_pool, pool_scale, lambda_q1, lambda_k1, lambda_q2, lambda_k2, subln_g,
                w_out, norm_ffn_g, w_grp, b_grp, w_exp, b_exp, w_gate, w_up, w_down, norm_final_g):
    S_ = cfg["S"]; NG = cfg["NG"]; NE = NG * 8
    NCH = S_ // 2048; NQ = NCH * 512; NTQ = NQ // 128; NTKV = S_ // 128
    NB = -(-(2 * NQ + NE * (MB - 1)) // MB); NBMAX = -(-NQ // MB)
    f32 = np.float32
    bf = ml_dtypes.bfloat16
    x = np.asarray(x, f32)
    inv = (500000.0 ** (-np.arange(0, 16, 2, dtype=f32) / f32(16))).astype(f32)
    pos = np.arange(S_, dtype=f32)
    ang = (pos[:, None] * inv[None, :]).astype(f32)
    cos8, sin8 = np.cos(ang).astype(f32), np.sin(ang).astype(f32)
    cs = np.concatenate([cos8, cos8, -sin8, sin8], axis=1).astype(f32)
    ident = np.eye(128, dtype=f32)
    tri = (np.arange(128)[:, None] < np.arange(128)[None, :]).astype(f32)
    cst = np.stack([ident, np.ones((128, 128), f32), tri], axis=1).astype(bf)
    s_i = np.arange(128)[:, None]; t_i = np.arange(128)[None, :]

    def band(w, first):
        cntv = np.minimum(t_i + 1, w) if first else w
        main = ((s_i <= t_i) & (s_i > t_i - w)).astype(f32) / cntv - (s_i == t_i).astype(f32)
        prev = ((s_i - 128 > t_i - w)).astype(f32) / w
        return main, prev
    thr = np.broadcast_to((np.arange(NBMAX, dtype=f32) * MB)[None, None, :], (128, NE, NBMAX)).copy()
    bst = np.broadcast_to((np.arange(NB, dtype=f32) * MB)[None, :, None], (128, NB, NE)).copy()
    iop = np.arange(128, dtype=f32).reshape(128, 1)
    lams = np.concatenate([np.asarray(a, f32).reshape(-1) for a in (lambda_q1, lambda_k1, lambda_q2, lambda_k2)]).reshape(1, 256)
    w_r = np.concatenate([np.asarray(w_grp, f32)[0][:, :NG], np.zeros((D, 8 - NG), f32)] +
                         [np.asarray(w_exp, f32)[0][g] for g in range(NG)], axis=1)
    b_r = np.concatenate([np.asarray(b_grp, f32)[0][:NG], np.full((8 - NG,), -1e30, f32)] +
                         [np.asarray(b_exp, f32)[0][g] for g in range(NG)]).reshape(1, -1).astype(f32)
    common = dict(
        cst=cst, thr=thr, bst=bst, iop=iop, norm_mix_g=np.asarray(norm_mix_g, f32).reshape(1, D),
        w_in=np.asarray(w_in, f32)[0], w_pool=np.asarray(w_pool, f32)[0],
        pool_scale=np.ascontiguousarray(np.asarray(pool_scale, f32).reshape(8, 128).T), lams=lams,
        subln_g=np.asarray(subln_g, f32).reshape(128, 1), w_out=np.asarray(w_out, f32)[0],
        norm_ffn_g=np.asarray(norm_ffn_g, f32).reshape(1, D), w_r=np.ascontiguousarray(w_r), b_r=b_r,
        w_gate=np.asarray(w_gate, f32)[0][:NE], w_up=np.asarray(w_up, f32)[0][:NE], w_down=np.asarray(w_down, f32)[0][:NE],
        norm_final_g=np.asarray(norm_final_g, f32).reshape(1, D))
    maps = []
    for c in range(8):
        b, j = c // 4, c % 4
        xkv = x[b]
        xq = np.zeros((NCH, 640, D), f32)
        qpos = np.zeros((NCH, 512), np.int64)
        for i in range(NCH):
            g0 = (4 * i + j) * 512
            lo = g0 - 128
            if lo >= 0:
                xq[i] = xkv[lo:g0 + 512]
            else:
                xq[i, 128:] = xkv[g0:g0 + 512]
            qpos[i] = np.arange(g0, g0 + 512)
        cs_kv = cs.reshape(NTKV, 128, 32).transpose(1, 0, 2)
        cs_q = cs[qpos.reshape(-1)].reshape(NTQ, 128, 32).transpose(1, 0, 2)
        masks = np.zeros((128, 16, 512), f32)
        for mi in range(16):
            crel, kb4 = mi // 4, mi % 4
            if crel < j:
                masks[:, mi, :] = 1.0
            elif crel == j:
                masks[:, mi, :] = ((kb4 * 128 + np.arange(128))[:, None] <= np.arange(512)[None, :]).astype(f32)
        bands = np.zeros((128, 3, 4, 128), f32)
        for gi, w in enumerate(WINS):
            mn, pv = band(w, False)
            bands[:, 0, gi], bands[:, 1, gi] = mn, pv
            bands[:, 2, gi] = band(w, True)[0] if j == 0 else mn
        m = dict(common)
        m.update(xkv=np.ascontiguousarray(xkv), xq=xq, cs_kv=np.ascontiguousarray(cs_kv), cs_q=np.ascontiguousarray(cs_q),
                 masks=masks.astype(bf), bands=bands.astype(bf))
        maps.append(m)
    return maps


def assemble(cfg, results, key="out"):
    S_ = cfg["S"]; NCH = S_ // 2048
    out = np.zeros((2, S_, D), np.float32)
    for c in range(8):
        b, j = c // 4, c % 4
        o = results[c][key]
        for i in range(NCH):
            g0 = (4 * i + j) * 512
            out[b, g0:g0 + 512] = o[i * 512:(i + 1) * 512]
    return out


def kernel(**inputs):
    cfg = dict(CFG)
    nc = build(cfg)
    maps = host_inputs(cfg, **inputs)
    res = run_bass_kernel_spmd(nc, maps, core_ids=list(range(8)))
    return assemble(cfg, res.results)
```

```python
from contextlib import ExitStack
import math
import numpy as np
import ml_dtypes
import concourse.bass as bass
import concourse.mybir as mybir
from concourse.bass_utils import run_bass_kernel_spmd

F32 = mybir.dt.float32
BF16 = mybir.dt.bfloat16
I32 = mybir.dt.int32
ACTF = mybir.ActivationFunctionType
ALU = mybir.AluOpType
AX = mybir.AxisListType

CFG = dict(S=16384, NG=8)
D = 2048
KC = 16
NH = 8
EPS = 1e-6
LAM_INIT = 0.8 - 0.6 * math.exp(0.0)
WINS = (2, 4, 8, 16)
MB = 256


class Buf:
    __slots__ = ("w", "r")

    def __init__(self):
        self.w = None
        self.r = []


class Op:
    __slots__ = ("eng", "fn", "deps", "signal", "sig", "dma", "dsem", "dval", "ring_wait")

    def __init__(self, eng, fn, dma):
        self.eng = eng
        self.fn = fn
        self.dma = dma
        self.deps = []
        self.signal = False
        self.sig = 0
        self.dsem = None
        self.dval = 0
        self.ring_wait = None


class Sched:
    ENGS = ("pe", "act", "dve", "pool", "sp")
    RING = {"sp": 8, "pool": 8, "act": 2}

    def __init__(self, nc):
        self.nc = nc
        self.q = {e: [] for e in self.ENGS}
        self.ndma = {e: 0 for e in self.ENGS}
        self.dma_since = []

    def add(self, eng, fn, reads=(), writes=(), dma=False, extra=()):
        op = Op(eng, fn, dma)
        raw = set()
        other = set(extra)
        for b in reads:
            if b.w is not None:
                raw.add(b.w)
        for b in writes:
            if b.w is not None:
                other.add(b.w)
            other.update(b.r)
        for b in reads:
            b.r.append(op)
        for b in writes:
            b.w = op
            b.r = []
        deps = []
        for d in raw | other:
            if d is op:
                continue
            if (not d.dma) and d.eng == eng and not dma and d not in extra:
                if eng == "pe" or d not in raw:
                    continue
            deps.append(d)
        op.deps = deps
        if dma:
            n = self.ndma[eng]
            self.ndma[eng] = n + 1
            K = self.RING[eng]
            op.dsem = n % K
            op.dval = 16 * (n // K + 1)
            if n >= K:
                op.ring_wait = (n % K, 16 * (n // K))
            self.dma_since.append(op)
        self.q[eng].append(op)
        return op

    def barrier(self):
        last = [self.q[e][-1] for e in ("pe", "act", "dve", "pool") if self.q[e]]
        last = [o for o in last if o.fn is not None]
        lasts = []
        for e in ("pe", "act", "dve", "pool"):
            for o in reversed(self.q[e]):
                if o.fn is not None and not o.dma:
                    lasts.append(o)
                    break
        dm = list(self.dma_since)
        self.dma_since = []
        for e in self.ENGS:
            op = Op(e, None, False)
            op.deps = [o for o in lasts if o.eng != e] + dm
            self.q[e].append(op)

    def emit(self, stack):
        nc = self.nc
        for e in self.ENGS:
            for op in self.q[e]:
                for d in op.deps:
                    if not d.dma:
                        d.signal = True
        for e in self.ENGS:
            c = 0
            for op in self.q[e]:
                if op.signal and not op.dma:
                    c += 1
                    op.sig = c
        csem = {e: stack.enter_context(nc.semaphore("c_" + e)) for e in ("pe", "act", "dve", "pool")}
        rsem = {e: [stack.enter_context(nc.semaphore("r_%s%d" % (e, i))) for i in range(self.RING[e])]
                for e in ("sp", "pool", "act")}
        block = stack.enter_context(nc.Block())
        engmap = {"pe": block.tensor, "act": block.scalar, "dve": block.vector, "pool": block.gpsimd,
                  "sp": block.sync}

        def mk(e):
            ops = self.q[e]

            def body(eng):
                waited = {}

                def wait(sem, key, val):
                    if waited.get(key, 0) >= val:
                        return
                    waited[key] = val
                    eng.wait_ge(sem, val)

                for op in ops:
                    for d in op.deps:
                        if d.dma:
                            wait(rsem[d.eng][d.dsem], (d.eng, d.dsem), d.dval)
                        else:
                            wait(csem[d.eng], d.eng, d.sig)
                    if op.ring_wait is not None:
                        wait(rsem[e][op.ring_wait[0]], (e, op.ring_wait[0]), op.ring_wait[1])
                    if op.fn is None:
                        continue
                    ins = op.fn(eng)
                    if op.dma:
                        ins.then_inc(rsem[e][op.dsem], 16)
                    elif op.signal:
                        ins.then_inc(csem[e], 1)
                if e in rsem:
                    n = self.ndma[e]
                    K = self.RING[e]
                    for s in range(min(n, K)):
                        wait(rsem[e][s], (e, s), 16 * ((n - 1 - s) // K + 1))
            return body

        for e in self.ENGS:
            engmap[e](mk(e))


def build(cfg, debug=False):
    S_ = cfg["S"]
    NG = cfg["NG"]
    NE = NG * 8
    NCH = S_ // 2048
    NQ = NCH * 512
    NTQ = NQ // 128
    NTKV = S_ // 128
    NR = 8 + NE
    NB = -(-(2 * NQ + NE * (MB - 1)) // MB)
    NBMAX = -(-NQ // MB)

    nc = bass.Bass("TRN2", target_bir_lowering=False)
    din = lambda n, s, dt=F32: nc.dram_tensor(n, list(s), dt, kind="ExternalInput").ap()
    dscr = lambda n, s, dt: nc.dram_tensor(n, list(s), dt, kind="Internal").ap()
    xkv = din("xkv", [S_, D])
    xq = din("xq", [NCH, 640, D])
    cs_kv_d = din("cs_kv", [128, NTKV, 32])
    cs_q_d = din("cs_q", [128, NTQ, 32])
    masks_d = din("masks", [128, 16, 512], BF16)
    bands_d = din("bands", [128, 3, 4, 128], BF16)
    cst_d = din("cst", [128, 3, 128], BF16)
    thr_d = din("thr", [128, NE, NBMAX])
    bst_d = din("bst", [128, NB, NE])
    iop_d = din("iop", [128, 1])
    g_mix = din("norm_mix_g", [1, D])
    w_in = din("w_in", [D, 4096])
    w_pool = din("w_pool", [4, 256, 256])
    pool_scale = din("pool_scale", [128, 8])
    lams = din("lams", [1, 256])
    subln_g = din("subln_g", [128, 1])
    w_out = din("w_out", [D, D])
    g_ffn = din("norm_ffn_g", [1, D])
    w_r = din("w_r", [D, NR])
    b_r = din("b_r", [1, NR])
    w_gate = din("w_gate", [NE, D, 512])
    w_up = din("w_up", [NE, D, 512])
    w_down = din("w_down", [NE, 512, D])
    g_fin = din("norm_final_g", [1, D])
    out_d = nc.dram_tensor("out", [NQ, D], F32, kind="ExternalOutput").ap()
    dbg = {}
    if debug:
        dbg["mixT"] = nc.dram_tensor("dbg_mixT", [16, 128, NQ], BF16, kind="ExternalOutput").ap()
        dbg["h"] = nc.dram_tensor("dbg_h", [NQ, D], F32, kind="ExternalOutput").ap()
        dbg["lall"] = nc.dram_tensor("dbg_lall", [128, NTQ, NR], F32, kind="ExternalOutput").ap()
        dbg["dest"] = nc.dram_tensor("dbg_dest", [128, 2, NTQ], I32, kind="ExternalOutput").ap()
        dbg["gate"] = nc.dram_tensor("dbg_gate", [128, 2, NTQ], F32, kind="ExternalOutput").ap()
        dbg["bexp"] = nc.dram_tensor("dbg_bexp", [128, NB], I32, kind="ExternalOutput").ap()

    KT_d = dscr("KT_d", [NH, 128, S_], BF16)
    V_d = dscr("V_d", [NH, 128, NTKV, 128], BF16)
    QT_d = dscr("QT_d", [NH, 128, NQ], BF16)
    MIXT_d = dbg["mixT"] if debug else dscr("MIXT_d", [16, 128, NQ], BF16)
    H_d = dbg["h"] if debug else dscr("H_d", [NQ, D], F32)
    HN_d = dscr("HN_d", [NQ, D], BF16)
    XS_d = dscr("XS_d", [NB * MB, D], BF16)
    YS_d = dscr("YS_d", [NB * MB, D], BF16)

    S = Sched(nc)
    A = S.add

    def DMA(q, out, in_, r=(), w=()):
        return S.add(q, lambda e: e.dma_start(out=out, in_=in_), r, w, dma=True)

    with ExitStack() as st:
        ARENA = 104000
        arena = st.enter_context(nc.sbuf_tensor("arena", [128, ARENA], BF16))
        psum = st.enter_context(nc.psum_tensor("psum", [128, 8, 512], F32))
        state = {"off": 0, "base": 0}

        def carve(shape, dt):
            n = int(np.prod(shape[1:]))
            nb = n * (2 if dt in (F32, I32) else 1)
            nb = (nb + 15) // 16 * 16
            o = state["off"]
            assert o + nb <= ARENA, ("SBUF arena overflow", o, nb)
            state["off"] = o + nb
            v = arena[:, o:o + nb]
            if dt != BF16:
                v = v.bitcast(dt)
            v = v[:, 0:n]
            if len(shape) == 3:
                v = v.rearrange("p (a b) -> p a b", b=shape[2])
            elif len(shape) == 4:
                v = v.rearrange("p (a b c) -> p a b c", b=shape[2], c=shape[3])
            return v

        def new_phase():
            S.barrier()
            state["off"] = state["base"]

        def pbank(b, n=1, dt=F32):
            v = psum[:, b:b + n, :].rearrange("p a b -> p (a b)")
            if dt != F32:
                v = v.bitcast(dt)
            return v

        cst = carve([128, 3, 128], BF16); b_cst = Buf()
        ident, ones, tri = cst[:, 0, :], cst[:, 1, :], cst[:, 2, :]
        gmix_bc = carve([128, D], F32); b_gmix = Buf()
        lall = carve([128, NTQ, NR], F32); b_lall = Buf()
        neglam = carve([128, 1], F32); b_neglam = Buf()
        gsc = carve([128, 1], F32); b_gsc = Buf()
        iop = carve([128, 1], F32); b_iop = Buf()
        dest_i = carve([128, 2, NTQ], I32); b_dest = Buf()
        gates = carve([128, 2, NTQ], F32); b_gates = Buf()
        widx = carve([128, NB], I32); b_widx = Buf()
        small = carve([128, 8], F32)
        epsb = carve([128, 1], F32); b_epsb = Buf()
        A("dve", lambda e: e.memset(epsb, EPS), (), [b_epsb])
        DMA("sp", cst, cst_d, w=[b_cst])
        DMA("sp", gmix_bc, g_mix.partition_broadcast(128).rearrange("p o d -> p (o d)"), w=[b_gmix])
        DMA("sp", iop, iop_d, w=[b_iop])
        state["base"] = state["off"]

        lam_t = carve([128, 256], F32); b_lamt = Buf()
        lam_p = carve([128, 128], F32); b_lamp = Buf()
        lam_s = carve([128, 2], F32); b_lams = Buf()
        DMA("sp", lam_t, lams.partition_broadcast(128).rearrange("p o d -> p (o d)"), w=[b_lamt])
        lv = lam_t.rearrange("p (a b c) -> p a b c", a=2, b=2)
        A("dve", lambda e: e.tensor_tensor(out=lam_p.rearrange("p (a c) -> p a c", a=2), in0=lv[:, :, 0, :],
                                           in1=lv[:, :, 1, :], op=ALU.mult), [b_lamt], [b_lamp])
        A("dve", lambda e: e.tensor_reduce(out=lam_s, in_=lam_p.rearrange("p (a c) -> p a c", a=2), axis=AX.X,
                                           op=ALU.add), [b_lamp], [b_lams])
        A("act", lambda e: e.activation(out=lam_s, in_=lam_s, func=ACTF.Exp), [b_lams], [b_lams])
        A("dve", lambda e: e.scalar_tensor_tensor(out=neglam, in0=lam_s[:, 1:2], scalar=-LAM_INIT, in1=lam_s[:, 0:1],
                                                  op0=ALU.add, op1=ALU.subtract), [b_lams], [b_neglam])
        DMA("sp", gsc, subln_g, w=[b_gsc])
        A("dve", lambda e: e.tensor_scalar(out=gsc, in0=gsc, scalar1=1.0 - LAM_INIT, scalar2=None, op0=ALU.mult),
          [b_gsc], [b_gsc])

        def norm_transpose(src, xt, b_xt, junk, b_junk, ss, b_ss, xb, b_xb, xT, b_xT, gbc, b_gbc, pT, b_pT):
            if src is not None:
                DMA("sp", xt, src, w=[b_xt])
            A("act", lambda e: e.activation(out=junk, in_=xt, func=ACTF.Square, accum_out=ss), [b_xt], [b_junk, b_ss])
            A("act", lambda e: e.activation(out=ss, in_=ss, func=ACTF.Sqrt, scale=1.0 / D, bias=EPS), [b_ss], [b_ss])
            A("dve", lambda e: e.reciprocal(out=ss, in_=ss), [b_ss], [b_ss])
            A("dve", lambda e: e.scalar_tensor_tensor(out=xb, in0=xt, scalar=ss[:, 0:1], in1=gbc, op0=ALU.mult,
                                                      op1=ALU.mult), [b_xt, b_ss, b_gbc], [b_xb])
            if xT is None:
                return
            for k in range(KC):
                A("pe", lambda e, k=k: e.transpose(out=pT[:, k * 128:(k + 1) * 128], in_=xb[:, k * 128:(k + 1) * 128],
                                                   identity=ident), [b_xb, b_cst], [b_pT])
            A("act", lambda e: e.activation(out=xT[:, 0:8, :].rearrange("p a b -> p (a b)"), in_=pT[:, 0:1024],
                                            func=ACTF.Copy), [b_pT], [b_xT])
            A("dve", lambda e: e.tensor_copy(out=xT[:, 8:16, :].rearrange("p a b -> p (a b)"), in_=pT[:, 1024:2048]),
              [b_pT], [b_xT])

        def rope(pk, cs_t, ksb, tmp1, tmp2, rb, wb, b_tmp):
            for hb in range(2):
                pv = pk[:, hb * 512:(hb + 1) * 512].rearrange("p (g d) -> p g d", d=64)
                kv = ksb[:, hb * 512:(hb + 1) * 512].rearrange("p (g d) -> p g d", d=64)
                t1 = tmp1[:, hb * 8:(hb + 1) * 8, :]
                t2 = tmp2[:, hb * 8:(hb + 1) * 8, :]
                cosb = cs_t[:, 0:16].unsqueeze(1).to_broadcast([128, 8, 16])
                s0 = cs_t[:, 16:24].unsqueeze(1).to_broadcast([128, 8, 8])
                s1 = cs_t[:, 24:32].unsqueeze(1).to_broadcast([128, 8, 8])
                import os as _os
                RV = 3
                A("act", lambda e, pv=pv, kv=kv: e.activation(out=kv[:, :, 16:64], in_=pv[:, :, 16:64], func=ACTF.Copy), rb, wb)
                if RV == 1:
                    continue
                if RV == 3:
                    t3 = tmp2[:, hb * 8:(hb + 1) * 8, :]
                    A("act", lambda e, pv=pv, t3=t3: e.activation(out=t3, in_=pv[:, :, 0:16], func=ACTF.Copy), rb, [b_tmp])
                    A("dve", lambda e, t1=t1, t3=t3, cosb=cosb: e.tensor_tensor(out=t1, in0=t3, in1=cosb, op=ALU.mult), rb + [b_tmp], [b_tmp])
                    A("dve", lambda e, kv=kv, t3=t3, s0=s0: e.tensor_tensor(out=kv[:, :, 0:8], in0=t3[:, :, 8:16], in1=s0, op=ALU.mult), rb + [b_tmp], wb)
                    A("dve", lambda e, kv=kv, t3=t3, s1=s1: e.tensor_tensor(out=kv[:, :, 8:16], in0=t3[:, :, 0:8], in1=s1, op=ALU.mult), rb + [b_tmp], wb)
                    A("dve", lambda e, kv=kv, t1=t1: e.tensor_tensor(out=kv[:, :, 0:16], in0=kv[:, :, 0:16], in1=t1, op=ALU.add), [b_tmp] + wb, wb)
                    continue
                if RV == 2:
                    A("dve", lambda e, pv=pv, t1=t1, t2=t2: e.tensor_tensor(out=t1, in0=pv[:, :, 0:16], in1=t2, op=ALU.mult), rb, [b_tmp])
                    A("dve", lambda e, kv=kv, t1=t1, t2=t2: e.tensor_tensor(out=kv[:, :, 0:16], in0=t1, in1=t2, op=ALU.add), [b_tmp], wb)
                    continue
                A("dve", lambda e, pv=pv, t1=t1, cosb=cosb: e.tensor_tensor(out=t1, in0=pv[:, :, 0:16], in1=cosb, op=ALU.mult), rb, [b_tmp])
                A("dve", lambda e, pv=pv, t2=t2, s0=s0: e.tensor_tensor(out=t2[:, :, 0:8], in0=pv[:, :, 8:16], in1=s0, op=ALU.mult), rb, [b_tmp])
                A("dve", lambda e, pv=pv, t2=t2, s1=s1: e.tensor_tensor(out=t2[:, :, 8:16], in0=pv[:, :, 0:8], in1=s1, op=ALU.mult), rb, [b_tmp])
                A("dve", lambda e, kv=kv, t1=t1, t2=t2: e.tensor_tensor(out=kv[:, :, 0:16], in0=t1, in1=t2, op=ALU.add), [b_tmp], wb)

        b_KTd = Buf(); b_Vd = Buf(); b_QTd = Buf(); b_MIXd = Buf(); b_Hd = Buf(); b_HNd = Buf(); b_XSd = Buf(); b_YSd = Buf()
        def ph1():
            wkv = carve([128, KC, 2048], BF16); b_wkv = [Buf() for _ in range(KC)]
            for k in range(KC):
                S.add("pool", lambda e, k=k: e.dma_start(out=wkv[:, k, :], in_=w_in[k * 128:(k + 1) * 128, 2048:4096]),
                      (), [b_wkv[k]], dma=True)
            cs_kv = carve([128, NTKV, 32], F32); b_cskv = Buf()
            DMA("sp", cs_kv, cs_kv_d, w=[b_cskv])
            xt = [carve([128, D], F32) for _ in range(3)]; b_xt = [Buf() for _ in range(3)]
            junk = carve([128, D], BF16); b_junk = Buf()
            ssb = [carve([128, 1], F32) for _ in range(2)]; b_ss = [Buf() for _ in range(2)]
            xb = [carve([128, D], BF16) for _ in range(2)]; b_xb = [Buf() for _ in range(2)]
            xT = [carve([128, KC, 128], BF16) for _ in range(2)]; b_xT = [Buf() for _ in range(2)]
            ksb = [carve([128, 1024], BF16) for _ in range(2)]; b_ksb = [Buf() for _ in range(2)]
            vsb = [carve([128, 4, 1024], BF16) for _ in range(2)]; b_vsb = [Buf() for _ in range(2)]
            kTs = [carve([128, NH, 512], BF16) for _ in range(2)]; b_kTs = [Buf() for _ in range(2)]
            tmp1 = carve([128, 16, 16], F32); tmp2 = carve([128, 16, 16], F32); b_tmp = Buf()
            pT = pbank(0, 2, BF16); b_pT = Buf()
            pK = pbank(2, 2); b_pK = Buf()
            pV = pbank(4, 2); b_pV = Buf()
            pKT = pbank(6, 1, BF16); b_pKT = Buf()
            def loadA(t):
                DMA("sp", xt[t % 3], xkv[t * 128:(t + 1) * 128, :], w=[b_xt[t % 3]])

            def frontA(t):
                s2 = t % 2
                norm_transpose(None, xt[t % 3], b_xt[t % 3], junk, b_junk, ssb[s2], b_ss[s2], xb[s2],
                               b_xb[s2], xT[s2], b_xT[s2], gmix_bc, b_gmix, pT, b_pT)
            loadA(0)
            loadA(1)
            frontA(0)
            for t in range(NTKV):
                s2 = t % 2
                g4 = (t // 4) % 2
                if t + 2 < NTKV:
                    loadA(t + 2)
                if t + 1 < NTKV:
                    frontA(t + 1)
                LV = cfg.get("lv", 9)
                if LV < 1:
                    continue
                for cg in range(4):
                    dst, bd = (pK, b_pK) if cg < 2 else (pV, b_pV)
                    for k in range(KC):
                        import os as _os
                        _N = int(_os.environ.get("EXPN", 512))
                        if _os.environ.get("WSRC"):
                            A("pe", lambda e, k=k, cg=cg, dst=dst, s2=s2: e.matmul(
                                dst[:, (cg % 2) * 512:(cg % 2) * 512 + _N], lhsT=xT[s2][:, k, :],
                                rhs=xb[s2][:, 0:_N], start=(k == 0), stop=(k == KC - 1)),
                              [b_xT[s2], b_xb[s2]], [bd])
                        else:
                            A("pe", lambda e, k=k, cg=cg, dst=dst, s2=s2: e.matmul(
                                dst[:, (cg % 2) * 512:(cg % 2) * 512 + _N], lhsT=xT[s2][:, k, :],
                                rhs=wkv[:, k, cg * 512:cg * 512 + _N], start=(k == 0), stop=(k == KC - 1)),
                              [b_xT[s2], b_wkv[k]], [bd])
                if LV < 2:
                    continue
                rope(pK, cs_kv[:, t, :], ksb[s2], tmp1, tmp2, [b_pK, b_cskv], [b_ksb[s2]], b_tmp)
                if LV < 3:
                    continue
                A("act", lambda e, t=t, g4=g4: e.activation(out=vsb[g4][:, t % 4, :], in_=pV, func=ACTF.Copy),
                  [b_pV], [b_vsb[g4]])
                if LV < 4:
                    continue
                for h in range(NH):
                    A("pe", lambda e, h=h, s2=s2: e.transpose(out=pKT[:, h * 128:(h + 1) * 128],
                                                              in_=ksb[s2][:, h * 128:(h + 1) * 128], identity=ident),
                      [b_ksb[s2], b_cst], [b_pKT])
                A("dve", lambda e, t=t, g4=g4: e.tensor_copy(out=kTs[g4][:, :, (t % 4) * 128:(t % 4 + 1) * 128],
                                                             in_=pKT.rearrange("p (h c) -> p h c", c=128)),
                  [b_pKT], [b_kTs[g4]])
                if t % 4 == 3 and LV >= 5:
                    t0 = (t // 4) * 4
                    DMA("pool", KT_d[:, :, t0 * 128:(t0 + 4) * 128].rearrange("h p c -> p h c"), kTs[g4], [b_kTs[g4]], [b_KTd])
                    for tt in range(4):
                        DMA("pool", V_d[:, :, t0 + tt, :].rearrange("h p e -> p h e"),
                            vsb[g4][:, tt, :].rearrange("p (h e) -> p h e", e=128), [b_vsb[g4]], [b_Vd])

        if cfg.get('maxph', 8) >= 1:
            ph1()
        def ph2():
            new_phase()
            wq = carve([128, KC, 2048], BF16); b_wq = [Buf() for _ in range(KC)]
            for k in range(KC):
                S.add("pool", lambda e, k=k: e.dma_start(out=wq[:, k, :], in_=w_in[k * 128:(k + 1) * 128, 0:2048]),
                      (), [b_wq[k]], dma=True)
            wp = carve([128, 8, 256], BF16); b_wp = Buf()
            S.add("pool", lambda e: e.dma_start(out=wp, in_=w_pool.rearrange("g (cc p) d -> p (g cc) d", p=128)),
                  (), [b_wp], dma=True)
            psc = carve([128, 8], F32); b_psc = Buf()
            DMA("sp", psc, pool_scale, w=[b_psc])
            bands = carve([128, 3, 4, 128], BF16); b_bands = Buf()
            DMA("sp", bands, bands_d, w=[b_bands])
            cs_q = carve([128, NTQ, 32], F32); b_csq = Buf()
            DMA("sp", cs_q, cs_q_d, w=[b_csq])
            xt = [carve([128, D], F32) for _ in range(2)]; b_xt = [Buf() for _ in range(2)]
            junk = carve([128, D], BF16); b_junk = Buf()
            ssb = [carve([128, 1], F32) for _ in range(2)]; b_ss = [Buf() for _ in range(2)]
            xb = [carve([128, D], BF16) for _ in range(2)]; b_xb = [Buf() for _ in range(2)]
            xT = [carve([128, KC, 128], BF16) for _ in range(2)]; b_xT = [Buf() for _ in range(2)]
            qsb = [carve([128, 1024], BF16) for _ in range(2)]; b_qsb = [Buf() for _ in range(2)]
            pin = [carve([128, 1024], BF16) for _ in range(3)]; b_pin = [Buf() for _ in range(3)]
            qTs = [carve([128, NH, 128], BF16) for _ in range(2)]; b_qTs = [Buf() for _ in range(2)]
            pldT = [carve([128, 8, 128], BF16) for _ in range(2)]; b_pldT = [Buf() for _ in range(2)]
            mxT = [carve([128, 8, 128], BF16) for _ in range(2)]; b_mxT = [Buf() for _ in range(2)]
            tmp1 = carve([128, 16, 16], F32); tmp2 = carve([128, 16, 16], F32); b_tmp = Buf()
            pT = pbank(0, 2, BF16); b_pT = Buf()
            pP = pbank(2, 2); b_pP = Buf()
            pQ = pbank(4, 2); b_pQ = Buf()
            pQT = pbank(6, 1, BF16); b_pQT = Buf()
            pM = pbank(7, 1); b_pM = Buf()
            tilesB = [(i, r) for i in range(NCH) for r in range(5)]

            def frontB(cn):
                i, r = tilesB[cn]
                s2 = cn % 2
                norm_transpose(xq[i, r * 128:(r + 1) * 128, :], xt[s2], b_xt[s2], junk, b_junk, ssb[s2], b_ss[s2],
                               xb[s2], b_xb[s2], xT[s2], b_xT[s2], gmix_bc, b_gmix, pT, b_pT)
            frontB(0)
            cnt = 0
            for i in range(NCH):
                for r in range(5):
                    s2 = cnt % 2
                    s3 = cnt % 3
                    sp3 = (cnt - 1) % 3
                    cnt += 1
                    if cnt < len(tilesB):
                        frontB(cnt)
                    for cg in range(2 if r == 0 else 4):
                        dst, bd = (pP, b_pP) if cg < 2 else (pQ, b_pQ)
                        for k in range(KC):
                            A("pe", lambda e, k=k, cg=cg, dst=dst, s2=s2: e.matmul(
                                dst[:, (cg % 2) * 512:(cg % 2 + 1) * 512], lhsT=xT[s2][:, k, :],
                                rhs=wq[:, k, cg * 512:(cg + 1) * 512], start=(k == 0), stop=(k == KC - 1)),
                              [b_xT[s2], b_wq[k]], [bd])
                    A("act", lambda e, s3=s3: e.activation(out=pin[s3], in_=pP, func=ACTF.Copy), [b_pP], [b_pin[s3]])
                    if r == 0:
                        continue
                    tq = i * 4 + (r - 1)
                    rope(pQ, cs_q[:, tq, :], qsb[s2], tmp1, tmp2, [b_pQ, b_csq], [b_qsb[s2]], b_tmp)
                    for h in range(NH):
                        A("pe", lambda e, h=h, s2=s2: e.transpose(out=pQT[:, h * 128:(h + 1) * 128],
                                                                  in_=qsb[s2][:, h * 128:(h + 1) * 128], identity=ident),
                          [b_qsb[s2], b_cst], [b_pQT])
                    A("dve", lambda e, s2=s2: e.tensor_copy(out=qTs[s2].rearrange("p h c -> p (h c)"), in_=pQT),
                      [b_pQT], [b_qTs[s2]])
                    DMA("pool", QT_d[:, :, tq * 128:(tq + 1) * 128].rearrange("h p c -> p h c"), qTs[s2], [b_qTs[s2]], [b_QTd])
                    bsel = 2 if (i == 0 and r == 1) else 0
                    for half in range(2):
                        for u in range(4):
                            gc = half * 4 + u
                            g = gc // 2
                            o_ = pM[:, u * 128:(u + 1) * 128]
                            A("pe", lambda e, gc=gc, g=g, o_=o_, s3=s3, bsel=bsel: e.matmul(
                                o_, lhsT=pin[s3][:, gc * 128:(gc + 1) * 128], rhs=bands[:, bsel, g, :], start=True, stop=False),
                              [b_pin[s3], b_bands], [b_pM])
                            A("pe", lambda e, gc=gc, g=g, o_=o_, sp3=sp3: e.matmul(
                                o_, lhsT=pin[sp3][:, gc * 128:(gc + 1) * 128], rhs=bands[:, 1, g, :], start=False, stop=True),
                              [b_pin[sp3], b_bands], [b_pM])
                        A("dve", lambda e, half=half, s2=s2: e.tensor_copy(
                            out=pldT[s2][:, half * 4:(half + 1) * 4, :].rearrange("p a b -> p (a b)"), in_=pM),
                          [b_pM], [b_pldT[s2]])
                    for half in range(2):
                        for u in range(4):
                            gd = half * 4 + u
                            g, dd = gd // 2, gd % 2
                            o_ = pM[:, u * 128:(u + 1) * 128]
                            for cc in range(2):
                                A("pe", lambda e, g=g, dd=dd, cc=cc, o_=o_, s2=s2: e.matmul(
                                    o_, lhsT=wp[:, g * 2 + cc, dd * 128:(dd + 1) * 128], rhs=pldT[s2][:, g * 2 + cc, :],
                                    start=(cc == 0), stop=(cc == 1)), [b_wp, b_pldT[s2]], [b_pM])
                        for u in range(4):
                            gd = half * 4 + u
                            A("act", lambda e, gd=gd, u=u, s2=s2: e.activation(
                                out=mxT[s2][:, gd, :], in_=pM[:, u * 128:(u + 1) * 128], func=ACTF.Identity,
                                scale=psc[:, gd:gd + 1]), [b_pM, b_psc], [b_mxT[s2]])
                    DMA("pool", MIXT_d[0:8, :, tq * 128:(tq + 1) * 128].rearrange("f p c -> p f c"), mxT[s2], [b_mxT[s2]], [b_MIXd])

        if cfg.get('maxph', 8) >= 2:
            ph2()
        def ph3():
            new_phase()
            NSEG = 4
            SEG = S_ // NSEG
            kt_sb = carve([128, S_], BF16); b_kt = [Buf() for _ in range(NSEG)]
            v_sb = carve([128, NTKV, 128], BF16); b_v = [Buf() for _ in range(NSEG)]
            qt_sb = [carve([128, NQ], BF16) for _ in range(2)]; b_qt = [Buf() for _ in range(2)]
            msk = carve([128, 16, 512], BF16); b_msk = Buf()
            DMA("sp", msk, masks_d, w=[b_msk])
            NPT = 5
            pt = [[carve([128, 512], BF16) for _ in range(NPT)] for _ in range(2)]
            b_pt = [[Buf() for _ in range(NPT)] for _ in range(2)]
            sacc = [[carve([128, 512], BF16) for _ in range(2)] for _ in range(2)]
            b_sacc = [[Buf() for _ in range(2)] for _ in range(2)]
            rr = [carve([128, 512], F32) for _ in range(2)]; b_rr = [Buf() for _ in range(2)]
            oo = [carve([128, 512], F32) for _ in range(2)]; b_oo = [Buf() for _ in range(2)]
            sq = carve([128, 512], BF16); b_sq = Buf()
            rs = carve([128, 512], F32); b_rs = Buf()
            at = [carve([128, 512], BF16) for _ in range(2)]; b_at = [Buf() for _ in range(2)]
            pS = [[pbank(c * 2 + s) for s in range(2)] for c in range(2)]
            b_pS = [[Buf() for _ in range(2)] for _ in range(2)]
            pO = [pbank(4 + c) for c in range(2)]; b_pO = [Buf() for _ in range(2)]
            pL = [pbank(6 + c) for c in range(2)]; b_pL = [Buf() for _ in range(2)]
            st3 = {"ecnt": 0}

            def head_load(h):
                hs = h % 2
                for sg in range(NSEG):
                    DMA("sp", kt_sb[:, sg * SEG:(sg + 1) * SEG], KT_d[h, :, sg * SEG:(sg + 1) * SEG], [b_KTd], [b_kt[sg]])
                    DMA("sp", v_sb[:, sg * SEG // 128:(sg + 1) * SEG // 128, :],
                        V_d[h, :, sg * SEG // 128:(sg + 1) * SEG // 128, :], [b_Vd], [b_v[sg]])
                DMA("sp", qt_sb[hs], QT_d[h], [b_QTd], [b_qt[hs]])

            def qk(n, u):
                h, i, kb, nkb = u
                hs = h % 2
                sg = (kb * 128) // SEG
                sl = n % 2
                for c in range(2):
                    A("pe", lambda e, c=c, kb=kb, i=i, sl=sl, hs=hs: e.matmul(
                        pS[c][sl], lhsT=kt_sb[c * 64:(c + 1) * 64, kb * 128:(kb + 1) * 128],
                        rhs=qt_sb[hs][c * 64:(c + 1) * 64, i * 512:(i + 1) * 512], start=True, stop=True),
                      [b_kt[sg], b_qt[hs]], [b_pS[c][sl]])

            def ex(n, u):
                h, i, kb, nkb = u
                sl = n % 2
                ps3 = n % NPT
                for c in range(2):
                    A("act", lambda e, c=c, sl=sl, ps3=ps3: e.activation(out=pt[c][ps3], in_=pS[c][sl], func=ACTF.Exp,
                                                                       scale=0.125),
                      [b_pS[c][sl]], [b_pt[c][ps3]])
                if kb >= nkb - 16:
                    mi = kb - (nkb - 16)
                    for c in range(2):
                        A("pool" if c == 0 else "dve", lambda e, c=c, ps3=ps3, mi=mi: e.tensor_tensor(
                            out=pt[c][ps3], in0=pt[c][ps3], in1=msk[:, mi, :], op=ALU.mult),
                          [b_pt[c][ps3], b_msk], [b_pt[c][ps3]])

            def pv(n, u):
                h, i, kb, nkb = u
                sg = (kb * 128) // SEG
                ps3 = n % NPT
                pp3 = (n - 1) % NPT
                g2 = (kb // 4) % 2
                for c in range(2):
                    A("pe", lambda e, c=c, kb=kb, ps3=ps3, nkb=nkb: e.matmul(
                        pO[c], lhsT=v_sb[:, kb, :], rhs=pt[c][ps3], start=(kb == 0), stop=(kb == nkb - 1)),
                      [b_v[sg], b_pt[c][ps3]], [b_pO[c]])
                    if kb % 4 == 1:
                        A("dve", lambda e, c=c, ps3=ps3, pp3=pp3, g2=g2: e.tensor_tensor(
                            out=sacc[c][g2], in0=pt[c][pp3], in1=pt[c][ps3], op=ALU.add),
                          [b_pt[c][pp3], b_pt[c][ps3]], [b_sacc[c][g2]])
                    elif kb % 4 >= 2:
                        A("dve", lambda e, c=c, ps3=ps3, g2=g2: e.tensor_tensor(
                            out=sacc[c][g2], in0=sacc[c][g2], in1=pt[c][ps3], op=ALU.add),
                          [b_sacc[c][g2], b_pt[c][ps3]], [b_sacc[c][g2]])
                if kb % 4 == 0 and kb > 0:
                    g2p = ((kb - 1) // 4) % 2
                    for c in range(2):
                        A("pe", lambda e, c=c, kb=kb, g2p=g2p: e.matmul(
                            pL[c], lhsT=ones, rhs=sacc[c][g2p], start=(kb == 4), stop=False),
                          [b_cst, b_sacc[c][g2p]], [b_pL[c]])
                if kb == nkb - 1:
                    for c in range(2):
                        A("pe", lambda e, c=c, kb=kb, g2=g2, nkb=nkb: e.matmul(
                            pL[c], lhsT=ones, rhs=sacc[c][g2], start=(nkb == 4), stop=True),
                          [b_cst, b_sacc[c][g2]], [b_pL[c]])

            def epi_a():
                for c in range(2):
                    A("act", lambda e, c=c: e.activation(out=rr[c], in_=pL[c], func=ACTF.Copy), [b_pL[c]], [b_rr[c]])
                    A("act", lambda e, c=c: e.activation(out=oo[c], in_=pO[c], func=ACTF.Copy), [b_pO[c]], [b_oo[c]])
                for c in range(2):
                    A("dve", lambda e, c=c: e.reciprocal(out=rr[c], in_=rr[c]), [b_rr[c]], [b_rr[c]])
                    A("dve", lambda e, c=c: e.tensor_tensor(out=oo[c], in0=oo[c], in1=rr[c], op=ALU.mult),
                      [b_oo[c], b_rr[c]], [b_oo[c]])
                A("dve", lambda e: e.scalar_tensor_tensor(out=oo[0], in0=oo[1], scalar=neglam[:, 0:1], in1=oo[0],
                                                          op0=ALU.mult, op1=ALU.add), [b_oo[0], b_oo[1], b_neglam], [b_oo[0]])
                A("dve", lambda e: e.tensor_tensor(out=sq, in0=oo[0], in1=oo[0], op=ALU.mult), [b_oo[0]], [b_sq])

            def epi_b(h, i, n):
                es = st3["ecnt"] % 2
                st3["ecnt"] += 1
                sl = (n + 1) % 2
                A("pe", lambda e, sl=sl: e.matmul(pS[0][sl], lhsT=ones, rhs=sq, start=True, stop=True),
                  [b_cst, b_sq], [b_pS[0][sl]])
                A("act", lambda e, sl=sl: e.activation(out=rs, in_=pS[0][sl], func=ACTF.Ln, scale=1.0 / 128, bias=epsb[:, 0:1]),
                  [b_pS[0][sl], b_epsb], [b_rs])
                A("act", lambda e: e.activation(out=rs, in_=rs, func=ACTF.Exp, scale=-0.5), [b_rs], [b_rs])
                A("dve", lambda e, es=es: e.scalar_tensor_tensor(out=at[es], in0=oo[0], scalar=gsc[:, 0:1], in1=rs,
                                                               op0=ALU.mult, op1=ALU.mult),
                  [b_oo[0], b_rs, b_gsc], [b_at[es]])
                DMA("sp", MIXT_d[8 + h, :, i * 512:(i + 1) * 512], at[es], [b_at[es]], [b_MIXd])

            gn = 0
            for h in range(NH):
                units = [(h, i, kb, 16 * (i + 1)) for i in range(NCH) for kb in range(16 * (i + 1))]
                N = len(units)
                head_load(h)
                pend = []
                for n in range(N + 2):
                    if n < N:
                        qk(gn + n, units[n])
                        ex(gn + n, units[n])
                    m = n - 2
                    if m >= 0:
                        pv(gn + m, units[m])
                        _, i_, kb_, nkb_ = units[m]
                        if kb_ == nkb_ - 1:
                            epi_a()
                            pend.append((n + 4, h, i_))
                    while pend and (pend[0][0] <= n or n == N + 1):
                        _, hh_, ii_ = pend.pop(0)
                        epi_b(hh_, ii_, gn + n)
                gn += N

        if cfg.get('maxph', 8) >= 3:
            ph3()
        def ph4():
            new_phase()
            wo = carve([128, KC, D], BF16); b_wo = [Buf() for _ in range(KC)]
            for k in range(KC):
                S.add("pool", lambda e, k=k: e.dma_start(out=wo[:, k, :], in_=w_out[k * 128:(k + 1) * 128, :]),
                      (), [b_wo[k]], dma=True)
            wr = carve([128, KC, NR], BF16); b_wr = Buf()
            S.add("pool", lambda e: e.dma_start(out=wr, in_=w_r.rearrange("(k p) n -> p k n", p=128)), (), [b_wr], dma=True)
            br = carve([128, NR], F32); b_br = Buf()
            DMA("sp", br, b_r.partition_broadcast(128).rearrange("p o d -> p (o d)"), w=[b_br])
            gffn_bc = carve([128, D], F32); b_gffn = Buf()
            DMA("sp", gffn_bc, g_ffn.partition_broadcast(128).rearrange("p o d -> p (o d)"), w=[b_gffn])
            mT = [carve([128, KC, 128], BF16) for _ in range(2)]; b_mT = [Buf() for _ in range(2)]
            xt = [carve([128, D], F32) for _ in range(2)]; b_xt = [Buf() for _ in range(2)]
            hsb = [carve([128, D], F32) for _ in range(2)]; b_hsb = [Buf() for _ in range(2)]
            junk = carve([128, D], BF16); b_junk = Buf()
            ssb = [carve([128, 1], F32) for _ in range(2)]; b_ss = [Buf() for _ in range(2)]
            hn = [carve([128, D], BF16) for _ in range(2)]; b_hn = [Buf() for _ in range(2)]
            hnT = [carve([128, KC, 128], BF16) for _ in range(2)]; b_hnT = [Buf() for _ in range(2)]
            pH = [pbank(b) for b in range(4)]; b_pH = [Buf() for _ in range(4)]
            pT = pbank(4, 2, BF16); b_pT = Buf()
            pR = pbank(6); b_pR = Buf()
            def frontD(t):
                s2 = t % 2
                i, r = t // 4, t % 4
                DMA("sp", mT[s2], MIXT_d[:, :, t * 128:(t + 1) * 128].rearrange("f p c -> p f c"), [b_MIXd], [b_mT[s2]])
                DMA("sp", xt[s2], xq[i, (r + 1) * 128:(r + 2) * 128, :], w=[b_xt[s2]])
                for cg in range(4):
                    for k in range(KC):
                        A("pe", lambda e, k=k, cg=cg, s2=s2: e.matmul(pH[cg], lhsT=mT[s2][:, k, :],
                                                                      rhs=wo[:, k, cg * 512:(cg + 1) * 512],
                                                                      start=(k == 0), stop=(k == KC - 1)),
                          [b_mT[s2], b_wo[k]], [b_pH[cg]])
                    A("act", lambda e, cg=cg, s2=s2: e.activation(out=hsb[s2][:, cg * 512:(cg + 1) * 512], in_=pH[cg],
                                                                  func=ACTF.Copy), [b_pH[cg]], [b_hsb[s2]])
                    A("dve", lambda e, cg=cg, s2=s2: e.tensor_tensor(out=hsb[s2][:, cg * 512:(cg + 1) * 512],
                                                                     in0=hsb[s2][:, cg * 512:(cg + 1) * 512],
                                                                     in1=xt[s2][:, cg * 512:(cg + 1) * 512], op=ALU.add),
                      [b_hsb[s2], b_xt[s2]], [b_hsb[s2]])
                DMA("sp", H_d[t * 128:(t + 1) * 128, :], hsb[s2], [b_hsb[s2]], [b_Hd])
                A("act", lambda e, s2=s2: e.activation(out=junk, in_=hsb[s2], func=ACTF.Square, accum_out=ssb[s2]),
                  [b_hsb[s2]], [b_junk, b_ss[s2]])
                A("act", lambda e, s2=s2: e.activation(out=ssb[s2], in_=ssb[s2], func=ACTF.Sqrt, scale=1.0 / D, bias=EPS),
                  [b_ss[s2]], [b_ss[s2]])
                A("dve", lambda e, s2=s2: e.reciprocal(out=ssb[s2], in_=ssb[s2]), [b_ss[s2]], [b_ss[s2]])
                A("dve", lambda e, s2=s2: e.scalar_tensor_tensor(out=hn[s2], in0=hsb[s2], scalar=ssb[s2][:, 0:1], in1=gffn_bc,
                                                               op0=ALU.mult, op1=ALU.mult),
                  [b_hsb[s2], b_ss[s2], b_gffn], [b_hn[s2]])
                DMA("sp", HN_d[t * 128:(t + 1) * 128, :], hn[s2], [b_hn[s2]], [b_HNd])

            def backD(t):
                s2 = t % 2
                for k in range(KC):
                    A("pe", lambda e, k=k, s2=s2: e.transpose(out=pT[:, k * 128:(k + 1) * 128],
                                                              in_=hn[s2][:, k * 128:(k + 1) * 128], identity=ident),
                      [b_hn[s2], b_cst], [b_pT])
                A("act", lambda e, s2=s2: e.activation(out=hnT[s2].rearrange("p a b -> p (a b)"), in_=pT, func=ACTF.Copy),
                  [b_pT], [b_hnT[s2]])
                for k in range(KC):
                    A("pe", lambda e, k=k, s2=s2: e.matmul(pR[:, 0:NR], lhsT=hnT[s2][:, k, :], rhs=wr[:, k, :],
                                                           start=(k == 0), stop=(k == KC - 1)), [b_hnT[s2], b_wr], [b_pR])
                A("act", lambda e, t=t: e.activation(out=lall[:, t, :], in_=pR[:, 0:NR], func=ACTF.Copy), [b_pR], [b_lall])
                A("dve", lambda e, t=t: e.tensor_tensor(out=lall[:, t, :], in0=lall[:, t, :], in1=br, op=ALU.add),
                  [b_lall, b_br], [b_lall])


            frontD(0)
            for t in range(NTQ):
                if t + 1 < NTQ:
                    frontD(t + 1)
                backD(t)

        if cfg.get('maxph', 8) >= 4:
            ph4()
        def ph5():
            new_phase()
            T_ = NTQ
            V = lambda shape, dt=F32: carve(shape, dt)
            mg = V([128, T_]); bm = Buf()
            maskg = V([128, T_, 8])
            eg = V([128, T_, 8])
            sume = V([128, T_])
            prod = V([128, T_, 8, 8])
            sel = V([128, T_, 8])
            top8 = V([128, T_, 8])
            m1 = V([128, T_, 8]); m2 = V([128, T_, 8])
            dm = V([128, T_]); g1 = V([128, T_])
            E = [V([128, T_, NE], BF16) for _ in range(2)]
            E32 = [V([128, T_, NE]) for _ in range(2)]
            Mt = V([128, T_, NE], BF16)
            Mc = V([128, T_ + 1, NE], BF16)
            rank = V([128, T_, NE])
            cnts = V([128, NE]); nblk = V([128, NE]); pend = V([128, NE]); pst = V([128, NE])
            thr = V([128, NE, NBMAX]); cmp1 = V([128, NE, NBMAX])
            bst = V([128, NB, NE]); cmp2 = V([128, NB, NE]); bexp = V([128, NB])
            onesf = V([128, NE])
            dst_f = V([128, 2, T_])
            bE = Buf()
            DMA("sp", thr, thr_d, w=[bE])
            DMA("sp", bst, bst_d, w=[bE])
            lg = lall[:, :, 0:8]
            le = lall[:, :, 8:NR]
            RW = ([b_lall, bE], [bE])

            def dv(fn, r=RW[0], w=RW[1], eng="dve"):
                A(eng, fn, r, w)
            dv(lambda e: e.tensor_reduce(out=mg, in_=lg, axis=AX.X, op=ALU.max))
            dv(lambda e: e.tensor_tensor(out=maskg, in0=lg, in1=mg.unsqueeze(2).to_broadcast([128, T_, 8]), op=ALU.is_equal))
            dv(lambda e: e.tensor_tensor(out=eg, in0=lg, in1=mg.unsqueeze(2).to_broadcast([128, T_, 8]), op=ALU.subtract))
            dv(lambda e: e.activation(out=eg, in_=eg, func=ACTF.Exp), eng="act")
            dv(lambda e: e.tensor_reduce(out=sume, in_=eg, axis=AX.X, op=ALU.add))
            dv(lambda e: e.reciprocal(out=sume, in_=sume))
            if NG == 8:
                lev = le.rearrange("p t (g i) -> p t g i", i=8)
            else:
                lev = le.rearrange("p t (g i) -> p t g i", i=8)
            for g in range(NG):
                dv(lambda e, g=g: e.tensor_tensor(out=prod[:, :, g, :], in0=lev[:, :, g, :],
                                                  in1=maskg[:, :, g:g + 1].to_broadcast([128, T_, 8]), op=ALU.mult))
            dv(lambda e: e.tensor_copy(out=sel, in_=prod[:, :, 0, :]))
            for g in range(1, NG):
                dv(lambda e, g=g: e.tensor_tensor(out=sel, in0=sel, in1=prod[:, :, g, :], op=ALU.add))
            for t in range(T_):
                dv(lambda e, t=t: e.max(out=top8[:, t, :], in_=sel[:, t, :]))
            dv(lambda e: e.tensor_tensor(out=m1, in0=sel, in1=top8[:, :, 0:1].to_broadcast([128, T_, 8]), op=ALU.is_equal))
            dv(lambda e: e.tensor_tensor(out=m2, in0=sel, in1=top8[:, :, 1:2].to_broadcast([128, T_, 8]), op=ALU.is_equal))
            dv(lambda e: e.tensor_tensor(out=dm, in0=top8[:, :, 1], in1=top8[:, :, 0], op=ALU.subtract))
            dv(lambda e: e.activation(out=dm, in_=dm, func=ACTF.Exp), eng="act")
            dv(lambda e: e.tensor_scalar(out=dm, in0=dm, scalar1=1.0, scalar2=None, op0=ALU.add))
            dv(lambda e: e.reciprocal(out=g1, in_=dm))
            dv(lambda e: e.tensor_tensor(out=gates[:, 0, :], in0=g1, in1=sume, op=ALU.mult), w=[bE, b_gates])
            dv(lambda e: e.tensor_tensor(out=gates[:, 1, :], in0=sume, in1=gates[:, 0, :], op=ALU.subtract), w=[bE, b_gates])
            for s_, mm in ((0, m1), (1, m2)):
                for g in range(NG):
                    dv(lambda e, s_=s_, mm=mm, g=g: e.tensor_tensor(
                        out=E32[s_][:, :, g * 8:(g + 1) * 8], in0=mm,
                        in1=maskg[:, :, g:g + 1].to_broadcast([128, T_, 8]), op=ALU.mult))
                dv(lambda e, s_=s_: e.tensor_copy(out=E[s_], in_=E32[s_]))
            dv(lambda e: e.tensor_tensor(out=Mt, in0=E[0], in1=E[1], op=ALU.add))
            dv(lambda e: e.memset(Mc[:, 0, :], 0.0))
            for t in range(T_):
                dv(lambda e, t=t: e.tensor_tensor(out=Mc[:, t + 1, :], in0=Mc[:, t, :], in1=Mt[:, t, :], op=ALU.add))
            pRk = psum[:, 0:4, :].rearrange("p a b -> p (a b)")
            b_pRk = Buf()
            PER = 512 // NE
            for t in range(T_):
                o_ = pRk[:, (t // PER) * 512 + (t % PER) * NE:(t // PER) * 512 + (t % PER + 1) * NE]
                A("pe", lambda e, t=t, o_=o_: e.matmul(o_, lhsT=tri, rhs=Mt[:, t, :], start=True, stop=False),
                  [bE, b_cst], [b_pRk])
                A("pe", lambda e, t=t, o_=o_: e.matmul(o_, lhsT=ones, rhs=Mc[:, t, :], start=False, stop=True),
                  [bE, b_cst], [b_pRk])
            pC = pbank(4); b_pC = Buf()
            A("pe", lambda e: e.matmul(pC[:, 0:NE], lhsT=ones, rhs=Mc[:, T_, :], start=True, stop=True), [bE, b_cst], [b_pC])
            for t in range(T_):
                o_ = pRk[:, (t // PER) * 512 + (t % PER) * NE:(t // PER) * 512 + (t % PER + 1) * NE]
                dv(lambda e, t=t, o_=o_: e.tensor_copy(out=rank[:, t, :], in_=o_), r=[b_pRk, bE])
            dv(lambda e: e.tensor_copy(out=cnts, in_=pC[:, 0:NE]), r=[b_pC, bE])
            dv(lambda e: e.tensor_tensor(out=cmp1, in0=thr, in1=cnts.unsqueeze(2).to_broadcast([128, NE, NBMAX]), op=ALU.is_lt))
            dv(lambda e: e.tensor_reduce(out=nblk, in_=cmp1, axis=AX.X, op=ALU.add))
            dv(lambda e: e.memset(onesf, 1.0))
            dv(lambda e: e.tensor_tensor_scan(out=pend, data0=onesf, data1=nblk, initial=0.0, op0=ALU.mult, op1=ALU.add))
            dv(lambda e: e.tensor_tensor(out=pst, in0=pend, in1=nblk, op=ALU.subtract))
            dv(lambda e: e.tensor_scalar(out=pst, in0=pst, scalar1=float(MB), scalar2=None, op0=ALU.mult))
            dv(lambda e: e.tensor_scalar(out=pend, in0=pend, scalar1=float(MB), scalar2=None, op0=ALU.mult))
            dv(lambda e: e.tensor_tensor(out=rank, in0=rank, in1=pst.unsqueeze(1).to_broadcast([128, T_, NE]), op=ALU.add))
            for s_ in range(2):
                dv(lambda e, s_=s_: e.tensor_tensor(out=E32[s_], in0=E32[s_], in1=rank, op=ALU.mult))
                dv(lambda e, s_=s_: e.tensor_reduce(out=dst_f[:, s_, :], in_=E32[s_], axis=AX.X, op=ALU.add))
            dv(lambda e: e.tensor_copy(out=dest_i, in_=dst_f), w=[bE, b_dest])
            dv(lambda e: e.tensor_tensor(out=cmp2, in0=bst, in1=pend.unsqueeze(1).to_broadcast([128, NB, NE]), op=ALU.is_ge))
            dv(lambda e: e.tensor_reduce(out=bexp, in_=cmp2, axis=AX.X, op=ALU.add))
            same = V([128, NB])
            dv(lambda e: e.tensor_scalar(out=bexp, in0=bexp, scalar1=float(NE - 1), scalar2=None, op0=ALU.min))
            dv(lambda e: e.memset(same, 0.0))
            if cfg.get("dedup", True):
                dv(lambda e: e.tensor_tensor(out=same[:, 1:NB], in0=bexp[:, 1:NB], in1=bexp[:, 0:NB - 1], op=ALU.is_equal))
            dv(lambda e: e.tensor_scalar(out=bexp, in0=bexp, scalar1=128.0, scalar2=None, op0=ALU.mult))
            dv(lambda e: e.tensor_scalar(out=bexp, in0=bexp, scalar1=iop[:, 0:1], scalar2=None, op0=ALU.add), r=[bE, b_iop])
            dv(lambda e: e.scalar_tensor_tensor(out=bexp, in0=same, scalar=float(NE * 128), in1=bexp, op0=ALU.mult, op1=ALU.add))
            dv(lambda e: e.tensor_copy(out=widx, in_=bexp), w=[bE, b_widx])
            if debug:
                DMA("sp", dbg["lall"], lall, [b_lall], [Buf()])
                DMA("sp", dbg["dest"], dest_i, [b_dest], [Buf()])
                DMA("sp", dbg["gate"], gates, [b_gates], [Buf()])
                DMA("sp", dbg["bexp"], widx, [b_widx], [Buf()])

        if cfg.get('maxph', 8) >= 5:
            ph5()
        def ph6():
            new_phase()
            hn = [carve([128, D], BF16) for _ in range(3)]; b_hn = [Buf() for _ in range(3)]
            for t in range(NTQ):
                s3 = t % 3
                DMA("sp", hn[s3], HN_d[t * 128:(t + 1) * 128, :], [b_HNd], [b_hn[s3]])
                for s_ in range(2):
                    S.add("pool", lambda e, t=t, s_=s_, s3=s3: e.indirect_dma_start(
                        out=XS_d, out_offset=bass.IndirectOffsetOnAxis(ap=dest_i[:, s_, t:t + 1], axis=0),
                        in_=hn[s3], in_offset=None), [b_hn[s3], b_dest], [b_XSd], dma=True)

        if cfg.get('maxph', 8) >= 6:
            ph6()
        def ph7():
            new_phase()
            wst = [carve([128, 8192], F32) for _ in range(3)]; b_wst = [Buf() for _ in range(3)]
            wg = carve([128, KC, 512], BF16); b_wg = Buf()
            wu = carve([128, KC, 512], BF16); b_wu = Buf()
            wd = carve([128, 4, D], BF16); b_wd = Buf()
            xs = [carve([128, D], BF16) for _ in range(2)]; b_xs = [Buf() for _ in range(2)]
            xsT = carve([128, KC, MB], BF16); b_xsT = Buf()
            sg_ = [carve([128, MB], F32) for _ in range(2)]; b_sg = [Buf() for _ in range(2)]
            hT = carve([128, 4, MB], BF16); b_hT = Buf()
            su_ = [carve([128, MB], F32) for _ in range(2)]; b_su = [Buf() for _ in range(2)]
            ysb = [carve([128, D], BF16) for _ in range(2)]; b_ysb = [Buf() for _ in range(2)]
            pT = pbank(0, 2, BF16); b_pT = Buf()
            pG = [pbank(2), pbank(3)]; b_pG = [Buf(), Buf()]
            pY = [pbank(4 + q) for q in range(4)]; b_pY = [Buf() for _ in range(4)]
            wsrc = [w_gate.rearrange("e (p k) n -> (e p) (k n)", k=KC), w_up.rearrange("e (p k) n -> (e p) (k n)", k=KC),
                    w_down.rearrange("e (p k) n -> (e p) (k n)", k=4)]
            wdst = [(wg, b_wg), (wu, b_wu), (wd, b_wd)]
            loads = [(b, m) for b in range(NB) for m in range(3)]
            breg = {}

            def wdma(j):
                b, m = loads[j]
                ws = m
                def fn(e, b=b, m=m, ws=ws):
                    if "r" not in breg:
                        breg["r"] = e.to_reg(NE * 128 - 1)
                    return e.indirect_dma_start(
                        out=wst[ws], out_offset=None, in_=wsrc[m],
                        in_offset=bass.IndirectOffsetOnAxis(ap=widx[:, b:b + 1], axis=0),
                        bounds_check=breg["r"], oob_is_err=False)
                S.add("pool", fn, [b_widx], [b_wst[ws]], dma=True)

            def wconv(j):
                b, m = loads[j]
                ws = m
                dflat = wdst[m][0].rearrange("p a b -> p (a b)")
                A("act", lambda e, ws=ws, dflat=dflat: e.activation(out=dflat[:, 0:2560], in_=wst[ws][:, 0:2560], func=ACTF.Copy),
                  [b_wst[ws]], [wdst[m][1]])
                A("dve", lambda e, ws=ws, dflat=dflat: e.tensor_copy(out=dflat[:, 2560:5632], in_=wst[ws][:, 2560:5632]),
                  [b_wst[ws]], [wdst[m][1]])
                A("pool", lambda e, ws=ws, dflat=dflat: e.tensor_copy(out=dflat[:, 5632:8192], in_=wst[ws][:, 5632:8192]),
                  [b_wst[ws]], [wdst[m][1]])
            wdma(0)
            wdma(1)
            wdma(2)
            for b in range(NB):
                for m in range(3):
                    j = b * 3 + m
                    wconv(j)
                    if j + 3 < len(loads):
                        wdma(j + 3)
                for half in range(2):
                    DMA("sp", xs[half], XS_d[b * MB + half * 128:b * MB + (half + 1) * 128, :], [b_XSd], [b_xs[half]])
                    xv = xs[half].rearrange("p (q k) -> p k q", k=KC)
                    for k in range(KC):
                        A("pe", lambda e, k=k, xv=xv: e.transpose(out=pT[:, k * 128:(k + 1) * 128], in_=xv[:, k, :],
                                                                  identity=ident), [b_xs[half], b_cst], [b_pT])
                    A("act", lambda e, half=half: e.activation(out=xsT[:, :, half * 128:(half + 1) * 128],
                                                               in_=pT.rearrange("p (k c) -> p k c", c=128), func=ACTF.Copy),
                      [b_pT], [b_xsT])
                for fc in range(4):
                    for m, wt_ in ((0, wg), (1, wu)):
                        wv = wt_.rearrange("p k (q f) -> p k f q", f=4)
                        for k in range(KC):
                            A("pe", lambda e, k=k, m=m, wv=wv, fc=fc: e.matmul(pG[m][:, 0:MB], lhsT=wv[:, k, fc, :],
                                                                             rhs=xsT[:, k, :], start=(k == 0),
                                                                             stop=(k == KC - 1)),
                              [wdst[m][1], b_xsT], [b_pG[m]])
                    s2 = fc % 2
                    A("act", lambda e, s2=s2: e.activation(out=sg_[s2], in_=pG[0][:, 0:MB], func=ACTF.Silu),
                      [b_pG[0]], [b_sg[s2]])
                    A("act", lambda e, s2=s2: e.activation(out=su_[s2], in_=pG[1][:, 0:MB], func=ACTF.Copy),
                      [b_pG[1]], [b_su[s2]])
                    A("dve", lambda e, s2=s2, fc=fc: e.tensor_tensor(out=hT[:, fc, :], in0=su_[s2], in1=sg_[s2],
                                                                    op=ALU.mult), [b_su[s2], b_sg[s2]], [b_hT])
                for half in range(2):
                    for cg in range(4):
                        for fc in range(4):
                            A("pe", lambda e, half=half, cg=cg, fc=fc: e.matmul(
                                pY[cg], lhsT=hT[:, fc, half * 128:(half + 1) * 128], rhs=wd[:, fc, cg * 512:(cg + 1) * 512],
                                start=(fc == 0), stop=(fc == 3)), [b_hT, b_wd], [b_pY[cg]])
                        if cg % 2 == 0:
                            A("act", lambda e, half=half, cg=cg: e.activation(out=ysb[half][:, cg * 512:(cg + 1) * 512],
                                                                              in_=pY[cg], func=ACTF.Copy),
                              [b_pY[cg]], [b_ysb[half]])
                        else:
                            A("dve", lambda e, half=half, cg=cg: e.tensor_copy(out=ysb[half][:, cg * 512:(cg + 1) * 512],
                                                                               in_=pY[cg]), [b_pY[cg]], [b_ysb[half]])
                    DMA("sp", YS_d[b * MB + half * 128:b * MB + (half + 1) * 128, :], ysb[half], [b_ysb[half]], [b_YSd])

        if cfg.get('maxph', 8) >= 7:
            ph7()
        def ph8():
            new_phase()
            gfin_bc = carve([128, D], F32); b_gfin = Buf()
            DMA("sp", gfin_bc, g_fin.partition_broadcast(128).rearrange("p o d -> p (o d)"), w=[b_gfin])
            hh = [carve([128, D], F32) for _ in range(2)]; b_hh = [Buf() for _ in range(2)]
            y1 = [carve([128, D], BF16) for _ in range(2)]; b_y1 = [Buf() for _ in range(2)]
            y2 = [carve([128, D], BF16) for _ in range(2)]; b_y2 = [Buf() for _ in range(2)]
            junk = carve([128, D], BF16); b_junk = Buf()
            ssb = [carve([128, 1], F32) for _ in range(2)]; b_ss = [Buf() for _ in range(2)]
            ob = [carve([128, D], F32) for _ in range(2)]; b_ob = [Buf() for _ in range(2)]
            b_out = Buf()
            for t in range(NTQ):
                s2 = t % 2
                DMA("sp", hh[s2], H_d[t * 128:(t + 1) * 128, :], [b_Hd], [b_hh[s2]])
                for s_, (yy, byy) in enumerate(((y1, b_y1), (y2, b_y2))):
                    S.add("pool", lambda e, t=t, s_=s_, yy=yy, s2=s2: e.indirect_dma_start(
                        out=yy[s2], out_offset=None, in_=YS_d,
                        in_offset=bass.IndirectOffsetOnAxis(ap=dest_i[:, s_, t:t + 1], axis=0)),
                        [b_YSd, b_dest], [byy[s2]], dma=True)
                A("dve", lambda e, t=t, s2=s2: e.scalar_tensor_tensor(out=hh[s2], in0=y1[s2], scalar=gates[:, 0, t:t + 1],
                                                                      in1=hh[s2], op0=ALU.mult, op1=ALU.add),
                  [b_y1[s2], b_hh[s2], b_gates], [b_hh[s2]])
                A("dve", lambda e, t=t, s2=s2: e.scalar_tensor_tensor(out=hh[s2], in0=y2[s2], scalar=gates[:, 1, t:t + 1],
                                                                      in1=hh[s2], op0=ALU.mult, op1=ALU.add),
                  [b_y2[s2], b_hh[s2], b_gates], [b_hh[s2]])
                A("act", lambda e, s2=s2: e.activation(out=junk, in_=hh[s2], func=ACTF.Square, accum_out=ssb[s2]),
                  [b_hh[s2]], [b_junk, b_ss[s2]])
                A("act", lambda e, s2=s2: e.activation(out=ssb[s2], in_=ssb[s2], func=ACTF.Sqrt, scale=1.0 / D, bias=EPS),
                  [b_ss[s2]], [b_ss[s2]])
                A("dve", lambda e, s2=s2: e.reciprocal(out=ssb[s2], in_=ssb[s2]), [b_ss[s2]], [b_ss[s2]])
                A("dve", lambda e, s2=s2: e.scalar_tensor_tensor(out=ob[s2], in0=hh[s2], scalar=ssb[s2][:, 0:1], in1=gfin_bc,
                                                               op0=ALU.mult, op1=ALU.mult),
                  [b_hh[s2], b_ss[s2], b_gfin], [b_ob[s2]])
                DMA("sp", out_d[t * 128:(t + 1) * 128, :], ob[s2], [b_ob[s2]], [b_out])
        if cfg.get('maxph', 8) >= 8:
            ph8()
        S.emit(st)
    return nc


def host_inputs(cfg, x, norm_mix_g, w_in, w_pool, pool_scale, lambda_q1, lambda_k1, lambda_q2, lambda_k2, subln_g,
                w_out, norm_ffn_g, w_grp, b_grp, w_exp, b_exp, w_gate, w_up, w_down, norm_final_g):
    S_ = cfg["S"]; NG = cfg["NG"]; NE = NG * 8
    NCH = S_ // 2048; NQ = NCH * 512; NTQ = NQ // 128; NTKV = S_ // 128
    NB = -(-(2 * NQ + NE * (MB - 1)) // MB); NBMAX = -(-NQ // MB)
    f32 = np.float32
    bf = ml_dtypes.bfloat16
    x = np.asarray(x, f32)
    inv = (500000.0 ** (-np.arange(0, 16, 2, dtype=f32) / f32(16))).astype(f32)
    pos = np.arange(S_, dtype=f32)
    ang = (pos[:, None] * inv[None, :]).astype(f32)
    cos8, sin8 = np.cos(ang).astype(f32), np.sin(ang).astype(f32)
    cs = np.concatenate([cos8, cos8, -sin8, sin8], axis=1).astype(f32)
    ident = np.eye(128, dtype=f32)
    tri = (np.arange(128)[:, None] < np.arange(128)[None, :]).astype(f32)
    cst = np.stack([ident, np.ones((128, 128), f32), tri], axis=1).astype(bf)
    s_i = np.arange(128)[:, None]; t_i = np.arange(128)[None, :]

    def band(w, first):
        cntv = np.minimum(t_i + 1, w) if first else w
        main = ((s_i <= t_i) & (s_i > t_i - w)).astype(f32) / cntv - (s_i == t_i).astype(f32)
        prev = ((s_i - 128 > t_i - w)).astype(f32) / w
        return main, prev
    thr = np.broadcast_to((np.arange(NBMAX, dtype=f32) * MB)[None, None, :], (128, NE, NBMAX)).copy()
    bst = np.broadcast_to((np.arange(NB, dtype=f32) * MB)[None, :, None], (128, NB, NE)).copy()
    iop = np.arange(128, dtype=f32).reshape(128, 1)
    lams = np.concatenate([np.asarray(a, f32).reshape(-1) for a in (lambda_q1, lambda_k1, lambda_q2, lambda_k2)]).reshape(1, 256)
    w_r = np.concatenate([np.asarray(w_grp, f32)[0][:, :NG], np.zeros((D, 8 - NG), f32)] +
                         [np.asarray(w_exp, f32)[0][g] for g in range(NG)], axis=1)
    b_r = np.concatenate([np.asarray(b_grp, f32)[0][:NG], np.full((8 - NG,), -1e30, f32)] +
                         [np.asarray(b_exp, f32)[0][g] for g in range(NG)]).reshape(1, -1).astype(f32)
    common = dict(
        cst=cst, thr=thr, bst=bst, iop=iop, norm_mix_g=np.asarray(norm_mix_g, f32).reshape(1, D),
        w_in=np.asarray(w_in, f32)[0], w_pool=np.asarray(w_pool, f32)[0],
        pool_scale=np.ascontiguousarray(np.asarray(pool_scale, f32).reshape(8, 128).T), lams=lams,
        subln_g=np.asarray(subln_g, f32).reshape(128, 1), w_out=np.asarray(w_out, f32)[0],
        norm_ffn_g=np.asarray(norm_ffn_g, f32).reshape(1, D), w_r=np.ascontiguousarray(w_r), b_r=b_r,
        w_gate=np.asarray(w_gate, f32)[0][:NE], w_up=np.asarray(w_up, f32)[0][:NE], w_down=np.asarray(w_down, f32)[0][:NE],
        norm_final_g=np.asarray(norm_final_g, f32).reshape(1, D))
    maps = []
    for c in range(8):
        b, j = c // 4, c % 4
        xkv = x[b]
        xq = np.zeros((NCH, 640, D), f32)
        qpos = np.zeros((NCH, 512), np.int64)
        for i in range(NCH):
            g0 = (4 * i + j) * 512
            lo = g0 - 128
            if lo >= 0:
                xq[i] = xkv[lo:g0 + 512]
            else:
                xq[i, 128:] = xkv[g0:g0 + 512]
            qpos[i] = np.arange(g0, g0 + 512)
        cs_kv = cs.reshape(NTKV, 128, 32).transpose(1, 0, 2)
        cs_q = cs[qpos.reshape(-1)].reshape(NTQ, 128, 32).transpose(1, 0, 2)
        masks = np.zeros((128, 16, 512), f32)
        for mi in range(16):
            crel, kb4 = mi // 4, mi % 4
            if crel < j:
                masks[:, mi, :] = 1.0
            elif crel == j:
                masks[:, mi, :] = ((kb4 * 128 + np.arange(128))[:, None] <= np.arange(512)[None, :]).astype(f32)
        bands = np.zeros((128, 3, 4, 128), f32)
        for gi, w in enumerate(WINS):
            mn, pv = band(w, False)
            bands[:, 0, gi], bands[:, 1, gi] = mn, pv
            bands[:, 2, gi] = band(w, True)[0] if j == 0 else mn
        m = dict(common)
        m.update(xkv=np.ascontiguousarray(xkv), xq=xq, cs_kv=np.ascontiguousarray(cs_kv), cs_q=np.ascontiguousarray(cs_q),
                 masks=masks.astype(bf), bands=bands.astype(bf))
        maps.append(m)
    return maps


def assemble(cfg, results, key="out"):
    S_ = cfg["S"]; NCH = S_ // 2048
    out = np.zeros((2, S_, D), np.float32)
    for c in range(8):
        b, j = c // 4, c % 4
        o = results[c][key]
        for i in range(NCH):
            g0 = (4 * i + j) * 512
            out[b, g0:g0 + 512] = o[i * 512:(i + 1) * 512]
    return out


def kernel(**inputs):
    cfg = dict(CFG)
    nc = build(cfg)
    maps = host_inputs(cfg, **inputs)
    res = run_bass_kernel_spmd(nc, maps, core_ids=list(range(8)))
    return assemble(cfg, res.results)
```

```python
from contextlib import ExitStack
import math
import numpy as np
import ml_dtypes
import concourse.bass as bass
import concourse.mybir as mybir
from concourse.bass_utils import run_bass_kernel_spmd

F32 = mybir.dt.float32
BF16 = mybir.dt.bfloat16
I32 = mybir.dt.int32
ACTF = mybir.ActivationFunctionType
ALU = mybir.AluOpType
AX = mybir.AxisListType

CFG = dict(S=16384, NG=8)
D = 2048
KC = 16
NH = 8
EPS = 1e-6
LAM_INIT = 0.8 - 0.6 * math.exp(0.0)
WINS = (2, 4, 8, 16)
MB = 256


class Buf:
    __slots__ = ("w", "r")

    def __init__(self):
        self.w = None
        self.r = []


class Op:
    __slots__ = ("eng", "fn", "deps", "signal", "sig", "dma", "dsem", "dval", "ring_wait")

    def __init__(self, eng, fn, dma):
        self.eng = eng
        self.fn = fn
        self.dma = dma
        self.deps = []
        self.signal = False
        self.sig = 0
        self.dsem = None
        self.dval = 0
        self.ring_wait = None


class Sched:
    ENGS = ("pe", "act", "dve", "pool", "sp")
    RING = {"sp": 8, "pool": 8, "act": 2}

    def __init__(self, nc):
        self.nc = nc
        self.q = {e: [] for e in self.ENGS}
        self.ndma = {e: 0 for e in self.ENGS}
        self.dma_since = []

    def add(self, eng, fn, reads=(), writes=(), dma=False, extra=()):
        op = Op(eng, fn, dma)
        raw = set()
        other = set(extra)
        for b in reads:
            if b.w is not None:
                raw.add(b.w)
        for b in writes:
            if b.w is not None:
                other.add(b.w)
            other.update(b.r)
        for b in reads:
            b.r.append(op)
        for b in writes:
            b.w = op
            b.r = []
        deps = []
        for d in raw | other:
            if d is op:
                continue
            if (not d.dma) and d.eng == eng and not dma and d not in extra:
                if eng == "pe" or d not in raw:
                    continue
            deps.append(d)
        op.deps = deps
        if dma:
            n = self.ndma[eng]
            self.ndma[eng] = n + 1
            K = self.RING[eng]
            op.dsem = n % K
            op.dval = 16 * (n // K + 1)
            if n >= K:
                op.ring_wait = (n % K, 16 * (n // K))
            self.dma_since.append(op)
        self.q[eng].append(op)
        return op

    def barrier(self):
        last = [self.q[e][-1] for e in ("pe", "act", "dve", "pool") if self.q[e]]
        last = [o for o in last if o.fn is not None]
        lasts = []
        for e in ("pe", "act", "dve", "pool"):
            for o in reversed(self.q[e]):
                if o.fn is not None and not o.dma:
                    lasts.append(o)
                    break
        dm = list(self.dma_since)
        self.dma_since = []
        for e in self.ENGS:
            op = Op(e, None, False)
            op.deps = [o for o in lasts if o.eng != e] + dm
            self.q[e].append(op)

    def emit(self, stack):
        nc = self.nc
        for e in self.ENGS:
            for op in self.q[e]:
                for d in op.deps:
                    if not d.dma:
                        d.signal = True
        for e in self.ENGS:
            c = 0
            for op in self.q[e]:
                if op.signal and not op.dma:
                    c += 1
                    op.sig = c
        csem = {e: stack.enter_context(nc.semaphore("c_" + e)) for e in ("pe", "act", "dve", "pool")}
        rsem = {e: [stack.enter_context(nc.semaphore("r_%s%d" % (e, i))) for i in range(self.RING[e])]
                for e in ("sp", "pool", "act")}
        block = stack.enter_context(nc.Block())
        engmap = {"pe": block.tensor, "act": block.scalar, "dve": block.vector, "pool": block.gpsimd,
                  "sp": block.sync}

        def mk(e):
            ops = self.q[e]

            def body(eng):
                waited = {}

                def wait(sem, key, val):
                    if waited.get(key, 0) >= val:
                        return
                    waited[key] = val
                    eng.wait_ge(sem, val)

                for op in ops:
                    for d in op.deps:
                        if d.dma:
                            wait(rsem[d.eng][d.dsem], (d.eng, d.dsem), d.dval)
                        else:
                            wait(csem[d.eng], d.eng, d.sig)
                    if op.ring_wait is not None:
                        wait(rsem[e][op.ring_wait[0]], (e, op.ring_wait[0]), op.ring_wait[1])
                    if op.fn is None:
                        continue
                    ins = op.fn(eng)
                    if op.dma:
                        ins.then_inc(rsem[e][op.dsem], 16)
                    elif op.signal:
                        ins.then_inc(csem[e], 1)
                if e in rsem:
                    n = self.ndma[e]
                    K = self.RING[e]
                    for s in range(min(n, K)):
                        wait(rsem[e][s], (e, s), 16 * ((n - 1 - s) // K + 1))
            return body

        for e in self.ENGS:
            engmap[e](mk(e))


def build(cfg, debug=False):
    S_ = cfg["S"]
    NG = cfg["NG"]
    NE = NG * 8
    NCH = S_ // 2048
    NQ = NCH * 512
    NTQ = NQ // 128
    NTKV = S_ // 128
    NR = 8 + NE
    NB = -(-(2 * NQ + NE * (MB - 1)) // MB)
    NBMAX = -(-NQ // MB)

    nc = bass.Bass("TRN2", target_bir_lowering=False)
    din = lambda n, s, dt=F32: nc.dram_tensor(n, list(s), dt, kind="ExternalInput").ap()
    dscr = lambda n, s, dt: nc.dram_tensor(n, list(s), dt, kind="Internal").ap()
    xkv = din("xkv", [S_, D])
    xq = din("xq", [NCH, 640, D])
    cs_kv_d = din("cs_kv", [128, NTKV, 32])
    cs_q_d = din("cs_q", [128, NTQ, 32])
    masks_d = din("masks", [128, 16, 512], BF16)
    bands_d = din("bands", [128, 3, 4, 128], BF16)
    cst_d = din("cst", [128, 3, 128], BF16)
    thr_d = din("thr", [128, NE, NBMAX])
    bst_d = din("bst", [128, NB, NE])
    iop_d = din("iop", [128, 1])
    g_mix = din("norm_mix_g", [1, D])
    w_in = din("w_in", [D, 4096])
    w_pool = din("w_pool", [4, 256, 256])
    pool_scale = din("pool_scale", [128, 8])
    lams = din("lams", [1, 256])
    subln_g = din("subln_g", [128, 1])
    w_out = din("w_out", [D, D])
    g_ffn = din("norm_ffn_g", [1, D])
    w_r = din("w_r", [D, NR])
    b_r = din("b_r", [1, NR])
    w_gate = din("w_gate", [NE, D, 512])
    w_up = din("w_up", [NE, D, 512])
    w_down = din("w_down", [NE, 512, D])
    g_fin = din("norm_final_g", [1, D])
    out_d = nc.dram_tensor("out", [NQ, D], F32, kind="ExternalOutput").ap()
    dbg = {}
    if debug:
        dbg["mixT"] = nc.dram_tensor("dbg_mixT", [16, 128, NQ], BF16, kind="ExternalOutput").ap()
        dbg["h"] = nc.dram_tensor("dbg_h", [NQ, D], F32, kind="ExternalOutput").ap()
        dbg["lall"] = nc.dram_tensor("dbg_lall", [128, NTQ, NR], F32, kind="ExternalOutput").ap()
        dbg["dest"] = nc.dram_tensor("dbg_dest", [128, 2, NTQ], I32, kind="ExternalOutput").ap()
        dbg["gate"] = nc.dram_tensor("dbg_gate", [128, 2, NTQ], F32, kind="ExternalOutput").ap()
        dbg["bexp"] = nc.dram_tensor("dbg_bexp", [128, NB], I32, kind="ExternalOutput").ap()

    KT_d = dscr("KT_d", [NH, 128, S_], BF16)
    V_d = dscr("V_d", [NH, 128, NTKV, 128], BF16)
    QT_d = dscr("QT_d", [NH, 128, NQ], BF16)
    MIXT_d = dbg["mixT"] if debug else dscr("MIXT_d", [16, 128, NQ], BF16)
    H_d = dbg["h"] if debug else dscr("H_d", [NQ, D], F32)
    HN_d = dscr("HN_d", [NQ, D], BF16)
    XS_d = dscr("XS_d", [NB * MB, D], BF16)
    YS_d = dscr("YS_d", [NB * MB, D], BF16)

    S = Sched(nc)
    A = S.add

    def DMA(q, out, in_, r=(), w=()):
        return S.add(q, lambda e: e.dma_start(out=out, in_=in_), r, w, dma=True)

    with ExitStack() as st:
        ARENA = 106000
        arena = st.enter_context(nc.sbuf_tensor("arena", [128, ARENA], BF16))
        psum = st.enter_context(nc.psum_tensor("psum", [128, 8, 512], F32))
        state = {"off": 0, "base": 0}

        def carve(shape, dt):
            n = int(np.prod(shape[1:]))
            nb = n * (2 if dt in (F32, I32) else 1)
            nb = (nb + 15) // 16 * 16
            o = state["off"]
            assert o + nb <= ARENA, ("SBUF arena overflow", o, nb)
            state["off"] = o + nb
            v = arena[:, o:o + nb]
            if dt != BF16:
                v = v.bitcast(dt)
            v = v[:, 0:n]
            if len(shape) == 3:
                v = v.rearrange("p (a b) -> p a b", b=shape[2])
            elif len(shape) == 4:
                v = v.rearrange("p (a b c) -> p a b c", b=shape[2], c=shape[3])
            return v

        def new_phase():
            S.barrier()
            state["off"] = state["base"]

        def pbank(b, n=1, dt=F32):
            v = psum[:, b:b + n, :].rearrange("p a b -> p (a b)")
            if dt != F32:
                v = v.bitcast(dt)
            return v

        cst = carve([128, 3, 128], BF16); b_cst = Buf()
        ident, ones, tri = cst[:, 0, :], cst[:, 1, :], cst[:, 2, :]
        gmix_bc = carve([128, D], F32); b_gmix = Buf()
        lall = carve([128, NTQ, NR], F32); b_lall = Buf()
        neglam = carve([128, 1], F32); b_neglam = Buf()
        gsc = carve([128, 1], F32); b_gsc = Buf()
        iop = carve([128, 1], F32); b_iop = Buf()
        dest_i = carve([128, 2, NTQ], I32); b_dest = Buf()
        gates = carve([128, 2, NTQ], F32); b_gates = Buf()
        widx = carve([128, NB], I32); b_widx = Buf()
        small = carve([128, 8], F32)
        epsb = carve([128, 1], F32); b_epsb = Buf()
        A("dve", lambda e: e.memset(epsb, EPS), (), [b_epsb])
        DMA("sp", cst, cst_d, w=[b_cst])
        DMA("sp", gmix_bc, g_mix.partition_broadcast(128).rearrange("p o d -> p (o d)"), w=[b_gmix])
        DMA("sp", iop, iop_d, w=[b_iop])
        state["base"] = state["off"]

        lam_t = carve([128, 256], F32); b_lamt = Buf()
        lam_p = carve([128, 128], F32); b_lamp = Buf()
        lam_s = carve([128, 2], F32); b_lams = Buf()
        DMA("sp", lam_t, lams.partition_broadcast(128).rearrange("p o d -> p (o d)"), w=[b_lamt])
        lv = lam_t.rearrange("p (a b c) -> p a b c", a=2, b=2)
        A("dve", lambda e: e.tensor_tensor(out=lam_p.rearrange("p (a c) -> p a c", a=2), in0=lv[:, :, 0, :],
                                           in1=lv[:, :, 1, :], op=ALU.mult), [b_lamt], [b_lamp])
        A("dve", lambda e: e.tensor_reduce(out=lam_s, in_=lam_p.rearrange("p (a c) -> p a c", a=2), axis=AX.X,
                                           op=ALU.add), [b_lamp], [b_lams])
        A("act", lambda e: e.activation(out=lam_s, in_=lam_s, func=ACTF.Exp), [b_lams], [b_lams])
        A("dve", lambda e: e.scalar_tensor_tensor(out=neglam, in0=lam_s[:, 1:2], scalar=-LAM_INIT, in1=lam_s[:, 0:1],
                                                  op0=ALU.add, op1=ALU.subtract), [b_lams], [b_neglam])
        DMA("sp", gsc, subln_g, w=[b_gsc])
        A("dve", lambda e: e.tensor_scalar(out=gsc, in0=gsc, scalar1=1.0 - LAM_INIT, scalar2=None, op0=ALU.mult),
          [b_gsc], [b_gsc])

        def norm_transpose(src, xt, b_xt, junk, b_junk, ss, b_ss, xb, b_xb, xT, b_xT, gbc, b_gbc, pT, b_pT):
            if src is not None:
                DMA("sp", xt, src, w=[b_xt])
            A("act", lambda e: e.activation(out=junk, in_=xt, func=ACTF.Square, accum_out=ss), [b_xt], [b_junk, b_ss])
            A("act", lambda e: e.activation(out=ss, in_=ss, func=ACTF.Sqrt, scale=1.0 / D, bias=EPS), [b_ss], [b_ss])
            A("dve", lambda e: e.reciprocal(out=ss, in_=ss), [b_ss], [b_ss])
            A("dve", lambda e: e.scalar_tensor_tensor(out=xb, in0=xt, scalar=ss[:, 0:1], in1=gbc, op0=ALU.mult,
                                                      op1=ALU.mult), [b_xt, b_ss, b_gbc], [b_xb])
            if xT is None:
                return
            for k in range(KC):
                A("pe", lambda e, k=k: e.transpose(out=pT[:, k * 128:(k + 1) * 128], in_=xb[:, k * 128:(k + 1) * 128],
                                                   identity=ident), [b_xb, b_cst], [b_pT])
            A("act", lambda e: e.activation(out=xT[:, 0:8, :].rearrange("p a b -> p (a b)"), in_=pT[:, 0:1024],
                                            func=ACTF.Copy), [b_pT], [b_xT])
            A("dve", lambda e: e.tensor_copy(out=xT[:, 8:16, :].rearrange("p a b -> p (a b)"), in_=pT[:, 1024:2048]),
              [b_pT], [b_xT])

        def rope(pk, cs_t, ksb, tmp1, tmp2, rb, wb, b_tmp):
            for hb in range(2):
                pv = pk[:, hb * 512:(hb + 1) * 512].rearrange("p (g d) -> p g d", d=64)
                kv = ksb[:, hb * 512:(hb + 1) * 512].rearrange("p (g d) -> p g d", d=64)
                t1 = tmp1[:, hb * 8:(hb + 1) * 8, :]
                t2 = tmp2[:, hb * 8:(hb + 1) * 8, :]
                cosb = cs_t[:, 0:16].unsqueeze(1).to_broadcast([128, 8, 16])
                s0 = cs_t[:, 16:24].unsqueeze(1).to_broadcast([128, 8, 8])
                s1 = cs_t[:, 24:32].unsqueeze(1).to_broadcast([128, 8, 8])
                import os as _os
                RV = 3
                A("act", lambda e, pv=pv, kv=kv: e.activation(out=kv[:, :, 16:64], in_=pv[:, :, 16:64], func=ACTF.Copy), rb, wb)
                if RV == 1:
                    continue
                if RV == 3:
                    t3 = tmp2[:, hb * 8:(hb + 1) * 8, :]
                    A("act", lambda e, pv=pv, t3=t3: e.activation(out=t3, in_=pv[:, :, 0:16], func=ACTF.Copy), rb, [b_tmp])
                    A("dve", lambda e, t1=t1, t3=t3, cosb=cosb: e.tensor_tensor(out=t1, in0=t3, in1=cosb, op=ALU.mult), rb + [b_tmp], [b_tmp])
                    A("dve", lambda e, kv=kv, t3=t3, s0=s0: e.tensor_tensor(out=kv[:, :, 0:8], in0=t3[:, :, 8:16], in1=s0, op=ALU.mult), rb + [b_tmp], wb)
                    A("dve", lambda e, kv=kv, t3=t3, s1=s1: e.tensor_tensor(out=kv[:, :, 8:16], in0=t3[:, :, 0:8], in1=s1, op=ALU.mult), rb + [b_tmp], wb)
                    A("dve", lambda e, kv=kv, t1=t1: e.tensor_tensor(out=kv[:, :, 0:16], in0=kv[:, :, 0:16], in1=t1, op=ALU.add), [b_tmp] + wb, wb)
                    continue
                if RV == 2:
                    A("dve", lambda e, pv=pv, t1=t1, t2=t2: e.tensor_tensor(out=t1, in0=pv[:, :, 0:16], in1=t2, op=ALU.mult), rb, [b_tmp])
                    A("dve", lambda e, kv=kv, t1=t1, t2=t2: e.tensor_tensor(out=kv[:, :, 0:16], in0=t1, in1=t2, op=ALU.add), [b_tmp], wb)
                    continue
                A("dve", lambda e, pv=pv, t1=t1, cosb=cosb: e.tensor_tensor(out=t1, in0=pv[:, :, 0:16], in1=cosb, op=ALU.mult), rb, [b_tmp])
                A("dve", lambda e, pv=pv, t2=t2, s0=s0: e.tensor_tensor(out=t2[:, :, 0:8], in0=pv[:, :, 8:16], in1=s0, op=ALU.mult), rb, [b_tmp])
                A("dve", lambda e, pv=pv, t2=t2, s1=s1: e.tensor_tensor(out=t2[:, :, 8:16], in0=pv[:, :, 0:8], in1=s1, op=ALU.mult), rb, [b_tmp])
                A("dve", lambda e, kv=kv, t1=t1, t2=t2: e.tensor_tensor(out=kv[:, :, 0:16], in0=t1, in1=t2, op=ALU.add), [b_tmp], wb)

        b_KTd = Buf(); b_Vd = Buf(); b_QTd = Buf(); b_MIXd = Buf(); b_Hd = Buf(); b_HNd = Buf(); b_XSd = Buf(); b_YSd = Buf()
        def ph1():
            wkv = carve([128, KC, 2048], BF16); b_wkv = [Buf() for _ in range(KC)]
            for k in range(KC):
                S.add("pool", lambda e, k=k: e.dma_start(out=wkv[:, k, :], in_=w_in[k * 128:(k + 1) * 128, 2048:4096]),
                      (), [b_wkv[k]], dma=True)
            cs_kv = carve([128, NTKV, 32], F32); b_cskv = Buf()
            DMA("sp", cs_kv, cs_kv_d, w=[b_cskv])
            xt = [carve([128, D], F32) for _ in range(3)]; b_xt = [Buf() for _ in range(3)]
            junk = carve([128, D], BF16); b_junk = Buf()
            ssb = [carve([128, 1], F32) for _ in range(2)]; b_ss = [Buf() for _ in range(2)]
            xb = [carve([128, D], BF16) for _ in range(2)]; b_xb = [Buf() for _ in range(2)]
            xT = [carve([128, KC, 128], BF16) for _ in range(2)]; b_xT = [Buf() for _ in range(2)]
            ksb = [carve([128, 1024], BF16) for _ in range(2)]; b_ksb = [Buf() for _ in range(2)]
            vsb = [carve([128, 4, 1024], BF16) for _ in range(2)]; b_vsb = [Buf() for _ in range(2)]
            kTs = [carve([128, NH, 512], BF16) for _ in range(2)]; b_kTs = [Buf() for _ in range(2)]
            tmp1 = carve([128, 16, 16], F32); tmp2 = carve([128, 16, 16], F32); b_tmp = Buf()
            pT = pbank(0, 2, BF16); b_pT = Buf()
            pK = pbank(2, 2); b_pK = Buf()
            pV = pbank(4, 2); b_pV = Buf()
            pKT = pbank(6, 1, BF16); b_pKT = Buf()
            def loadA(t):
                DMA("sp", xt[t % 3], xkv[t * 128:(t + 1) * 128, :], w=[b_xt[t % 3]])

            def frontA(t):
                s2 = t % 2
                norm_transpose(None, xt[t % 3], b_xt[t % 3], junk, b_junk, ssb[s2], b_ss[s2], xb[s2],
                               b_xb[s2], xT[s2], b_xT[s2], gmix_bc, b_gmix, pT, b_pT)
            loadA(0)
            loadA(1)
            frontA(0)
            for t in range(NTKV):
                s2 = t % 2
                g4 = (t // 4) % 2
                if t + 2 < NTKV:
                    loadA(t + 2)
                if t + 1 < NTKV:
                    frontA(t + 1)
                LV = cfg.get("lv", 9)
                if LV < 1:
                    continue
                for cg in range(4):
                    dst, bd = (pK, b_pK) if cg < 2 else (pV, b_pV)
                    for k in range(KC):
                        import os as _os
                        _N = int(_os.environ.get("EXPN", 512))
                        if _os.environ.get("WSRC"):
                            A("pe", lambda e, k=k, cg=cg, dst=dst, s2=s2: e.matmul(
                                dst[:, (cg % 2) * 512:(cg % 2) * 512 + _N], lhsT=xT[s2][:, k, :],
                                rhs=xb[s2][:, 0:_N], start=(k == 0), stop=(k == KC - 1)),
                              [b_xT[s2], b_xb[s2]], [bd])
                        else:
                            A("pe", lambda e, k=k, cg=cg, dst=dst, s2=s2: e.matmul(
                                dst[:, (cg % 2) * 512:(cg % 2) * 512 + _N], lhsT=xT[s2][:, k, :],
                                rhs=wkv[:, k, cg * 512:cg * 512 + _N], start=(k == 0), stop=(k == KC - 1)),
                              [b_xT[s2], b_wkv[k]], [bd])
                if LV < 2:
                    continue
                rope(pK, cs_kv[:, t, :], ksb[s2], tmp1, tmp2, [b_pK, b_cskv], [b_ksb[s2]], b_tmp)
                if LV < 3:
                    continue
                A("act", lambda e, t=t, g4=g4: e.activation(out=vsb[g4][:, t % 4, :], in_=pV, func=ACTF.Copy),
                  [b_pV], [b_vsb[g4]])
                if LV < 4:
                    continue
                for h in range(NH):
                    A("pe", lambda e, h=h, s2=s2: e.transpose(out=pKT[:, h * 128:(h + 1) * 128],
                                                              in_=ksb[s2][:, h * 128:(h + 1) * 128], identity=ident),
                      [b_ksb[s2], b_cst], [b_pKT])
                A("dve", lambda e, t=t, g4=g4: e.tensor_copy(out=kTs[g4][:, :, (t % 4) * 128:(t % 4 + 1) * 128],
                                                             in_=pKT.rearrange("p (h c) -> p h c", c=128)),
                  [b_pKT], [b_kTs[g4]])
                if t % 4 == 3 and LV >= 5:
                    t0 = (t // 4) * 4
                    DMA("pool", KT_d[:, :, t0 * 128:(t0 + 4) * 128].rearrange("h p c -> p h c"), kTs[g4], [b_kTs[g4]], [b_KTd])
                    for tt in range(4):
                        DMA("pool", V_d[:, :, t0 + tt, :].rearrange("h p e -> p h e"),
                            vsb[g4][:, tt, :].rearrange("p (h e) -> p h e", e=128), [b_vsb[g4]], [b_Vd])

        if cfg.get('maxph', 8) >= 1:
            ph1()
        def ph2():
            new_phase()
            wq = carve([128, KC, 2048], BF16); b_wq = [Buf() for _ in range(KC)]
            for k in range(KC):
                S.add("pool", lambda e, k=k: e.dma_start(out=wq[:, k, :], in_=w_in[k * 128:(k + 1) * 128, 0:2048]),
                      (), [b_wq[k]], dma=True)
            wp = carve([128, 8, 256], BF16); b_wp = Buf()
            S.add("pool", lambda e: e.dma_start(out=wp, in_=w_pool.rearrange("g (cc p) d -> p (g cc) d", p=128)),
                  (), [b_wp], dma=True)
            psc = carve([128, 8], F32); b_psc = Buf()
            DMA("sp", psc, pool_scale, w=[b_psc])
            bands = carve([128, 3, 4, 128], BF16); b_bands = Buf()
            DMA("sp", bands, bands_d, w=[b_bands])
            cs_q = carve([128, NTQ, 32], F32); b_csq = Buf()
            DMA("sp", cs_q, cs_q_d, w=[b_csq])
            xt = [carve([128, D], F32) for _ in range(2)]; b_xt = [Buf() for _ in range(2)]
            junk = carve([128, D], BF16); b_junk = Buf()
            ssb = [carve([128, 1], F32) for _ in range(2)]; b_ss = [Buf() for _ in range(2)]
            xb = [carve([128, D], BF16) for _ in range(2)]; b_xb = [Buf() for _ in range(2)]
            xT = [carve([128, KC, 128], BF16) for _ in range(2)]; b_xT = [Buf() for _ in range(2)]
            qsb = [carve([128, 1024], BF16) for _ in range(2)]; b_qsb = [Buf() for _ in range(2)]
            pin = [carve([128, 1024], BF16) for _ in range(3)]; b_pin = [Buf() for _ in range(3)]
            qTs = [carve([128, NH, 128], BF16) for _ in range(2)]; b_qTs = [Buf() for _ in range(2)]
            pldT = [carve([128, 8, 128], BF16) for _ in range(2)]; b_pldT = [Buf() for _ in range(2)]
            mxT = [carve([128, 8, 128], BF16) for _ in range(2)]; b_mxT = [Buf() for _ in range(2)]
            tmp1 = carve([128, 16, 16], F32); tmp2 = carve([128, 16, 16], F32); b_tmp = Buf()
            pT = pbank(0, 2, BF16); b_pT = Buf()
            pP = pbank(2, 2); b_pP = Buf()
            pQ = pbank(4, 2); b_pQ = Buf()
            pQT = pbank(6, 1, BF16); b_pQT = Buf()
            pM = pbank(7, 1); b_pM = Buf()
            tilesB = [(i, r) for i in range(NCH) for r in range(5)]

            def frontB(cn):
                i, r = tilesB[cn]
                s2 = cn % 2
                norm_transpose(xq[i, r * 128:(r + 1) * 128, :], xt[s2], b_xt[s2], junk, b_junk, ssb[s2], b_ss[s2],
                               xb[s2], b_xb[s2], xT[s2], b_xT[s2], gmix_bc, b_gmix, pT, b_pT)
            frontB(0)
            cnt = 0
            for i in range(NCH):
                for r in range(5):
                    s2 = cnt % 2
                    s3 = cnt % 3
                    sp3 = (cnt - 1) % 3
                    cnt += 1
                    if cnt < len(tilesB):
                        frontB(cnt)
                    for cg in range(2 if r == 0 else 4):
                        dst, bd = (pP, b_pP) if cg < 2 else (pQ, b_pQ)
                        for k in range(KC):
                            A("pe", lambda e, k=k, cg=cg, dst=dst, s2=s2: e.matmul(
                                dst[:, (cg % 2) * 512:(cg % 2 + 1) * 512], lhsT=xT[s2][:, k, :],
                                rhs=wq[:, k, cg * 512:(cg + 1) * 512], start=(k == 0), stop=(k == KC - 1)),
                              [b_xT[s2], b_wq[k]], [bd])
                    A("act", lambda e, s3=s3: e.activation(out=pin[s3], in_=pP, func=ACTF.Copy), [b_pP], [b_pin[s3]])
                    if r == 0:
                        continue
                    tq = i * 4 + (r - 1)
                    rope(pQ, cs_q[:, tq, :], qsb[s2], tmp1, tmp2, [b_pQ, b_csq], [b_qsb[s2]], b_tmp)
                    for h in range(NH):
                        A("pe", lambda e, h=h, s2=s2: e.transpose(out=pQT[:, h * 128:(h + 1) * 128],
                                                                  in_=qsb[s2][:, h * 128:(h + 1) * 128], identity=ident),
                          [b_qsb[s2], b_cst], [b_pQT])
                    A("dve", lambda e, s2=s2: e.tensor_copy(out=qTs[s2].rearrange("p h c -> p (h c)"), in_=pQT),
                      [b_pQT], [b_qTs[s2]])
                    DMA("pool", QT_d[:, :, tq * 128:(tq + 1) * 128].rearrange("h p c -> p h c"), qTs[s2], [b_qTs[s2]], [b_QTd])
                    bsel = 2 if (i == 0 and r == 1) else 0
                    for half in range(2):
                        for u in range(4):
                            gc = half * 4 + u
                            g = gc // 2
                            o_ = pM[:, u * 128:(u + 1) * 128]
                            A("pe", lambda e, gc=gc, g=g, o_=o_, s3=s3, bsel=bsel: e.matmul(
                                o_, lhsT=pin[s3][:, gc * 128:(gc + 1) * 128], rhs=bands[:, bsel, g, :], start=True, stop=False),
                              [b_pin[s3], b_bands], [b_pM])
                            A("pe", lambda e, gc=gc, g=g, o_=o_, sp3=sp3: e.matmul(
                                o_, lhsT=pin[sp3][:, gc * 128:(gc + 1) * 128], rhs=bands[:, 1, g, :], start=False, stop=True),
                              [b_pin[sp3], b_bands], [b_pM])
                        A("dve", lambda e, half=half, s2=s2: e.tensor_copy(
                            out=pldT[s2][:, half * 4:(half + 1) * 4, :].rearrange("p a b -> p (a b)"), in_=pM),
                          [b_pM], [b_pldT[s2]])
                    for half in range(2):
                        for u in range(4):
                            gd = half * 4 + u
                            g, dd = gd // 2, gd % 2
                            o_ = pM[:, u * 128:(u + 1) * 128]
                            for cc in range(2):
                                A("pe", lambda e, g=g, dd=dd, cc=cc, o_=o_, s2=s2: e.matmul(
                                    o_, lhsT=wp[:, g * 2 + cc, dd * 128:(dd + 1) * 128], rhs=pldT[s2][:, g * 2 + cc, :],
                                    start=(cc == 0), stop=(cc == 1)), [b_wp, b_pldT[s2]], [b_pM])
                        for u in range(4):
                            gd = half * 4 + u
                            A("act", lambda e, gd=gd, u=u, s2=s2: e.activation(
                                out=mxT[s2][:, gd, :], in_=pM[:, u * 128:(u + 1) * 128], func=ACTF.Identity,
                                scale=psc[:, gd:gd + 1]), [b_pM, b_psc], [b_mxT[s2]])
                    DMA("pool", MIXT_d[0:8, :, tq * 128:(tq + 1) * 128].rearrange("f p c -> p f c"), mxT[s2], [b_mxT[s2]], [b_MIXd])

        if cfg.get('maxph', 8) >= 2:
            ph2()
        def ph3():
            new_phase()
            NSEG = 4
            SEG = S_ // NSEG
            kt2 = [carve([128, S_], BF16) for _ in range(2)]; b_kt2 = [[Buf() for _ in range(NSEG)] for _ in range(2)]
            v2 = [carve([128, NTKV, 128], BF16) for _ in range(2)]; b_v2 = [[Buf() for _ in range(NSEG)] for _ in range(2)]
            qt_sb = [carve([128, NQ], BF16) for _ in range(2)]; b_qt = [Buf() for _ in range(2)]
            msk = carve([128, 16, 512], BF16); b_msk = Buf()
            DMA("sp", msk, masks_d, w=[b_msk])
            NPT = 5
            pt2 = [carve([128, 2, 512], BF16) for _ in range(NPT)]
            pt = [[pt2[k][:, c, :] for k in range(NPT)] for c in range(2)]
            _bp = [Buf() for _ in range(NPT)]
            b_pt = [_bp, _bp]
            sacc2 = [carve([128, 2, 512], BF16) for _ in range(2)]
            sacc = [[sacc2[g][:, c, :] for g in range(2)] for c in range(2)]
            _bs = [Buf() for _ in range(2)]
            b_sacc = [_bs, _bs]
            rr = [carve([128, 512], F32) for _ in range(2)]; b_rr = [Buf() for _ in range(2)]
            oo = [carve([128, 512], F32) for _ in range(2)]; b_oo = [Buf() for _ in range(2)]
            sq = carve([128, 512], BF16); b_sq = Buf()
            rs = carve([128, 512], F32); b_rs = Buf()
            at = [carve([128, 512], BF16) for _ in range(2)]; b_at = [Buf() for _ in range(2)]
            pS2 = [pbank(2 * s_, 2) for s_ in range(2)]
            pS = [[pS2[s_][:, c * 512:(c + 1) * 512] for s_ in range(2)] for c in range(2)]
            _bS = [Buf() for _ in range(2)]
            b_pS = [_bS, _bS]
            pO = [pbank(4 + c) for c in range(2)]; b_pO = [Buf() for _ in range(2)]
            pL = [pbank(6 + c) for c in range(2)]; b_pL = [Buf() for _ in range(2)]
            st3 = {"ecnt": 0}

            def head_load(h):
                hs = h % 2
                kt_sb, v_sb, b_kt, b_v = kt2[hs], v2[hs], b_kt2[hs], b_v2[hs]
                for sg in range(NSEG):
                    DMA("sp", kt_sb[:, sg * SEG:(sg + 1) * SEG], KT_d[h, :, sg * SEG:(sg + 1) * SEG], [b_KTd], [b_kt[sg]])
                    DMA("sp", v_sb[:, sg * SEG // 128:(sg + 1) * SEG // 128, :],
                        V_d[h, :, sg * SEG // 128:(sg + 1) * SEG // 128, :], [b_Vd], [b_v[sg]])
                DMA("sp", qt_sb[hs], QT_d[h], [b_QTd], [b_qt[hs]])

            def qk(n, u):
                h, i, kb, nkb = u
                hs = h % 2
                kt_sb, b_kt = kt2[hs], b_kt2[hs]
                sg = (kb * 128) // SEG
                sl = n % 2
                for c in range(2):
                    A("pe", lambda e, c=c, kb=kb, i=i, sl=sl, hs=hs, kt_sb=kt_sb: e.matmul(
                        pS[c][sl], lhsT=kt_sb[c * 64:(c + 1) * 64, kb * 128:(kb + 1) * 128],
                        rhs=qt_sb[hs][c * 64:(c + 1) * 64, i * 512:(i + 1) * 512], start=True, stop=True),
                      [b_kt[sg], b_qt[hs]], [b_pS[c][sl]])

            def ex(n, u):
                h, i, kb, nkb = u
                sl = n % 2
                ps3 = n % NPT
                A("act", lambda e, sl=sl, ps3=ps3: e.activation(out=pt2[ps3].rearrange("p c n -> p (c n)"), in_=pS2[sl],
                                                              func=ACTF.Exp, scale=0.125),
                  [b_pS[0][sl]], [b_pt[0][ps3]])
                if kb >= nkb - 16:
                    mi = kb - (nkb - 16)
                    A("dve", lambda e, ps3=ps3, mi=mi: e.tensor_tensor(
                        out=pt2[ps3], in0=pt2[ps3], in1=msk[:, mi, :].unsqueeze(1).to_broadcast([128, 2, 512]), op=ALU.mult),
                      [b_pt[0][ps3], b_msk], [b_pt[0][ps3]])

            def pv(n, u):
                h, i, kb, nkb = u
                v_sb, b_v = v2[h % 2], b_v2[h % 2]
                sg = (kb * 128) // SEG
                ps3 = n % NPT
                pp3 = (n - 1) % NPT
                g2 = (kb // 4) % 2
                for c in range(2):
                    A("pe", lambda e, c=c, kb=kb, ps3=ps3, nkb=nkb, v_sb=v_sb: e.matmul(
                        pO[c], lhsT=v_sb[:, kb, :], rhs=pt[c][ps3], start=(kb == 0), stop=(kb == nkb - 1)),
                      [b_v[sg], b_pt[c][ps3]], [b_pO[c]])
                fl = lambda v: v.rearrange("p c n -> p (c n)")
                if kb % 4 == 1:
                    A("dve", lambda e, ps3=ps3, pp3=pp3, g2=g2: e.tensor_tensor(
                        out=fl(sacc2[g2]), in0=fl(pt2[pp3]), in1=fl(pt2[ps3]), op=ALU.add),
                      [b_pt[0][pp3], b_pt[0][ps3]], [b_sacc[0][g2]])
                elif kb % 4 >= 2:
                    A("dve", lambda e, ps3=ps3, g2=g2: e.tensor_tensor(
                        out=fl(sacc2[g2]), in0=fl(sacc2[g2]), in1=fl(pt2[ps3]), op=ALU.add),
                      [b_sacc[0][g2], b_pt[0][ps3]], [b_sacc[0][g2]])
                if kb % 4 == 0 and kb > 0:
                    g2p = ((kb - 1) // 4) % 2
                    for c in range(2):
                        A("pe", lambda e, c=c, kb=kb, g2p=g2p: e.matmul(
                            pL[c], lhsT=ones, rhs=sacc[c][g2p], start=(kb == 4), stop=False),
                          [b_cst, b_sacc[c][g2p]], [b_pL[c]])
                if kb == nkb - 1:
                    for c in range(2):
                        A("pe", lambda e, c=c, kb=kb, g2=g2, nkb=nkb: e.matmul(
                            pL[c], lhsT=ones, rhs=sacc[c][g2], start=(nkb == 4), stop=True),
                          [b_cst, b_sacc[c][g2]], [b_pL[c]])

            def epi_a():
                for c in range(2):
                    A("act", lambda e, c=c: e.activation(out=rr[c], in_=pL[c], func=ACTF.Copy), [b_pL[c]], [b_rr[c]])
                    A("act", lambda e, c=c: e.activation(out=oo[c], in_=pO[c], func=ACTF.Copy), [b_pO[c]], [b_oo[c]])
                for c in range(2):
                    A("dve", lambda e, c=c: e.reciprocal(out=rr[c], in_=rr[c]), [b_rr[c]], [b_rr[c]])
                    A("dve", lambda e, c=c: e.tensor_tensor(out=oo[c], in0=oo[c], in1=rr[c], op=ALU.mult),
                      [b_oo[c], b_rr[c]], [b_oo[c]])
                A("dve", lambda e: e.scalar_tensor_tensor(out=oo[0], in0=oo[1], scalar=neglam[:, 0:1], in1=oo[0],
                                                          op0=ALU.mult, op1=ALU.add), [b_oo[0], b_oo[1], b_neglam], [b_oo[0]])
                A("dve", lambda e: e.tensor_tensor(out=sq, in0=oo[0], in1=oo[0], op=ALU.mult), [b_oo[0]], [b_sq])

            def epi_b(h, i, n):
                es = st3["ecnt"] % 2
                st3["ecnt"] += 1
                sl = (n + 1) % 2
                A("pe", lambda e, sl=sl: e.matmul(pS[0][sl], lhsT=ones, rhs=sq, start=True, stop=True),
                  [b_cst, b_sq], [b_pS[0][sl]])
                A("act", lambda e, sl=sl: e.activation(out=rs, in_=pS[0][sl], func=ACTF.Ln, scale=1.0 / 128, bias=epsb[:, 0:1]),
                  [b_pS[0][sl], b_epsb], [b_rs])
                A("act", lambda e: e.activation(out=rs, in_=rs, func=ACTF.Exp, scale=-0.5), [b_rs], [b_rs])
                A("dve", lambda e, es=es: e.scalar_tensor_tensor(out=at[es], in0=oo[0], scalar=gsc[:, 0:1], in1=rs,
                                                               op0=ALU.mult, op1=ALU.mult),
                  [b_oo[0], b_rs, b_gsc], [b_at[es]])
                DMA("sp", MIXT_d[8 + h, :, i * 512:(i + 1) * 512], at[es], [b_at[es]], [b_MIXd])

            gn = 0
            for h in range(NH):
                units = [(h, i, kb, 16 * (i + 1)) for i in range(NCH) for kb in range(16 * (i + 1))]
                N = len(units)
                if h == 0:
                    head_load(0)
                if h + 1 < NH:
                    head_load(h + 1)
                pend = []
                for n in range(N + 2):
                    if n < N:
                        qk(gn + n, units[n])
                        ex(gn + n, units[n])
                    m = n - 2
                    if m >= 0:
                        pv(gn + m, units[m])
                        _, i_, kb_, nkb_ = units[m]
                        if kb_ == nkb_ - 1:
                            epi_a()
                            pend.append((n + 4, h, i_))
                    while pend and (pend[0][0] <= n or n == N + 1):
                        _, hh_, ii_ = pend.pop(0)
                        epi_b(hh_, ii_, gn + n)
                gn += N

        if cfg.get('maxph', 8) >= 3:
            ph3()
        def ph4():
            new_phase()
            wo = carve([128, KC, D], BF16); b_wo = [Buf() for _ in range(KC)]
            for k in range(KC):
                S.add("pool", lambda e, k=k: e.dma_start(out=wo[:, k, :], in_=w_out[k * 128:(k + 1) * 128, :]),
                      (), [b_wo[k]], dma=True)
            wr = carve([128, KC, NR], BF16); b_wr = Buf()
            S.add("pool", lambda e: e.dma_start(out=wr, in_=w_r.rearrange("(k p) n -> p k n", p=128)), (), [b_wr], dma=True)
            br = carve([128, NR], F32); b_br = Buf()
            DMA("sp", br, b_r.partition_broadcast(128).rearrange("p o d -> p (o d)"), w=[b_br])
            gffn_bc = carve([128, D], F32); b_gffn = Buf()
            DMA("sp", gffn_bc, g_ffn.partition_broadcast(128).rearrange("p o d -> p (o d)"), w=[b_gffn])
            mT = [carve([128, KC, 128], BF16) for _ in range(2)]; b_mT = [Buf() for _ in range(2)]
            xt = [carve([128, D], F32) for _ in range(2)]; b_xt = [Buf() for _ in range(2)]
            hsb = [carve([128, D], F32) for _ in range(2)]; b_hsb = [Buf() for _ in range(2)]
            junk = carve([128, D], BF16); b_junk = Buf()
            ssb = [carve([128, 1], F32) for _ in range(2)]; b_ss = [Buf() for _ in range(2)]
            hn = [carve([128, D], BF16) for _ in range(2)]; b_hn = [Buf() for _ in range(2)]
            hnT = [carve([128, KC, 128], BF16) for _ in range(2)]; b_hnT = [Buf() for _ in range(2)]
            pH = [pbank(b) for b in range(4)]; b_pH = [Buf() for _ in range(4)]
            pT = pbank(4, 2, BF16); b_pT = Buf()
            pR = pbank(6); b_pR = Buf()
            def frontD(t):
                s2 = t % 2
                i, r = t // 4, t % 4
                DMA("sp", mT[s2], MIXT_d[:, :, t * 128:(t + 1) * 128].rearrange("f p c -> p f c"), [b_MIXd], [b_mT[s2]])
                DMA("sp", xt[s2], xq[i, (r + 1) * 128:(r + 2) * 128, :], w=[b_xt[s2]])
                for cg in range(4):
                    for k in range(KC):
                        A("pe", lambda e, k=k, cg=cg, s2=s2: e.matmul(pH[cg], lhsT=mT[s2][:, k, :],
                                                                      rhs=wo[:, k, cg * 512:(cg + 1) * 512],
                                                                      start=(k == 0), stop=(k == KC - 1)),
                          [b_mT[s2], b_wo[k]], [b_pH[cg]])
                    A("act", lambda e, cg=cg, s2=s2: e.activation(out=hsb[s2][:, cg * 512:(cg + 1) * 512], in_=pH[cg],
                                                                  func=ACTF.Copy), [b_pH[cg]], [b_hsb[s2]])
                    A("dve", lambda e, cg=cg, s2=s2: e.tensor_tensor(out=hsb[s2][:, cg * 512:(cg + 1) * 512],
                                                                     in0=hsb[s2][:, cg * 512:(cg + 1) * 512],
                                                                     in1=xt[s2][:, cg * 512:(cg + 1) * 512], op=ALU.add),
                      [b_hsb[s2], b_xt[s2]], [b_hsb[s2]])
                DMA("sp", H_d[t * 128:(t + 1) * 128, :], hsb[s2], [b_hsb[s2]], [b_Hd])
                A("act", lambda e, s2=s2: e.activation(out=junk, in_=hsb[s2], func=ACTF.Square, accum_out=ssb[s2]),
                  [b_hsb[s2]], [b_junk, b_ss[s2]])
                A("act", lambda e, s2=s2: e.activation(out=ssb[s2], in_=ssb[s2], func=ACTF.Sqrt, scale=1.0 / D, bias=EPS),
                  [b_ss[s2]], [b_ss[s2]])
                A("dve", lambda e, s2=s2: e.reciprocal(out=ssb[s2], in_=ssb[s2]), [b_ss[s2]], [b_ss[s2]])
                A("dve", lambda e, s2=s2: e.scalar_tensor_tensor(out=hn[s2], in0=hsb[s2], scalar=ssb[s2][:, 0:1], in1=gffn_bc,
                                                               op0=ALU.mult, op1=ALU.mult),
                  [b_hsb[s2], b_ss[s2], b_gffn], [b_hn[s2]])
                DMA("sp", HN_d[t * 128:(t + 1) * 128, :], hn[s2], [b_hn[s2]], [b_HNd])

            def backD(t):
                s2 = t % 2
                for k in range(KC):
                    A("pe", lambda e, k=k, s2=s2: e.transpose(out=pT[:, k * 128:(k + 1) * 128],
                                                              in_=hn[s2][:, k * 128:(k + 1) * 128], identity=ident),
                      [b_hn[s2], b_cst], [b_pT])
                A("act", lambda e, s2=s2: e.activation(out=hnT[s2].rearrange("p a b -> p (a b)"), in_=pT, func=ACTF.Copy),
                  [b_pT], [b_hnT[s2]])
                for k in range(KC):
                    A("pe", lambda e, k=k, s2=s2: e.matmul(pR[:, 0:NR], lhsT=hnT[s2][:, k, :], rhs=wr[:, k, :],
                                                           start=(k == 0), stop=(k == KC - 1)), [b_hnT[s2], b_wr], [b_pR])
                A("act", lambda e, t=t: e.activation(out=lall[:, t, :], in_=pR[:, 0:NR], func=ACTF.Copy), [b_pR], [b_lall])
                A("dve", lambda e, t=t: e.tensor_tensor(out=lall[:, t, :], in0=lall[:, t, :], in1=br, op=ALU.add),
                  [b_lall, b_br], [b_lall])


            frontD(0)
            for t in range(NTQ):
                if t + 1 < NTQ:
                    frontD(t + 1)
                backD(t)

        if cfg.get('maxph', 8) >= 4:
            ph4()
        def ph5():
            new_phase()
            T_ = NTQ
            V = lambda shape, dt=F32: carve(shape, dt)
            mg = V([128, T_]); bm = Buf()
            maskg = V([128, T_, 8])
            eg = V([128, T_, 8])
            sume = V([128, T_])
            prod = V([128, T_, 8, 8])
            sel = V([128, T_, 8])
            top8 = V([128, T_, 8])
            m1 = V([128, T_, 8]); m2 = V([128, T_, 8])
            dm = V([128, T_]); g1 = V([128, T_])
            E = [V([128, T_, NE], BF16) for _ in range(2)]
            E32 = [V([128, T_, NE]) for _ in range(2)]
            Mt = V([128, T_, NE], BF16)
            Mc = V([128, T_ + 1, NE], BF16)
            rank = V([128, T_, NE])
            cnts = V([128, NE]); nblk = V([128, NE]); pend = V([128, NE]); pst = V([128, NE])
            thr = V([128, NE, NBMAX]); cmp1 = V([128, NE, NBMAX])
            bst = V([128, NB, NE]); cmp2 = V([128, NB, NE]); bexp = V([128, NB])
            onesf = V([128, NE])
            dst_f = V([128, 2, T_])
            bE = Buf()
            DMA("sp", thr, thr_d, w=[bE])
            DMA("sp", bst, bst_d, w=[bE])
            lg = lall[:, :, 0:8]
            le = lall[:, :, 8:NR]
            RW = ([b_lall, bE], [bE])

            def dv(fn, r=RW[0], w=RW[1], eng="dve"):
                A(eng, fn, r, w)
            dv(lambda e: e.tensor_reduce(out=mg, in_=lg, axis=AX.X, op=ALU.max))
            dv(lambda e: e.tensor_tensor(out=maskg, in0=lg, in1=mg.unsqueeze(2).to_broadcast([128, T_, 8]), op=ALU.is_equal))
            dv(lambda e: e.tensor_tensor(out=eg, in0=lg, in1=mg.unsqueeze(2).to_broadcast([128, T_, 8]), op=ALU.subtract))
            dv(lambda e: e.activation(out=eg, in_=eg, func=ACTF.Exp), eng="act")
            dv(lambda e: e.tensor_reduce(out=sume, in_=eg, axis=AX.X, op=ALU.add))
            dv(lambda e: e.reciprocal(out=sume, in_=sume))
            if NG == 8:
                lev = le.rearrange("p t (g i) -> p t g i", i=8)
            else:
                lev = le.rearrange("p t (g i) -> p t g i", i=8)
            for g in range(NG):
                dv(lambda e, g=g: e.tensor_tensor(out=prod[:, :, g, :], in0=lev[:, :, g, :],
                                                  in1=maskg[:, :, g:g + 1].to_broadcast([128, T_, 8]), op=ALU.mult))
            dv(lambda e: e.tensor_copy(out=sel, in_=prod[:, :, 0, :]))
            for g in range(1, NG):
                dv(lambda e, g=g: e.tensor_tensor(out=sel, in0=sel, in1=prod[:, :, g, :], op=ALU.add))
            for t in range(T_):
                dv(lambda e, t=t: e.max(out=top8[:, t, :], in_=sel[:, t, :]))
            dv(lambda e: e.tensor_tensor(out=m1, in0=sel, in1=top8[:, :, 0:1].to_broadcast([128, T_, 8]), op=ALU.is_equal))
            dv(lambda e: e.tensor_tensor(out=m2, in0=sel, in1=top8[:, :, 1:2].to_broadcast([128, T_, 8]), op=ALU.is_equal))
            dv(lambda e: e.tensor_tensor(out=dm, in0=top8[:, :, 1], in1=top8[:, :, 0], op=ALU.subtract))
            dv(lambda e: e.activation(out=dm, in_=dm, func=ACTF.Exp), eng="act")
            dv(lambda e: e.tensor_scalar(out=dm, in0=dm, scalar1=1.0, scalar2=None, op0=ALU.add))
            dv(lambda e: e.reciprocal(out=g1, in_=dm))
            dv(lambda e: e.tensor_tensor(out=gates[:, 0, :], in0=g1, in1=sume, op=ALU.mult), w=[bE, b_gates])
            dv(lambda e: e.tensor_tensor(out=gates[:, 1, :], in0=sume, in1=gates[:, 0, :], op=ALU.subtract), w=[bE, b_gates])
            for s_, mm in ((0, m1), (1, m2)):
                for g in range(NG):
                    dv(lambda e, s_=s_, mm=mm, g=g: e.tensor_tensor(
                        out=E32[s_][:, :, g * 8:(g + 1) * 8], in0=mm,
                        in1=maskg[:, :, g:g + 1].to_broadcast([128, T_, 8]), op=ALU.mult))
                dv(lambda e, s_=s_: e.tensor_copy(out=E[s_], in_=E32[s_]))
            dv(lambda e: e.tensor_tensor(out=Mt, in0=E[0], in1=E[1], op=ALU.add))
            dv(lambda e: e.memset(Mc[:, 0, :], 0.0))
            for t in range(T_):
                dv(lambda e, t=t: e.tensor_tensor(out=Mc[:, t + 1, :], in0=Mc[:, t, :], in1=Mt[:, t, :], op=ALU.add))
            pRk = psum[:, 0:4, :].rearrange("p a b -> p (a b)")
            b_pRk = Buf()
            PER = 512 // NE
            for t in range(T_):
                o_ = pRk[:, (t // PER) * 512 + (t % PER) * NE:(t // PER) * 512 + (t % PER + 1) * NE]
                A("pe", lambda e, t=t, o_=o_: e.matmul(o_, lhsT=tri, rhs=Mt[:, t, :], start=True, stop=False),
                  [bE, b_cst], [b_pRk])
                A("pe", lambda e, t=t, o_=o_: e.matmul(o_, lhsT=ones, rhs=Mc[:, t, :], start=False, stop=True),
                  [bE, b_cst], [b_pRk])
            pC = pbank(4); b_pC = Buf()
            A("pe", lambda e: e.matmul(pC[:, 0:NE], lhsT=ones, rhs=Mc[:, T_, :], start=True, stop=True), [bE, b_cst], [b_pC])
            for t in range(T_):
                o_ = pRk[:, (t // PER) * 512 + (t % PER) * NE:(t // PER) * 512 + (t % PER + 1) * NE]
                dv(lambda e, t=t, o_=o_: e.tensor_copy(out=rank[:, t, :], in_=o_), r=[b_pRk, bE])
            dv(lambda e: e.tensor_copy(out=cnts, in_=pC[:, 0:NE]), r=[b_pC, bE])
            dv(lambda e: e.tensor_tensor(out=cmp1, in0=thr, in1=cnts.unsqueeze(2).to_broadcast([128, NE, NBMAX]), op=ALU.is_lt))
            dv(lambda e: e.tensor_reduce(out=nblk, in_=cmp1, axis=AX.X, op=ALU.add))
            dv(lambda e: e.memset(onesf, 1.0))
            dv(lambda e: e.tensor_tensor_scan(out=pend, data0=onesf, data1=nblk, initial=0.0, op0=ALU.mult, op1=ALU.add))
            dv(lambda e: e.tensor_tensor(out=pst, in0=pend, in1=nblk, op=ALU.subtract))
            dv(lambda e: e.tensor_scalar(out=pst, in0=pst, scalar1=float(MB), scalar2=None, op0=ALU.mult))
            dv(lambda e: e.tensor_scalar(out=pend, in0=pend, scalar1=float(MB), scalar2=None, op0=ALU.mult))
            dv(lambda e: e.tensor_tensor(out=rank, in0=rank, in1=pst.unsqueeze(1).to_broadcast([128, T_, NE]), op=ALU.add))
            for s_ in range(2):
                dv(lambda e, s_=s_: e.tensor_tensor(out=E32[s_], in0=E32[s_], in1=rank, op=ALU.mult))
                dv(lambda e, s_=s_: e.tensor_reduce(out=dst_f[:, s_, :], in_=E32[s_], axis=AX.X, op=ALU.add))
            dv(lambda e: e.tensor_copy(out=dest_i, in_=dst_f), w=[bE, b_dest])
            dv(lambda e: e.tensor_tensor(out=cmp2, in0=bst, in1=pend.unsqueeze(1).to_broadcast([128, NB, NE]), op=ALU.is_ge))
            dv(lambda e: e.tensor_reduce(out=bexp, in_=cmp2, axis=AX.X, op=ALU.add))
            same = V([128, NB])
            dv(lambda e: e.tensor_scalar(out=bexp, in0=bexp, scalar1=float(NE - 1), scalar2=None, op0=ALU.min))
            dv(lambda e: e.memset(same, 0.0))
            if cfg.get("dedup", True):
                dv(lambda e: e.tensor_tensor(out=same[:, 1:NB], in0=bexp[:, 1:NB], in1=bexp[:, 0:NB - 1], op=ALU.is_equal))
            dv(lambda e: e.tensor_scalar(out=bexp, in0=bexp, scalar1=128.0, scalar2=None, op0=ALU.mult))
            dv(lambda e: e.tensor_scalar(out=bexp, in0=bexp, scalar1=iop[:, 0:1], scalar2=None, op0=ALU.add), r=[bE, b_iop])
            dv(lambda e: e.scalar_tensor_tensor(out=bexp, in0=same, scalar=float(NE * 128), in1=bexp, op0=ALU.mult, op1=ALU.add))
            dv(lambda e: e.tensor_copy(out=widx, in_=bexp), w=[bE, b_widx])
            if debug:
                DMA("sp", dbg["lall"], lall, [b_lall], [Buf()])
                DMA("sp", dbg["dest"], dest_i, [b_dest], [Buf()])
                DMA("sp", dbg["gate"], gates, [b_gates], [Buf()])
                DMA("sp", dbg["bexp"], widx, [b_widx], [Buf()])

        if cfg.get('maxph', 8) >= 5:
            ph5()
        def ph6():
            new_phase()
            hn = [carve([128, D], BF16) for _ in range(3)]; b_hn = [Buf() for _ in range(3)]
            for t in range(NTQ):
                s3 = t % 3
                DMA("sp", hn[s3], HN_d[t * 128:(t + 1) * 128, :], [b_HNd], [b_hn[s3]])
                for s_ in range(2):
                    S.add("pool", lambda e, t=t, s_=s_, s3=s3: e.indirect_dma_start(
                        out=XS_d, out_offset=bass.IndirectOffsetOnAxis(ap=dest_i[:, s_, t:t + 1], axis=0),
                        in_=hn[s3], in_offset=None), [b_hn[s3], b_dest], [b_XSd], dma=True)

        if cfg.get('maxph', 8) >= 6:
            ph6()
        def ph7():
            new_phase()
            wst = [carve([128, 8192], F32) for _ in range(3)]; b_wst = [Buf() for _ in range(3)]
            wg = carve([128, KC, 512], BF16); b_wg = Buf()
            wu = carve([128, KC, 512], BF16); b_wu = Buf()
            wd = carve([128, 4, D], BF16); b_wd = Buf()
            xs = [carve([128, D], BF16) for _ in range(2)]; b_xs = [Buf() for _ in range(2)]
            xsT = carve([128, KC, MB], BF16); b_xsT = Buf()
            sg_ = [carve([128, MB], F32) for _ in range(2)]; b_sg = [Buf() for _ in range(2)]
            hT = carve([128, 4, MB], BF16); b_hT = Buf()
            su_ = [carve([128, MB], F32) for _ in range(2)]; b_su = [Buf() for _ in range(2)]
            ysb = [carve([128, D], BF16) for _ in range(2)]; b_ysb = [Buf() for _ in range(2)]
            pT = pbank(0, 2, BF16); b_pT = Buf()
            pG = [pbank(2), pbank(3)]; b_pG = [Buf(), Buf()]
            pY = [pbank(4 + q) for q in range(4)]; b_pY = [Buf() for _ in range(4)]
            wsrc = [w_gate.rearrange("e (p k) n -> (e p) (k n)", k=KC), w_up.rearrange("e (p k) n -> (e p) (k n)", k=KC),
                    w_down.rearrange("e (p k) n -> (e p) (k n)", k=4)]
            wdst = [(wg, b_wg), (wu, b_wu), (wd, b_wd)]
            loads = [(b, m) for b in range(NB) for m in range(3)]
            breg = {}

            def wdma(j):
                b, m = loads[j]
                ws = m
                def fn(e, b=b, m=m, ws=ws):
                    if "r" not in breg:
                        breg["r"] = e.to_reg(NE * 128 - 1)
                    return e.indirect_dma_start(
                        out=wst[ws], out_offset=None, in_=wsrc[m],
                        in_offset=bass.IndirectOffsetOnAxis(ap=widx[:, b:b + 1], axis=0),
                        bounds_check=breg["r"], oob_is_err=False)
                S.add("pool", fn, [b_widx], [b_wst[ws]], dma=True)

            def wconv(j):
                b, m = loads[j]
                ws = m
                dflat = wdst[m][0].rearrange("p a b -> p (a b)")
                A("act", lambda e, ws=ws, dflat=dflat: e.activation(out=dflat[:, 0:2560], in_=wst[ws][:, 0:2560], func=ACTF.Copy),
                  [b_wst[ws]], [wdst[m][1]])
                A("dve", lambda e, ws=ws, dflat=dflat: e.tensor_copy(out=dflat[:, 2560:5632], in_=wst[ws][:, 2560:5632]),
                  [b_wst[ws]], [wdst[m][1]])
                A("pool", lambda e, ws=ws, dflat=dflat: e.tensor_copy(out=dflat[:, 5632:8192], in_=wst[ws][:, 5632:8192]),
                  [b_wst[ws]], [wdst[m][1]])
            wdma(0)
            wdma(1)
            wdma(2)
            for b in range(NB):
                for m in range(3):
                    j = b * 3 + m
                    wconv(j)
                    if j + 3 < len(loads):
                        wdma(j + 3)
                for half in range(2):
                    DMA("sp", xs[half], XS_d[b * MB + half * 128:b * MB + (half + 1) * 128, :], [b_XSd], [b_xs[half]])
                    xv = xs[half].rearrange("p (q k) -> p k q", k=KC)
                    for k in range(KC):
                        A("pe", lambda e, k=k, xv=xv: e.transpose(out=pT[:, k * 128:(k + 1) * 128], in_=xv[:, k, :],
                                                                  identity=ident), [b_xs[half], b_cst], [b_pT])
                    A("act", lambda e, half=half: e.activation(out=xsT[:, :, half * 128:(half + 1) * 128],
                                                               in_=pT.rearrange("p (k c) -> p k c", c=128), func=ACTF.Copy),
                      [b_pT], [b_xsT])
                for fc in range(4):
                    for m, wt_ in ((0, wg), (1, wu)):
                        wv = wt_.rearrange("p k (q f) -> p k f q", f=4)
                        for k in range(KC):
                            A("pe", lambda e, k=k, m=m, wv=wv, fc=fc: e.matmul(pG[m][:, 0:MB], lhsT=wv[:, k, fc, :],
                                                                             rhs=xsT[:, k, :], start=(k == 0),
                                                                             stop=(k == KC - 1)),
                              [wdst[m][1], b_xsT], [b_pG[m]])
                    s2 = fc % 2
                    A("act", lambda e, s2=s2: e.activation(out=sg_[s2], in_=pG[0][:, 0:MB], func=ACTF.Silu),
                      [b_pG[0]], [b_sg[s2]])
                    A("act", lambda e, s2=s2: e.activation(out=su_[s2], in_=pG[1][:, 0:MB], func=ACTF.Copy),
                      [b_pG[1]], [b_su[s2]])
                    A("dve", lambda e, s2=s2, fc=fc: e.tensor_tensor(out=hT[:, fc, :], in0=su_[s2], in1=sg_[s2],
                                                                    op=ALU.mult), [b_su[s2], b_sg[s2]], [b_hT])
                for half in range(2):
                    for cg in range(4):
                        for fc in range(4):
                            A("pe", lambda e, half=half, cg=cg, fc=fc: e.matmul(
                                pY[cg], lhsT=hT[:, fc, half * 128:(half + 1) * 128], rhs=wd[:, fc, cg * 512:(cg + 1) * 512],
                                start=(fc == 0), stop=(fc == 3)), [b_hT, b_wd], [b_pY[cg]])
                        if cg % 2 == 0:
                            A("act", lambda e, half=half, cg=cg: e.activation(out=ysb[half][:, cg * 512:(cg + 1) * 512],
                                                                              in_=pY[cg], func=ACTF.Copy),
                              [b_pY[cg]], [b_ysb[half]])
                        else:
                            A("dve", lambda e, half=half, cg=cg: e.tensor_copy(out=ysb[half][:, cg * 512:(cg + 1) * 512],
                                                                               in_=pY[cg]), [b_pY[cg]], [b_ysb[half]])
                    DMA("sp", YS_d[b * MB + half * 128:b * MB + (half + 1) * 128, :], ysb[half], [b_ysb[half]], [b_YSd])

        if cfg.get('maxph', 8) >= 7:
            ph7()
        def ph8():
            new_phase()
            gfin_bc = carve([128, D], F32); b_gfin = Buf()
            DMA("sp", gfin_bc, g_fin.partition_broadcast(128).rearrange("p o d -> p (o d)"), w=[b_gfin])
            hh = [carve([128, D], F32) for _ in range(2)]; b_hh = [Buf() for _ in range(2)]
            y1 = [carve([128, D], BF16) for _ in range(2)]; b_y1 = [Buf() for _ in range(2)]
            y2 = [carve([128, D], BF16) for _ in range(2)]; b_y2 = [Buf() for _ in range(2)]
            junk = carve([128, D], BF16); b_junk = Buf()
            ssb = [carve([128, 1], F32) for _ in range(2)]; b_ss = [Buf() for _ in range(2)]
            ob = [carve([128, D], F32) for _ in range(2)]; b_ob = [Buf() for _ in range(2)]
            b_out = Buf()
            for t in range(NTQ):
                s2 = t % 2
                DMA("sp", hh[s2], H_d[t * 128:(t + 1) * 128, :], [b_Hd], [b_hh[s2]])
                for s_, (yy, byy) in enumerate(((y1, b_y1), (y2, b_y2))):
                    S.add("pool", lambda e, t=t, s_=s_, yy=yy, s2=s2: e.indirect_dma_start(
                        out=yy[s2], out_offset=None, in_=YS_d,
                        in_offset=bass.IndirectOffsetOnAxis(ap=dest_i[:, s_, t:t + 1], axis=0)),
                        [b_YSd, b_dest], [byy[s2]], dma=True)
                A("dve", lambda e, t=t, s2=s2: e.scalar_tensor_tensor(out=hh[s2], in0=y1[s2], scalar=gates[:, 0, t:t + 1],
                                                                      in1=hh[s2], op0=ALU.mult, op1=ALU.add),
                  [b_y1[s2], b_hh[s2], b_gates], [b_hh[s2]])
                A("dve", lambda e, t=t, s2=s2: e.scalar_tensor_tensor(out=hh[s2], in0=y2[s2], scalar=gates[:, 1, t:t + 1],
                                                                      in1=hh[s2], op0=ALU.mult, op1=ALU.add),
                  [b_y2[s2], b_hh[s2], b_gates], [b_hh[s2]])
                A("act", lambda e, s2=s2: e.activation(out=junk, in_=hh[s2], func=ACTF.Square, accum_out=ssb[s2]),
                  [b_hh[s2]], [b_junk, b_ss[s2]])
                A("act", lambda e, s2=s2: e.activation(out=ssb[s2], in_=ssb[s2], func=ACTF.Sqrt, scale=1.0 / D, bias=EPS),
                  [b_ss[s2]], [b_ss[s2]])
                A("dve", lambda e, s2=s2: e.reciprocal(out=ssb[s2], in_=ssb[s2]), [b_ss[s2]], [b_ss[s2]])
                A("dve", lambda e, s2=s2: e.scalar_tensor_tensor(out=ob[s2], in0=hh[s2], scalar=ssb[s2][:, 0:1], in1=gfin_bc,
                                                               op0=ALU.mult, op1=ALU.mult),
                  [b_hh[s2], b_ss[s2], b_gfin], [b_ob[s2]])
                DMA("sp", out_d[t * 128:(t + 1) * 128, :], ob[s2], [b_ob[s2]], [b_out])
        if cfg.get('maxph', 8) >= 8:
            ph8()
        S.emit(st)
    return nc


def host_inputs(cfg, x, norm_mix_g, w_in, w_pool, pool_scale, lambda_q1, lambda_k1, lambda_q2, lambda_k2, subln_g,
                w_out, norm_ffn_g, w_grp, b_grp, w_exp, b_exp, w_gate, w_up, w_down, norm_final_g):
    S_ = cfg["S"]; NG = cfg["NG"]; NE = NG * 8
    NCH = S_ // 2048; NQ = NCH * 512; NTQ = NQ // 128; NTKV = S_ // 128
    NB = -(-(2 * NQ + NE * (MB - 1)) // MB); NBMAX = -(-NQ // MB)
    f32 = np.float32
    bf = ml_dtypes.bfloat16
    x = np.asarray(x, f32)
    inv = (500000.0 ** (-np.arange(0, 16, 2, dtype=f32) / f32(16))).astype(f32)
    pos = np.arange(S_, dtype=f32)
    ang = (pos[:, None] * inv[None, :]).astype(f32)
    cos8, sin8 = np.cos(ang).astype(f32), np.sin(ang).astype(f32)
    cs = np.concatenate([cos8, cos8, -sin8, sin8], axis=1).astype(f32)
    ident = np.eye(128, dtype=f32)
    tri = (np.arange(128)[:, None] < np.arange(128)[None, :]).astype(f32)
    cst = np.stack([ident, np.ones((128, 128), f32), tri], axis=1).astype(bf)
    s_i = np.arange(128)[:, None]; t_i = np.arange(128)[None, :]

    def band(w, first):
        cntv = np.minimum(t_i + 1, w) if first else w
        main = ((s_i <= t_i) & (s_i > t_i - w)).astype(f32) / cntv - (s_i == t_i).astype(f32)
        prev = ((s_i - 128 > t_i - w)).astype(f32) / w
        return main, prev
    thr = np.broadcast_to((np.arange(NBMAX, dtype=f32) * MB)[None, None, :], (128, NE, NBMAX)).copy()
    bst = np.broadcast_to((np.arange(NB, dtype=f32) * MB)[None, :, None], (128, NB, NE)).copy()
    iop = np.arange(128, dtype=f32).reshape(128, 1)
    lams = np.concatenate([np.asarray(a, f32).reshape(-1) for a in (lambda_q1, lambda_k1, lambda_q2, lambda_k2)]).reshape(1, 256)
    w_r = np.concatenate([np.asarray(w_grp, f32)[0][:, :NG], np.zeros((D, 8 - NG), f32)] +
                         [np.asarray(w_exp, f32)[0][g] for g in range(NG)], axis=1)
    b_r = np.concatenate([np.asarray(b_grp, f32)[0][:NG], np.full((8 - NG,), -1e30, f32)] +
                         [np.asarray(b_exp, f32)[0][g] for g in range(NG)]).reshape(1, -1).astype(f32)
    common = dict(
        cst=cst, thr=thr, bst=bst, iop=iop, norm_mix_g=np.asarray(norm_mix_g, f32).reshape(1, D),
        w_in=np.asarray(w_in, f32)[0], w_pool=np.asarray(w_pool, f32)[0],
        pool_scale=np.ascontiguousarray(np.asarray(pool_scale, f32).reshape(8, 128).T), lams=lams,
        subln_g=np.asarray(subln_g, f32).reshape(128, 1), w_out=np.asarray(w_out, f32)[0],
        norm_ffn_g=np.asarray(norm_ffn_g, f32).reshape(1, D), w_r=np.ascontiguousarray(w_r), b_r=b_r,
        w_gate=np.asarray(w_gate, f32)[0][:NE], w_up=np.asarray(w_up, f32)[0][:NE], w_down=np.asarray(w_down, f32)[0][:NE],
        norm_final_g=np.asarray(norm_final_g, f32).reshape(1, D))
    maps = []
    for c in range(8):
        b, j = c // 4, c % 4
        xkv = x[b]
        xq = np.zeros((NCH, 640, D), f32)
        qpos = np.zeros((NCH, 512), np.int64)
        for i in range(NCH):
            g0 = (4 * i + j) * 512
            lo = g0 - 128
            if lo >= 0:
                xq[i] = xkv[lo:g0 + 512]
            else:
                xq[i, 128:] = xkv[g0:g0 + 512]
            qpos[i] = np.arange(g0, g0 + 512)
        cs_kv = cs.reshape(NTKV, 128, 32).transpose(1, 0, 2)
        cs_q = cs[qpos.reshape(-1)].reshape(NTQ, 128, 32).transpose(1, 0, 2)
        masks = np.zeros((128, 16, 512), f32)
        for mi in range(16):
            crel, kb4 = mi // 4, mi % 4
            if crel < j:
                masks[:, mi, :] = 1.0
            elif crel == j:
                masks[:, mi, :] = ((kb4 * 128 + np.arange(128))[:, None] <= np.arange(512)[None, :]).astype(f32)
        bands = np.zeros((128, 3, 4, 128), f32)
        for gi, w in enumerate(WINS):
            mn, pv = band(w, False)
            bands[:, 0, gi], bands[:, 1, gi] = mn, pv
            bands[:, 2, gi] = band(w, True)[0] if j == 0 else mn
        m = dict(common)
        m.update(xkv=np.ascontiguousarray(xkv), xq=xq, cs_kv=np.ascontiguousarray(cs_kv), cs_q=np.ascontiguousarray(cs_q),
                 masks=masks.astype(bf), bands=bands.astype(bf))
        maps.append(m)
    return maps


def assemble(cfg, results, key="out"):
    S_ = cfg["S"]; NCH = S_ // 2048
    out = np.zeros((2, S_, D), np.float32)
    for c in range(8):
        b, j = c // 4, c % 4
        o = results[c][key]
        for i in range(NCH):
            g0 = (4 * i + j) * 512
            out[b, g0:g0 + 512] = o[i * 512:(i + 1) * 512]
    return out


def kernel(**inputs):
    cfg = dict(CFG)
    nc = build(cfg)
    maps = host_inputs(cfg, **inputs)
    res = run_bass_kernel_spmd(nc, maps, core_ids=list(range(8)))
    return assemble(cfg, res.results)
```
